# Optimizing a Trainium2 kernel written in Bass

```python
import jax, jax.numpy as jnp
from jax import lax
import numpy as np

D_MODEL = 1024
BATCH = 8
SEQ = 8192
DEPTH = 2

EPS = 1e-6
GLA_HEADS = 4
GLA_DK = 128
GLA_DV = 256
GLA_GATE_RANK = 16
GLA_GATE_TAU = 16.0
GLA_CHUNK = 64
GLA_PROJ = GLA_HEADS * (2 * GLA_DK + 2 * GLA_DV) + GLA_GATE_RANK
SWA_HEADS = 16
SWA_KV_HEADS = 4
SWA_GROUP = SWA_HEADS // SWA_KV_HEADS
SWA_HEAD_DIM = 64
SWA_WINDOW = 128
SWA_BLOCK = 128
ROT_DIM = SWA_HEAD_DIM // 4
ROPE_THETA = 500000.0
PEER_HEADS = 8
PEER_N_KEYS = 128
PEER_EXPERTS = PEER_N_KEYS * PEER_N_KEYS
PEER_TOPK = 16
PEER_QDIM = 256
PEER_BLOCK = 128

kernel_name = "yoco_gla_swa_sink_peer_block"


def rmsnorm(x, g):
    x32 = x.astype(jnp.float32)
    y = x32 * lax.rsqrt(jnp.mean(x32 * x32, axis=-1, keepdims=True) + EPS)
    return (y * g.astype(jnp.float32)).astype(x.dtype)


def modulate(x, g, shift, scale):
    return rmsnorm(x, g) * (1.0 + scale[:, None, :]) + shift[:, None, :]


def rope_tables(positions):
    inv = ROPE_THETA ** (-jnp.arange(0, ROT_DIM, 2, dtype=jnp.float32) / ROT_DIM)
    ang = positions.astype(jnp.float32)[..., None] * inv
    return jnp.cos(ang), jnp.sin(ang)


def apply_partial_rope(x, cos, sin):
    half = ROT_DIM // 2
    xr = x[..., :ROT_DIM].astype(jnp.float32)
    x1, x2 = xr[..., :half], xr[..., half:]
    c = cos[:, :, None, :]
    s = sin[:, :, None, :]
    rot = jnp.concatenate([x1 * c - x2 * s, x2 * c + x1 * s], axis=-1).astype(x.dtype)
    return jnp.concatenate([rot, x[..., ROT_DIM:]], axis=-1)


def gla_mixer(h, w_in, w_g2, b_g2, norm_g, w_out):
    bsz, s_len, _ = h.shape
    H, dk, dv, C = GLA_HEADS, GLA_DK, GLA_DV, GLA_CHUNK
    n_c = s_len // C
    proj = h @ w_in
    q, k, v, og, gl = jnp.split(proj, [H * dk, 2 * H * dk, 2 * H * dk + H * dv, 2 * H * dk + 2 * H * dv], axis=-1)
    log_a = jax.nn.log_sigmoid((gl @ w_g2 + b_g2).astype(jnp.float32)) / GLA_GATE_TAU
    shp = (bsz, n_c, C, H, dk)
    q = q.reshape(shp).astype(jnp.float32) * (dk ** -0.5)
    k = k.reshape(shp).astype(jnp.float32)
    v = v.reshape(bsz, n_c, C, H, dv).astype(jnp.float32)
    b = jnp.cumsum(log_a.reshape(shp), axis=2)
    b_last = b[:, :, -1:]
    q_t = q * jnp.exp(b)
    k_t = k * jnp.exp(-b)
    k_dec = k * jnp.exp(b_last - b)
    scores = jnp.einsum('bnchd,bnmhd->bnhcm', q_t, k_t)
    tri = jnp.tril(jnp.ones((C, C), dtype=bool))
    scores = jnp.where(tri, scores, 0.0)
    o_intra = jnp.einsum('bnhcm,bnmhe->bnche', scores, v)
    dec = jnp.exp(b_last[:, :, 0])

    def step(state, inp):
        q_n, k_n, v_n, d_n = inp
        o_n = jnp.einsum('bchd,bhde->bche', q_n, state)
        state = d_n[..., None] * state + jnp.einsum('bchd,bche->bhde', k_n, v_n)
        return state, o_n

    xs = (jnp.moveaxis(q_t, 1, 0), jnp.moveaxis(k_dec, 1, 0), jnp.moveaxis(v, 1, 0), jnp.moveaxis(dec, 1, 0))
    _, o_inter = lax.scan(step, jnp.zeros((bsz, H, dk, dv), jnp.float32), xs)
    o = (o_intra + jnp.moveaxis(o_inter, 0, 1)).reshape(bsz, s_len, H, dv)
    o = rmsnorm(o, norm_g) * jax.nn.silu(og.reshape(bsz, s_len, H, dv).astype(jnp.float32))
    return (o.reshape(bsz, s_len, H * dv) @ w_out).astype(h.dtype)


def shared_kv(h, kv_w, cos, sin):
    bsz, s_len, _ = h.shape
    kv = h @ kv_w
    k, v = jnp.split(kv, 2, axis=-1)
    k = apply_partial_rope(k.reshape(bsz, s_len, SWA_KV_HEADS, SWA_HEAD_DIM), cos, sin)
    v = v.reshape(bsz, s_len, SWA_KV_HEADS, SWA_HEAD_DIM)
    return k, v


def swa_sink_attention(h, w_q, sinks, w_out, k, v, cos, sin):
    bsz, s_len, _ = h.shape
    P = SWA_BLOCK
    n_b = s_len // P
    q = apply_partial_rope((h @ w_q).reshape(bsz, s_len, SWA_HEADS, SWA_HEAD_DIM), cos, sin)
    q = q.reshape(bsz, n_b, P, SWA_KV_HEADS, SWA_GROUP, SWA_HEAD_DIM)

    def band(t):
        prev = jnp.pad(t, ((0, 0), (P, 0), (0, 0), (0, 0)))[:, :s_len]
        shp = (bsz, n_b, P, SWA_KV_HEADS, SWA_HEAD_DIM)
        return jnp.concatenate([prev.reshape(shp), t.reshape(shp)], axis=2)

    k_band, v_band = band(k), band(v)
    s = jnp.einsum('bnqkgd,bnmkd->bnkgqm', q, k_band).astype(jnp.float32) * (SWA_HEAD_DIM ** -0.5)
    qi = jnp.arange(P)[:, None]
    mi = jnp.arange(2 * P)[None, :]
    rel = qi + P - mi
    in_window = (rel >= 0) & (rel < SWA_WINDOW)
    key_pos = jnp.arange(n_b)[:, None, None] * P - P + mi[None]
    mask = in_window[None] & (key_pos >= 0)
    s = jnp.where(mask[None, :, None, None], s, -jnp.inf)
    sink = sinks.astype(jnp.float32).reshape(SWA_KV_HEADS, SWA_GROUP)[None, None, :, :, None, None]
    m = jnp.maximum(jnp.max(s, axis=-1, keepdims=True), sink)
    p = jnp.exp(s - m)
    probs = p / (jnp.sum(p, axis=-1, keepdims=True) + jnp.exp(sink - m))
    o = jnp.einsum('bnkgqm,bnmkd->bnqkgd', probs, v_band.astype(jnp.float32))
    return (o.reshape(bsz, s_len, SWA_HEADS * SWA_HEAD_DIM) @ w_out).astype(h.dtype)


def peer(h, w_q, subkeys, u_tab, v_tab):
    bsz, s_len, d = h.shape
    half = PEER_QDIM // 2
    tokens = h.reshape(-1, PEER_BLOCK, d)

    def block(xb):
        q = (xb @ w_q).reshape(PEER_BLOCK, PEER_HEADS, PEER_QDIM)
        s1 = jnp.einsum('phd,nd->phn', q[..., :half], subkeys[0])
        s2 = jnp.einsum('phd,nd->phn', q[..., half:], subkeys[1])
        v1, i1 = lax.top_k(s1, PEER_TOPK)
        v2, i2 = lax.top_k(s2, PEER_TOPK)
        cand = (v1[..., :, None] + v2[..., None, :]).reshape(PEER_BLOCK, PEER_HEADS, PEER_TOPK * PEER_TOPK)
        cs, ci = lax.top_k(cand, PEER_TOPK)
        e = (jnp.take_along_axis(i1, ci // PEER_TOPK, axis=-1) * PEER_N_KEYS
             + jnp.take_along_axis(i2, ci % PEER_TOPK, axis=-1))
        g = jax.nn.softmax(cs.astype(jnp.float32), axis=-1)
        u = u_tab[e]
        a = jax.nn.gelu(jnp.einsum('pd,phkd->phk', xb, u).astype(jnp.float32), approximate=False)
        return jnp.einsum('phk,phkd->pd', g * a, v_tab[e].astype(jnp.float32)).astype(xb.dtype)

    return lax.map(block, tokens).reshape(bsz, s_len, d)


def setup_inputs(seed: int = 0) -> dict:
    key = jax.random.key(seed)
    ks = jax.random.split(key, 24)
    n_a = DEPTH // 2
    n_b = DEPTH - n_a
    D = D_MODEL
    f32 = jnp.float32

    def nrm(k, shape, scale):
        return jax.random.normal(k, shape, f32) * scale

    positions = (jax.random.randint(ks[2], (BATCH, 1), 0, 4096, dtype=jnp.int32)
                 + jnp.arange(SEQ, dtype=jnp.int32)[None, :])
    return {
        "x": nrm(ks[0], (BATCH, SEQ, D), 1.0),
        "c": nrm(ks[1], (BATCH, D), 1.0),
        "positions": positions,
        "mod_w": nrm(ks[3], (DEPTH, D, 6 * D), 0.5 * D ** -0.5),
        "mod_b": nrm(ks[4], (DEPTH, 6 * D), 0.02),
        "norm_g": 1.0 + nrm(ks[5], (DEPTH, 2, D), 0.02),
        "gla_w_in": nrm(ks[6], (n_a, D, GLA_PROJ), D ** -0.5),
        "gla_w_g2": nrm(ks[7], (n_a, GLA_GATE_RANK, GLA_HEADS * GLA_DK), GLA_GATE_RANK ** -0.5),
        "gla_b_g2": nrm(ks[8], (n_a, GLA_HEADS * GLA_DK), 0.1),
        "gla_norm_g": 1.0 + nrm(ks[9], (n_a, GLA_DV), 0.02),
        "gla_w_out": nrm(ks[10], (n_a, GLA_HEADS * GLA_DV, D), (GLA_HEADS * GLA_DV) ** -0.5),
        "kv_mod_w": nrm(ks[11], (D, 2 * D), 0.5 * D ** -0.5),
        "kv_mod_b": nrm(ks[12], (2 * D,), 0.02),
        "kv_norm_g": 1.0 + nrm(ks[13], (D,), 0.02),
        "kv_w": nrm(ks[14], (D, 2 * SWA_KV_HEADS * SWA_HEAD_DIM), D ** -0.5),
        "swa_w_q": nrm(ks[15], (n_b, D, SWA_HEADS * SWA_HEAD_DIM), D ** -0.5),
        "swa_sinks": nrm(ks[16], (n_b, SWA_HEADS), 0.5),
        "swa_w_out": nrm(ks[17], (n_b, SWA_HEADS * SWA_HEAD_DIM, D), (SWA_HEADS * SWA_HEAD_DIM) ** -0.5),
        "peer_w_q": nrm(ks[18], (DEPTH, D, PEER_HEADS * PEER_QDIM), D ** -0.5),
        "peer_subkeys": nrm(ks[19], (DEPTH, 2, PEER_N_KEYS, PEER_QDIM // 2), (PEER_QDIM // 2) ** -0.5),
        "peer_u": nrm(ks[20], (DEPTH, PEER_EXPERTS, D), D ** -0.5),
        "peer_v": nrm(ks[21], (DEPTH, PEER_EXPERTS, D), PEER_HEADS ** -0.5),
        "final_norm_g": 1.0 + nrm(ks[22], (D,), 0.02),
    }


def reference(x, c, positions, mod_w, mod_b, norm_g, gla_w_in, gla_w_g2, gla_b_g2, gla_norm_g,
              gla_w_out, kv_mod_w, kv_mod_b, kv_norm_g, kv_w, swa_w_q, swa_sinks, swa_w_out,
              peer_w_q, peer_subkeys, peer_u, peer_v, final_norm_g):
    n_a = DEPTH // 2
    cos, sin = rope_tables(positions)
    c_act = jax.nn.silu(c)
    k_sh, v_sh = None, None
    for l in range(DEPTH):
        mod = c_act @ mod_w[l] + mod_b[l]
        sh1, sc1, g1, sh2, sc2, g2 = jnp.split(mod, 6, axis=-1)
        h = modulate(x, norm_g[l, 0], sh1, sc1)
        if l < n_a:
            y = gla_mixer(h, gla_w_in[l], gla_w_g2[l], gla_b_g2[l], gla_norm_g[l], gla_w_out[l])
        else:
            j = l - n_a
            y = swa_sink_attention(h, swa_w_q[j], swa_sinks[j], swa_w_out[j], k_sh, v_sh, cos, sin)
        x = x + (g1[:, None, :] * y).astype(x.dtype)
        h = modulate(x, norm_g[l, 1], sh2, sc2)
        y = peer(h, peer_w_q[l], peer_subkeys[l], peer_u[l], peer_v[l])
        x = x + (g2[:, None, :] * y).astype(x.dtype)
        if l == n_a - 1:
            kv_mod = c_act @ kv_mod_w + kv_mod_b
            kv_shift, kv_scale = jnp.split(kv_mod, 2, axis=-1)
            k_sh, v_sh = shared_kv(modulate(x, kv_norm_g, kv_shift, kv_scale), kv_w, cos, sin)
    return rmsnorm(x, final_norm_g)
```

```python
import os
from contextlib import ExitStack
import numpy as np
import concourse.bass as bass
import concourse.mybir as mybir
from concourse.bass_utils import run_bass_kernel_spmd

F32 = mybir.dt.float32
BF16 = mybir.dt.bfloat16
I32 = mybir.dt.int32
ACT = mybir.ActivationFunctionType
ALU = mybir.AluOpType
AX = mybir.AxisListType

D = 1024
KC = 8
SEQ = 8192
NEXP = 16384
EPS = 1e-6
NEG = -1e30
SWA_CUT = int(os.environ.get('SWA_CUT', '99'))
MODEV = os.environ.get('MODEV', 'dve')
GLA_CUT = int(os.environ.get('GLA_CUT', '99'))
PI = float(np.pi)


class Prog:
    ENG = ("pe", "dve", "act", "pool", "sp")

    def __init__(self, nc, es):
        self.nc = nc
        self.es = es
        self.ops = {e: [] for e in self.ENG}
        self.sem = {e: es.enter_context(nc.semaphore("sem_" + e)) for e in self.ENG}
        self.cnt = {e: 0 for e in self.ENG}
        self.dsem = {}
        self.dcnt = {}
        self.lastw = {}
        self.readers = {}
        self.waited = {e: {} for e in self.ENG}
        self.nops = 0

    def _deps(self, eng, r, w):
        deps = {}

        def add(tok):
            if tok is None:
                return
            s, v = tok
            if eng == "pe" and s.name == self.sem["pe"].name:
                return
            if deps.get(s.name, (None, 0))[1] < v:
                deps[s.name] = (s, v)

        for b in r:
            add(self.lastw.get(b))
        for b in w:
            add(self.lastw.get(b))
            for tok in self.readers.get(b, {}).values():
                add(tok)
        out = []
        for key, (s, v) in deps.items():
            if self.waited[eng].get(key, 0) < v:
                self.waited[eng][key] = v
                out.append((s, v))
        return out

    def _commit(self, tok, r, w):
        for b in w:
            self.lastw[b] = tok
            self.readers[b] = {}
        for b in r:
            self.readers.setdefault(b, {})[tok[0].name] = tok

    def op(self, eng, meth, r, w, *a, **k):
        w = list(w) + [b for b in r if len(b) == 2 and b[0] == "p" and b[1].isdigit()]
        waits = self._deps(eng, r, w)
        self.cnt[eng] += 1
        tok = (self.sem[eng], self.cnt[eng])
        self.ops[eng].append((waits, meth, a, k, self.sem[eng], 1))
        self._commit(tok, r, w)
        self.nops += 1

    def dma(self, key, r, w, pairs, eng="sp"):
        if key not in self.dsem:
            self.dsem[key] = self.es.enter_context(self.nc.semaphore("dsem_" + key))
            self.dcnt[key] = 0
        waits = self._deps(eng, r, w)
        for (o, i) in pairs:
            self.dcnt[key] += 16
            self.ops[eng].append((waits, "dma_start", (), dict(out=o, in_=i), self.dsem[key], 16))
            waits = []
            self.nops += 1
        tok = (self.dsem[key], self.dcnt[key])
        self._commit(tok, r, w)

    def barrier(self):
        allw = [(self.sem[e], self.cnt[e]) for e in self.ENG if self.cnt[e] > 0]
        allw += [(s, self.dcnt[k]) for k, s in self.dsem.items()]
        for e in self.ENG:
            ws = []
            for (s, v) in allw:
                if s.name == self.sem[e].name:
                    continue
                if self.waited[e].get(s.name, 0) < v:
                    self.waited[e][s.name] = v
                    ws.append((s, v))
            if ws:
                self.ops[e].append((ws, None, (), {}, None, 0))
        self.lastw = {}
        self.readers = {}

    def emit(self):
        nc = self.nc
        with nc.Block() as block:
            def run(engname):
                def body(engine):
                    for waits, meth, a, k, sem, inc in self.ops[engname]:
                        for (s, v) in waits:
                            engine.wait_ge(s, v)
                        if meth is None:
                            continue
                        getattr(engine, meth)(*a, **k).then_inc(sem, inc)
                return body
            block.tensor(run("pe"))
            block.vector(run("dve"))
            block.scalar(run("act"))
            block.gpsimd(run("pool"))
            block.sync(run("sp"))


def build(NT, dbg=None):
    nc = bass.Bass("TRN2", target_bir_lowering=False)
    S = NT * 128

    def din(name, shape, dt=F32):
        return nc.dram_tensor(name, list(shape), dt, kind="ExternalInput").ap()

    x_d = din("x", [S, D])
    y_d = nc.dram_tensor("y", [S, D], F32, kind="ExternalOutput").ap()
    ccol_d = din("ccol", [128, 8])
    pos_d = din("pos", [128, NT], I32)
    invf_d = din("invf", [128, 8])
    modw_d = [din("modw0", [D, 6 * D]), din("modw1", [D, 6 * D])]
    modbcol_d = din("modbcol", [128, 2, 48])
    gaterow_d = din("gaterow", [128, 4, D])
    kvmodw_d = din("kvmodw", [D, 2 * D])
    kvmodbcol_d = din("kvmodbcol", [128, 16])
    ngcol_d = din("ngcol", [128, 5, 8])
    gfrow_d = din("gfrow", [128, D])
    w_in_d = din("w_in", [D, 3088])
    wg2_d = din("wg2", [16, 512])
    bg2row_d = din("bg2row", [128, 512])
    gnrow_d = din("gnrow", [128, 256])
    gwo_d = din("gwo", [D, D])
    kvw_d = din("kvw", [D, 512])
    swq_d = din("swq", [D, D])
    sinkrow_d = din("sinkrow", [128, 16])
    swo_d = din("swo", [D, D])
    pwq_d = [din("pwq0", [D, 2048]), din("pwq1", [D, 2048])]
    skT_d = din("skT", [128, 2, 2, 128])
    ut_d = [din("ut0", [D, NEXP]), din("ut1", [D, NEXP])]
    v_d = [din("v0", [NEXP, D]), din("v1", [NEXP, D])]
    ident_d = din("ident", [128, 128])
    tri_d = din("tri", [128, 128])
    blk_d = din("blk", [128, 128])
    csel_d = din("csel", [128, 2])
    maskT_d = din("maskT", [128, 128])
    swam_d = din("swam", [128, 2, 256])
    utb_d = [nc.dram_tensor("utb%d" % l, [D, NEXP], BF16, kind="Internal").ap() for l in range(2)]
    vb_d = [nc.dram_tensor("vb%d" % l, [NEXP, D], BF16, kind="Internal").ap() for l in range(2)]
    dbg_d = None
    if dbg is not None:
        dbg_d = nc.dram_tensor("dbg", [128, 8192], F32, kind="ExternalOutput").ap()

    es = ExitStack()
    with es:
        P = Prog(nc, es)

        def sb(name, shape, dt=F32):
            return es.enter_context(nc.sbuf_tensor("sb_" + name, list(shape), dt))

        def psum(name):
            return es.enter_context(nc.psum_tensor(name, [128, 512], F32))

        ident = sb("ident", [128, 128])
        identb = sb("identb", [128, 128], BF16)
        tri = sb("tri", [128, 128])
        blk = sb("blk", [128, 128])
        csel = sb("csel", [128, 2])
        maskT = sb("maskT", [128, 128])
        swam = sb("swam", [128, 2, 256])
        gates = sb("gates", [128, 4, D])
        gfrow = sb("gfrow", [128, D])
        gnrow = sb("gnrow", [128, 256])
        bg2row = sb("bg2row", [128, 512])
        sinkrow = sb("sinkrow", [128, 16])
        wg2 = sb("wg2", [16, 512])
        skT = sb("skT", [128, 2, 2, 128])
        modc = sb("modc", [128, 10, 8])
        ngcol = sb("ngcol", [128, 5, 8])
        modbcol = sb("modbcol", [128, 2, 48])
        kvmodbcol = sb("kvmodbcol", [128, 16])
        ccol = sb("ccol", [128, 8])
        cact = sb("cact", [128, 8])
        cbc = sb("cbc", [128, 8, 128])
        cosT = sb("cosT", [128, NT, 8])
        sinT = sb("sinT", [128, NT, 8])
        posi = sb("posi", [128, NT], I32)
        invf = sb("invf", [128, 8])
        xt = [sb("xt0", [128, D]), sb("xt1", [128, D])]
        xn = sb("xn", [128, D])
        hT = sb("hT", [128, KC, 128])
        hTb = sb("hTb", [128, KC, 128], BF16)
        junk = sb("junk", [128, D], BF16)
        ss = sb("ss", [128, 8])
        sm = sb("sm", [128, 1024])
        wbuf = [sb("wbuf0", [128, KC, 512]), sb("wbuf1", [128, KC, 512])]
        NSLOT = 12
        slot = [sb("slot%d" % i, [128, 2048]) for i in range(NSLOT)]
        Sst = [sb("Sa", [128, 4, 256]), sb("Sb", [128, 4, 256])]
        kTd = [sb("kTd0", [128, 4, 128]), sb("kTd1", [128, 4, 128])]
        vbd = [sb("vbd0", [128, 256]), sb("vbd1", [128, 256])]
        pb = [psum("p%d" % i) for i in range(8)]

        def SV(i, lo, hi):
            return slot[i][:, lo:hi]

        consts = [(ident, ident_d), (tri, tri_d), (blk, blk_d), (csel, csel_d), (maskT, maskT_d), (swam, swam_d),
                  (gates, gaterow_d), (gfrow, gfrow_d), (gnrow, gnrow_d), (bg2row, bg2row_d), (sinkrow, sinkrow_d),
                  (wg2, wg2_d), (skT, skT_d), (ngcol, ngcol_d), (modbcol, modbcol_d), (kvmodbcol, kvmodbcol_d),
                  (ccol, ccol_d), (posi, pos_d), (invf, invf_d)]
        P.dma("const", [], ["const"], [(t[:], d) for (t, d) in consts])
        P.op("dve", "memset", [], ["Sa"], Sst[0][:], 0.0)
        P.op("dve", "memset", [], ["k1"], kTd[1][:], 0.0)
        P.op("dve", "memset", [], ["v1"], vbd[1][:], 0.0)
        P.op("dve", "tensor_copy", ["const"], ["identb"], out=identb[:], in_=ident[:])
        P.op("act", "activation", ["const"], ["cact"], out=cact[:], in_=ccol[:], func=ACT.Silu)
        P.op("dve", "tensor_copy", ["cact"], ["cbc"], out=cbc[:], in_=cact[:].unsqueeze(2).to_broadcast([128, 8, 128]))
        posf = sm[:, 0:NT]
        ang = slot[0][:, 0:NT * 8].rearrange("p (a b) -> p a b", b=8)
        ang2 = slot[0][:, 1024:1024 + NT * 8].rearrange("p (a b) -> p a b", b=8)
        kf = slot[1][:, 0:NT * 8].rearrange("p (a b) -> p a b", b=8)
        ki = slot[2][:, 0:NT * 8].bitcast(I32).rearrange("p (a b) -> p a b", b=8)
        P.op("dve", "tensor_copy", ["const"], ["posf"], out=posf, in_=posi[:])
        P.op("dve", "tensor_tensor", ["posf", "const"], ["ang"], out=ang, in0=posf.unsqueeze(2).to_broadcast([128, NT, 8]),
             in1=invf[:].unsqueeze(1).to_broadcast([128, NT, 8]), op=ALU.mult)
        P.op("dve", "tensor_scalar_add", ["ang"], ["ang2"], out=ang2, in0=ang, scalar1=PI / 2)
        for (src, nm, dst) in ((ang, "ang", sinT), (ang2, "ang2", cosT)):
            P.op("dve", "tensor_scalar", [nm], ["ki"], out=ki, in0=src, scalar1=float(1.0 / (2 * PI)), scalar2=None, op0=ALU.mult)
            P.op("dve", "tensor_copy", ["ki"], ["kf"], out=kf, in_=ki)
            P.op("dve", "scalar_tensor_tensor", ["kf", nm], [nm], out=src, in0=kf, scalar=float(-2 * PI), in1=src, op0=ALU.mult, op1=ALU.add)
            P.op("dve", "tensor_single_scalar", [nm], ["kf"], out=kf, in_=src, scalar=PI, op=ALU.is_gt)
            P.op("dve", "scalar_tensor_tensor", ["kf", nm], [nm], out=src, in0=kf, scalar=float(-2 * PI), in1=src, op0=ALU.mult, op1=ALU.add)
            P.op("dve", "tensor_single_scalar", [nm], ["kf"], out=kf, in_=src, scalar=-PI, op=ALU.is_lt)
            P.op("dve", "scalar_tensor_tensor", ["kf", nm], [nm], out=src, in0=kf, scalar=float(2 * PI), in1=src, op0=ALU.mult, op1=ALU.add)
            P.op("act", "activation", [nm], [nm + "_out"], out=dst[:], in_=src, func=ACT.Sin)

        wsel = [0]

        def wload(dram_w, c0, ncols):
            i = wsel[0]
            wsel[0] ^= 1
            nm = "wbuf%d" % i
            P.dma(nm, [], [nm], [(wbuf[i][:, :, 0:ncols], dram_w[:, c0:c0 + ncols].rearrange("(kc p) n -> p kc n", p=128))])
            return nm, wbuf[i]

        mcol = sm[:, 64:64 + 96].rearrange("p (a b) -> p a b", b=8)
        for l in range(2):
            for bi in range(12):
                nm, wb = wload(modw_d[l], bi * 512, 512)
                kind = bi // 2
                if kind in (2, 5):
                    gi = l * 2 + (0 if kind == 2 else 1)
                    half = bi % 2
                    for kc in range(KC):
                        P.op("pe", "matmul", [nm, "cbc"], ["p0"], pb[0][:, :], lhsT=cbc[:, kc, :], rhs=wb[:, kc, :], start=(kc == 0), stop=(kc == KC - 1))
                    P.op("dve", "tensor_tensor", ["p0", "const"], ["gates"], out=gates[:, gi, half * 512:(half + 1) * 512], in0=pb[0][:, :],
                         in1=gates[:, gi, half * 512:(half + 1) * 512], op=ALU.add)
                else:
                    for j in range(4):
                        for kc in range(KC):
                            P.op("pe", "matmul", [nm, "cact"], ["p1"], pb[1][:, j:j + 1], lhsT=wb[:, kc, j * 128:(j + 1) * 128], rhs=cact[:, kc:kc + 1],
                                 start=(kc == 0), stop=(kc == KC - 1))
                    vi = {0: 0, 1: 1, 3: 2, 4: 3}[kind]
                    P.op("dve", "tensor_tensor", ["p1", "const"], ["mcol"], out=mcol[:, l * 4 + vi, (bi % 2) * 4:(bi % 2) * 4 + 4], in0=pb[1][:, 0:4],
                         in1=modbcol[:, l, bi * 4:bi * 4 + 4], op=ALU.add)
        for bi in range(4):
            nm, wb = wload(kvmodw_d, bi * 512, 512)
            for j in range(4):
                for kc in range(KC):
                    P.op("pe", "matmul", [nm, "cact"], ["p1"], pb[1][:, j:j + 1], lhsT=wb[:, kc, j * 128:(j + 1) * 128], rhs=cact[:, kc:kc + 1],
                         start=(kc == 0), stop=(kc == KC - 1))
            P.op("dve", "tensor_tensor", ["p1", "const"], ["mcol"], out=mcol[:, 8 + bi // 2, (bi % 2) * 4:(bi % 2) * 4 + 4], in0=pb[1][:, 0:4],
                 in1=kvmodbcol[:, bi * 4:bi * 4 + 4], op=ALU.add)
        for (mi, gi, shi, sci) in ((0, 0, 0, 1), (2, 1, 2, 3), (4, 4, 8, 9), (6, 2, 4, 5), (8, 3, 6, 7)):
            P.op("dve", "scalar_tensor_tensor", ["mcol", "const"], ["modc"], out=modc[:, mi, :], in0=mcol[:, sci, :], scalar=1.0, in1=ngcol[:, gi, :],
                 op0=ALU.add, op1=ALU.mult)
            P.op("dve", "tensor_copy", ["mcol"], ["modc"], out=modc[:, mi + 1, :], in_=mcol[:, shi, :])
        P.barrier()

        cv = [0]

        def convert(src_ap, dst_ap):
            i = cv[0] % 3
            cv[0] += 1
            P.dma("cin%d" % i, [], ["cin%d" % i], [(slot[2 * i][:, :], src_ap[:, 0:2048]), (slot[2 * i + 1][:, :], src_ap[:, 2048:4096])])
            ob = slot[6 + i][:, :].bitcast(BF16)
            eng = ("dve", "act", "pool")[i]
            if eng == "act":
                P.op("act", "activation", ["cin%d" % i], ["cob%d" % i], out=ob[:, 0:2048], in_=slot[2 * i][:, :], func=ACT.Copy)
                P.op("act", "activation", ["cin%d" % i], ["cob%d" % i], out=ob[:, 2048:4096], in_=slot[2 * i + 1][:, :], func=ACT.Copy)
            else:
                P.op(eng, "tensor_copy", ["cin%d" % i], ["cob%d" % i], out=ob[:, 0:2048], in_=slot[2 * i][:, :])
                P.op(eng, "tensor_copy", ["cin%d" % i], ["cob%d" % i], out=ob[:, 2048:4096], in_=slot[2 * i + 1][:, :])
            P.dma("cout%d" % i, ["cob%d" % i], [], [(dst_ap, ob)])

        for l in range(2):
            for kc in range(KC):
                for e4 in range(4):
                    convert(ut_d[l][kc * 128:(kc + 1) * 128, e4 * 4096:(e4 + 1) * 4096],
                            utb_d[l][kc * 128:(kc + 1) * 128, e4 * 4096:(e4 + 1) * 4096])
            for r in range(32):
                convert(v_d[l][r * 512:(r + 1) * 512, :].rearrange("(p j) d -> p (j d)", j=4),
                        vb_d[l][r * 512:(r + 1) * 512, :].rearrange("(p j) d -> p (j d)", j=4))
        P.barrier()

        def modulate(xa, xname, mi, bf16=False):
            P.op("act", "activation", [xname], ["junk", "ss0"], out=junk[:], in_=xa, func=ACT.Square, accum_out=ss[:, 0:1])
            P.op("dve", "tensor_scalar", ["ss0"], ["ss1"], out=ss[:, 1:2], in0=ss[:, 0:1], scalar1=1.0 / D, scalar2=EPS, op0=ALU.mult, op1=ALU.add)
            P.op("act", "activation", ["ss1"], ["ss2"], out=ss[:, 2:3], in_=ss[:, 1:2], func=ACT.Sqrt)
            P.op("dve", "reciprocal", ["ss2"], ["ss3"], out=ss[:, 3:4], in_=ss[:, 2:3])
            P.op("dve", "tensor_scalar", [xname, "ss3"], ["xn"], out=xn[:], in0=xa, scalar1=ss[:, 3:4], scalar2=None, op0=ALU.mult)
            for kc in range(KC):
                bnk = kc // 4
                P.op("pe", "transpose", ["xn", "const"], ["p%d" % bnk], out=pb[bnk][:, (kc % 4) * 128:(kc % 4 + 1) * 128],
                     in_=xn[:, kc * 128:(kc + 1) * 128], identity=ident[:])
            for kc in range(KC):
                bnk = kc // 4
                src = pb[bnk][:, (kc % 4) * 128:(kc % 4 + 1) * 128]
                if MODEV == "none":
                    continue
                if (kc % 2 == 0 and MODEV == "mix") or MODEV == "dve":
                    P.op("dve", "tensor_scalar", ["p%d" % bnk, "modc"], ["hT%d" % kc], out=hT[:, kc, :], in0=src,
                         scalar1=modc[:, mi, kc:kc + 1], scalar2=modc[:, mi + 1, kc:kc + 1], op0=ALU.mult, op1=ALU.add)
                else:
                    P.op("act", "activation", ["p%d" % bnk, "modc"], ["hT%d" % kc], out=hT[:, kc, :], in_=src, func=ACT.Identity,
                         scale=modc[:, mi, kc:kc + 1], bias=modc[:, mi + 1, kc:kc + 1])
            if bf16:
                P.op("pool", "tensor_copy", ["hT%d" % kc for kc in range(KC)], ["hTb"], out=hTb[:], in_=hT[:])
            return ["hT%d" % kc for kc in range(KC)]

        def dense_block(hTn, dram_w, c0, ncols, bank):
            nm, wb = wload(dram_w, c0, ncols)
            for kc in range(KC):
                P.op("pe", "matmul", [nm, "hT%d" % kc], ["p%d" % bank], pb[bank][:, 0:ncols], lhsT=hT[:, kc, :], rhs=wb[:, kc, 0:ncols],
                     start=(kc == 0), stop=(kc == KC - 1))

        def out_proj(onm, oap, dram_w, gi, xa, xname):
            oT = SV(5, 1024, 2048).rearrange("p (a b) -> p a b", a=8)
            for kc in range(KC):
                bnk = kc // 4
                P.op("pe", "transpose", [onm, "const"], ["p%d" % bnk], out=pb[bnk][:, (kc % 4) * 128:(kc % 4 + 1) * 128],
                     in_=oap[:, kc * 128:(kc + 1) * 128], identity=ident[:])
            P.op("act", "activation", ["p0"], ["oT0"], out=oT[:, 0:4, :], in_=pb[0][:, :].rearrange("p (a b) -> p a b", a=4), func=ACT.Copy)
            P.op("dve", "tensor_copy", ["p1"], ["oT1"], out=oT[:, 4:8, :], in_=pb[1][:, :].rearrange("p (a b) -> p a b", a=4))
            for half in range(2):
                nm, wb = wload(dram_w, half * 512, 512)
                bank = 2 + half
                for kc in range(KC):
                    P.op("pe", "matmul", [nm, "oT%d" % (kc // 4)], ["p%d" % bank], pb[bank][:, :], lhsT=oT[:, kc, :], rhs=wb[:, kc, :],
                         start=(kc == 0), stop=(kc == KC - 1))
                P.op("dve", "tensor_tensor", ["p%d" % bank, "gates"], ["ytmp%d" % half], out=sm[:, half * 512:(half + 1) * 512], in0=pb[bank][:, :],
                     in1=gates[:, gi, half * 512:(half + 1) * 512], op=ALU.mult)
                P.op("pool", "tensor_tensor", ["ytmp%d" % half, xname], [xname], out=xa[:, half * 512:(half + 1) * 512],
                     in0=xa[:, half * 512:(half + 1) * 512], in1=sm[:, half * 512:(half + 1) * 512], op=ALU.add)

        def gla(xa, xname, it):
            hTn = modulate(xa, xname, 0)
            if GLA_CUT <= 0:
                return
            qk = SV(0, 0, 1024)
            la = SV(0, 1024, 1536)
            zb = SV(0, 1536, 2048)
            vv = SV(1, 0, 1024)
            og = SV(1, 1024, 2048)
            eb = SV(2, 0, 512)
            enb = SV(2, 512, 1024)
            ebl = SV(2, 1024, 1536)
            scm = SV(2, 1536, 2048).rearrange("p (a b) -> p a b", a=4)
            qt = SV(3, 0, 512)
            kt = SV(3, 512, 1024)
            kdec = SV(3, 1024, 1536)
            glT = slot[3][0:16, 1536:1664]
            dec = SV(3, 1664, 1672)
            qtT0 = SV(4, 0, 512).rearrange("p (a b) -> p a b", a=4)
            qtT1 = SV(4, 512, 1024).rearrange("p (a b) -> p a b", a=4)
            ktT = SV(4, 1024, 1536).rearrange("p (a b) -> p a b", a=4)
            qtT = SV(4, 1536, 2048).rearrange("p (a b) -> p a b", a=4)
            osb = SV(5, 0, 1024)
            dsts = [(qk[:, 0:512], "q"), (qk[:, 512:1024], "k"), (vv[:, 0:512], "v0"), (vv[:, 512:1024], "v1"),
                    (og[:, 0:512], "og0"), (og[:, 512:1024], "og1")]
            for bi, (dst, dn) in enumerate(dsts):
                bank = 2 + (bi % 2)
                dense_block(hTn, w_in_d, bi * 512, 512, bank)
                if bi % 2 == 0:
                    P.op("act", "activation", ["p%d" % bank], [dn], out=dst, in_=pb[bank][:, :], func=ACT.Copy)
                else:
                    P.op("dve", "tensor_copy", ["p%d" % bank], [dn], out=dst, in_=pb[bank][:, :])
            if GLA_CUT <= 1:
                return
            nm, wb = wload(w_in_d, 3072, 16)
            for kc in range(KC):
                P.op("pe", "matmul", [nm, "hT%d" % kc], ["p4"], pb[4][0:16, 0:128], lhsT=wb[:, kc, 0:16], rhs=hT[:, kc, :], start=(kc == 0), stop=(kc == KC - 1))
            P.op("dve", "tensor_copy", ["p4"], ["glT"], out=glT, in_=pb[4][0:16, 0:128])
            P.op("pe", "matmul", ["glT", "const"], ["p5"], pb[5][:, :], lhsT=glT, rhs=wg2[:, :], start=True, stop=True)
            P.op("dve", "tensor_tensor", ["p5", "const"], ["zb"], out=zb, in0=pb[5][:, :], in1=bg2row[:], op=ALU.add)
            P.op("act", "activation", ["zb"], ["zb"], out=zb, in_=zb, func=ACT.Exp, scale=-1.0)
            P.op("act", "activation", ["zb"], ["la"], out=la, in_=zb, func=ACT.Ln, bias=1.0)
            if GLA_CUT <= 2:
                return
            P.op("pe", "matmul", ["la", "const"], ["p4"], pb[4][:, :], lhsT=tri[:], rhs=la, start=True, stop=True)
            P.op("pe", "matmul", ["la", "const"], ["p5"], pb[5][:, :], lhsT=blk[:], rhs=la, start=True, stop=True)
            for h in range(4):
                P.op("pe", "matmul", ["la", "const"], ["p6"], pb[6][:, 2 * h:2 * h + 2], lhsT=la[:, h * 128:(h + 1) * 128], rhs=csel[:], start=True, stop=True)
            P.op("act", "activation", ["p4"], ["eb"], out=eb, in_=pb[4][:, :], func=ACT.Exp)
            P.op("act", "activation", ["p4"], ["enb"], out=enb, in_=pb[4][:, :], func=ACT.Exp, scale=-1.0)
            P.op("act", "activation", ["p5"], ["ebl"], out=ebl, in_=pb[5][:, :], func=ACT.Exp)
            P.op("act", "activation", ["p6"], ["dec"], out=dec, in_=pb[6][:, 0:8], func=ACT.Exp)
            P.op("dve", "scalar_tensor_tensor", ["q", "eb"], ["qt"], out=qt, in0=qk[:, 0:512], scalar=float(128 ** -0.5), in1=eb, op0=ALU.mult, op1=ALU.mult)
            P.op("dve", "tensor_tensor", ["k", "enb"], ["kt"], out=kt, in0=qk[:, 512:1024], in1=enb, op=ALU.mult)
            P.op("pool", "tensor_tensor", ["kt", "ebl"], ["kdec"], out=kdec, in0=kt, in1=ebl, op=ALU.mult)
            if GLA_CUT <= 3:
                return
            for h in range(4):
                P.op("pe", "transpose", ["qt", "const"], ["p0"], out=pb[0][:, h * 128:(h + 1) * 128], in_=qt[:, h * 128:(h + 1) * 128], identity=ident[:])
            for h in range(4):
                P.op("pe", "transpose", ["kt", "const"], ["p1"], out=pb[1][:, h * 128:(h + 1) * 128], in_=kt[:, h * 128:(h + 1) * 128], identity=ident[:])
            p0v = pb[0][:, :].rearrange("p (a b) -> p a b", a=4)
            P.op("act", "activation", ["p0"], ["qtT"], out=qtT, in_=p0v, func=ACT.Copy)
            P.op("pool", "memset", [], ["qtT0", "qtT1"], slot[4][:, 0:1024], 0.0)
            P.op("dve", "tensor_copy", ["p0"], ["qtT0"], out=qtT0[:, :, 0:64], in_=p0v[:, :, 0:64])
            P.op("dve", "tensor_copy", ["p0"], ["qtT1"], out=qtT1[:, :, 64:128], in_=p0v[:, :, 64:128])
            P.op("act", "activation", ["p1"], ["ktT"], out=ktT, in_=pb[1][:, :].rearrange("p (a b) -> p a b", a=4), func=ACT.Copy)
            if GLA_CUT <= 4:
                return
            for h in range(4):
                P.op("pe", "matmul", ["ktT", "qtT"], ["p4"], pb[4][:, h * 128:(h + 1) * 128], lhsT=ktT[:, h, :], rhs=qtT[:, h, :], start=True, stop=True)
            P.op("dve", "tensor_tensor", ["p4", "const"], ["scm"], out=scm, in0=pb[4][:, :].rearrange("p (a b) -> p a b", a=4),
                 in1=maskT[:].unsqueeze(1).to_broadcast([128, 4, 128]), op=ALU.mult)
            if GLA_CUT <= 5:
                return
            Sa, Sb = Sst[0], Sst[1]
            for h in range(4):
                vh = vv[:, h * 256:(h + 1) * 256]
                vn = "v%d" % (h // 2)
                kvb = 5 + (h % 2)
                ob = 2 + (h // 2)
                oreg = pb[ob][:, (h % 2) * 256:(h % 2 + 1) * 256]
                P.op("pe", "matmul", ["kdec", vn], ["p%d" % kvb], pb[kvb][:, 0:256], lhsT=kdec[0:64, h * 128:(h + 1) * 128], rhs=vv[0:64, h * 256:(h + 1) * 256],
                     start=True, stop=True)
                P.op("dve", "scalar_tensor_tensor", ["Sa", "dec", "p%d" % kvb], ["Sb"], out=Sb[:, h, :], in0=Sa[:, h, :], scalar=dec[:, 2 * h:2 * h + 1],
                     in1=pb[kvb][:, 0:256], op0=ALU.mult, op1=ALU.add)
                P.op("pe", "matmul", ["scm", vn], ["p%d" % ob], oreg, lhsT=scm[:, h, :], rhs=vh, start=True, stop=False)
                P.op("pe", "matmul", ["qtT0", "Sa"], ["p%d" % ob], oreg, lhsT=qtT0[:, h, :], rhs=Sa[:, h, :], start=False, stop=False)
                P.op("pe", "matmul", ["qtT1", "Sb"], ["p%d" % ob], oreg, lhsT=qtT1[:, h, :], rhs=Sb[:, h, :], start=False, stop=True)
                P.op("pe", "matmul", ["kdec", vn], ["p%d" % kvb], pb[kvb][:, 256:512], lhsT=kdec[64:128, h * 128:(h + 1) * 128], rhs=vv[64:128, h * 256:(h + 1) * 256],
                     start=True, stop=True)
                P.op("dve", "scalar_tensor_tensor", ["Sb", "dec", "p%d" % kvb], ["Sa"], out=Sa[:, h, :], in0=Sb[:, h, :], scalar=dec[:, 2 * h + 1:2 * h + 2],
                     in1=pb[kvb][:, 256:512], op0=ALU.mult, op1=ALU.add)
            if GLA_CUT <= 6:
                return
            for h in range(4):
                ob = 2 + (h // 2)
                oreg = pb[ob][:, (h % 2) * 256:(h % 2 + 1) * 256]
                P.op("act", "activation", ["p%d" % ob], ["junk", "oss%d" % h], out=junk[:, 0:256], in_=oreg, func=ACT.Square, accum_out=ss[:, 4 + h:5 + h])
            P.op("dve", "tensor_scalar", ["oss%d" % h for h in range(4)], ["orv"], out=sm[:, 0:4], in0=ss[:, 4:8], scalar1=1.0 / 256, scalar2=EPS, op0=ALU.mult, op1=ALU.add)
            P.op("act", "activation", ["orv"], ["ors"], out=sm[:, 4:8], in_=sm[:, 0:4], func=ACT.Sqrt)
            P.op("dve", "reciprocal", ["ors"], ["orr"], out=sm[:, 8:12], in_=sm[:, 4:8])
            for h in range(4):
                ob = 2 + (h // 2)
                oreg = pb[ob][:, (h % 2) * 256:(h % 2 + 1) * 256]
                P.op("dve", "scalar_tensor_tensor", ["p%d" % ob, "orr", "const"], ["osb"], out=osb[:, h * 256:(h + 1) * 256], in0=oreg, scalar=sm[:, 8 + h:9 + h],
                     in1=gnrow[:], op0=ALU.mult, op1=ALU.mult)
            P.op("act", "activation", ["og0", "og1"], ["ogs"], out=og, in_=og, func=ACT.Silu)
            P.op("dve", "tensor_tensor", ["osb", "ogs"], ["osb"], out=osb, in0=osb, in1=og, op=ALU.mult)
            if GLA_CUT <= 7:
                return
            out_proj("osb", osb, gwo_d, 0, xa, xname)

        def kv_phase(xa, xname, it):
            cur = it % 2
            hTn = modulate(xa, xname, 4)
            dense_block(hTn, kvw_d, 0, 512, 2)
            kdup = SV(6, 0, 512).rearrange("p (g c d) -> p g c d", g=4, c=2)
            tmp = SV(6, 512, 768).rearrange("p (a g d) -> p a g d", a=8, g=4)
            kp = pb[2][:, 0:256].rearrange("p (g d) -> p g d", g=4)
            P.op("act", "activation", ["p2"], ["v%d" % cur], out=vbd[cur][:], in_=pb[2][:, 256:512], func=ACT.Copy)
            P.op("dve", "tensor_copy", ["p2"], ["kdup"], out=kdup[:, :, 0, :], in_=kp)
            cb = cosT[:, it, :].unsqueeze(1).to_broadcast([128, 4, 8])
            sbb = sinT[:, it, :].unsqueeze(1).to_broadcast([128, 4, 8])
            x1 = kdup[:, :, 0, 0:8]
            x2 = kdup[:, :, 0, 8:16]
            P.op("dve", "tensor_tensor", ["kdup"], ["t0"], out=tmp[:, 0], in0=x1, in1=cb, op=ALU.mult)
            P.op("dve", "tensor_tensor", ["kdup"], ["t1"], out=tmp[:, 1], in0=x2, in1=sbb, op=ALU.mult)
            P.op("dve", "tensor_tensor", ["kdup"], ["t2"], out=tmp[:, 2], in0=x2, in1=cb, op=ALU.mult)
            P.op("dve", "tensor_tensor", ["kdup"], ["t3"], out=tmp[:, 3], in0=x1, in1=sbb, op=ALU.mult)
            P.op("dve", "tensor_tensor", ["t0", "t1"], ["kdup"], out=x1, in0=tmp[:, 0], in1=tmp[:, 1], op=ALU.subtract)
            P.op("dve", "tensor_tensor", ["t2", "t3"], ["kdup"], out=x2, in0=tmp[:, 2], in1=tmp[:, 3], op=ALU.add)
            P.op("dve", "tensor_copy", ["kdup"], ["kdup"], out=kdup[:, :, 1, :], in_=kdup[:, :, 0, :])
            kflat = SV(6, 0, 512)
            for g in range(4):
                P.op("pe", "transpose", ["kdup", "const"], ["p0"], out=pb[0][:, g * 128:(g + 1) * 128], in_=kflat[:, g * 128:(g + 1) * 128], identity=ident[:])
            P.op("act", "activation", ["p0"], ["k%d" % cur], out=kTd[cur][:], in_=pb[0][:, :].rearrange("p (a b) -> p a b", a=4), func=ACT.Copy)

        def swa(xa, xname, it):
            cur = it % 2
            prv = 1 - cur
            hTn = modulate(xa, xname, 6)
            q = SV(0, 0, 1024)
            q3 = q.rearrange("p (h d) -> p h d", h=16)
            qT = SV(0, 1024, 2048).rearrange("p (a b) -> p a b", a=8)
            sc = [SV(1, 0, 2048).rearrange("p (h m) -> p h m", h=8), SV(2, 0, 2048).rearrange("p (h m) -> p h m", h=8)]
            pT = [SV(3, 0, 2048).rearrange("p (h c m) -> p h c m", h=8, c=2), SV(4, 0, 2048).rearrange("p (h c m) -> p h c m", h=8, c=2)]
            o = SV(5, 0, 1024)
            tmp = SV(6, 1024, 2048).rearrange("p (a h d) -> p a h d", a=4, h=16)
            for half in range(2):
                dense_block(hTn, swq_d, half * 512, 512, 2 + half)
                if half == 0:
                    P.op("act", "activation", ["p2"], ["q"], out=q[:, 0:512], in_=pb[2][:, :], func=ACT.Copy)
                else:
                    P.op("dve", "tensor_copy", ["p3"], ["q"], out=q[:, 512:1024], in_=pb[3][:, :])
            if SWA_CUT <= 1:
                return
            cb = cosT[:, it, :].unsqueeze(1).to_broadcast([128, 16, 8])
            sbb = sinT[:, it, :].unsqueeze(1).to_broadcast([128, 16, 8])
            x1 = q3[:, :, 0:8]
            x2 = q3[:, :, 8:16]
            tv = SV(6, 1024, 1536).rearrange("p (a h d) -> p a h d", a=4, h=16)
            P.op("dve", "tensor_tensor", ["q"], ["t0"], out=tv[:, 0], in0=x1, in1=cb, op=ALU.mult)
            P.op("dve", "tensor_tensor", ["q"], ["t1"], out=tv[:, 1], in0=x2, in1=sbb, op=ALU.mult)
            P.op("dve", "tensor_tensor", ["q"], ["t2"], out=tv[:, 2], in0=x2, in1=cb, op=ALU.mult)
            P.op("dve", "tensor_tensor", ["q"], ["t3"], out=tv[:, 3], in0=x1, in1=sbb, op=ALU.mult)
            P.op("dve", "tensor_tensor", ["t0", "t1"], ["q"], out=x1, in0=tv[:, 0], in1=tv[:, 1], op=ALU.subtract)
            P.op("dve", "tensor_tensor", ["t2", "t3"], ["q"], out=x2, in0=tv[:, 2], in1=tv[:, 3], op=ALU.add)
            if SWA_CUT <= 2:
                return
            for j in range(8):
                bnk = j // 4
                P.op("pe", "transpose", ["q", "const"], ["p%d" % bnk], out=pb[bnk][:, (j % 4) * 128:(j % 4 + 1) * 128], in_=q[:, j * 128:(j + 1) * 128], identity=ident[:])
            P.op("act", "activation", ["p0"], ["qT0"], out=qT[:, 0:4, :], in_=pb[0][:, :].rearrange("p (a b) -> p a b", a=4), func=ACT.Copy)
            P.op("dve", "tensor_copy", ["p1"], ["qT1"], out=qT[:, 4:8, :], in_=pb[1][:, :].rearrange("p (a b) -> p a b", a=4))
            if SWA_CUT <= 3:
                return
            mk = swam[:, 0 if it == 0 else 1, :]
            for grp in range(4):
                bankE = 4 + 2 * (grp % 2)
                bankO = bankE + 1
                for pj in range(2):
                    j = 2 * grp + pj
                    for hh in range(2):
                        h = 2 * j + hh
                        g = h // 4
                        base = 64 * hh
                        bank = bankE if hh == 0 else bankO
                        col = pj * 256
                        P.op("pe", "matmul", ["qT%d" % (j // 4), "k%d" % prv], ["p%d" % bank], pb[bank][:, col:col + 128],
                             lhsT=qT[base:base + 64, j, :], rhs=kTd[prv][base:base + 64, g, :], start=True, stop=True)
                        P.op("pe", "matmul", ["qT%d" % (j // 4), "k%d" % cur], ["p%d" % bank], pb[bank][:, col + 128:col + 256],
                             lhsT=qT[base:base + 64, j, :], rhs=kTd[cur][base:base + 64, g, :], start=True, stop=True)
                for hh in range(2):
                    bank = bankE if hh == 0 else bankO
                    for pj in range(2):
                        j = 2 * grp + pj
                        h = 2 * j + hh
                        P.op("dve", "scalar_tensor_tensor", ["p%d" % bank, "const"], ["sc%d" % j], out=sc[h // 8][:, h % 8, :],
                             in0=pb[bank][:, pj * 256:(pj + 1) * 256], scalar=0.125, in1=mk, op0=ALU.mult, op1=ALU.add)
            if SWA_CUT <= 4:
                return
            rmax = sm[:, 0:16]
            mm = sm[:, 16:32]
            negm = sm[:, 32:48]
            rs = sm[:, 48:64]
            sk = sm[:, 64:80]
            den = sm[:, 80:96]
            rden = sm[:, 96:112]
            for half in range(2):
                P.op("dve", "tensor_reduce", ["sc%d" % j for j in range(half * 4, half * 4 + 4)], ["rmax%d" % half], out=rmax[:, half * 8:(half + 1) * 8],
                     in_=sc[half], axis=AX.X, op=ALU.max)
            P.op("dve", "tensor_tensor", ["rmax0", "rmax1", "const"], ["mm"], out=mm, in0=rmax, in1=sinkrow[:], op=ALU.max)
            P.op("dve", "tensor_scalar", ["mm"], ["negm"], out=negm, in0=mm, scalar1=-1.0, scalar2=None, op0=ALU.mult)
            P.op("dve", "tensor_tensor", ["mm", "const"], ["sk"], out=sk, in0=sinkrow[:], in1=mm, op=ALU.subtract)
            P.op("act", "activation", ["sk"], ["sk"], out=sk, in_=sk, func=ACT.Exp)
            for h in range(16):
                j = h // 2
                sl = sc[h // 8][:, h % 8, :]
                P.op("act", "activation", ["sc%d" % j, "negm"], ["sc%d" % j, "rs%d" % h], out=sl, in_=sl, func=ACT.Exp, bias=negm[:, h:h + 1], scale=1.0,
                     accum_out=rs[:, h:h + 1])
            P.op("dve", "tensor_tensor", ["rs%d" % h for h in range(16)] + ["sk"], ["den"], out=den, in0=rs, in1=sk, op=ALU.add)
            P.op("dve", "reciprocal", ["den"], ["rden"], out=rden, in_=den)
            if SWA_CUT <= 5:
                return
            for j in range(8):
                bank = j % 4
                for hh in range(2):
                    h = 2 * j + hh
                    for part in range(2):
                        P.op("pe", "transpose", ["sc%d" % j, "const"], ["p%d" % bank], out=pb[bank][:, (hh * 2 + part) * 128:(hh * 2 + part + 1) * 128],
                             in_=sc[h // 8][:, h % 8, part * 128:(part + 1) * 128], identity=ident[:])
                dstv = pT[j // 4][:, 2 * (j % 4):2 * (j % 4) + 2, :, :]
                srcv = pb[bank][:, :].rearrange("p (h c m) -> p h c m", h=2, c=2)
                if j % 2 == 0:
                    P.op("act", "activation", ["p%d" % bank], ["pT%d" % j], out=dstv, in_=srcv, func=ACT.Copy)
                else:
                    P.op("dve", "tensor_copy", ["p%d" % bank], ["pT%d" % j], out=dstv, in_=srcv)
            if SWA_CUT <= 6:
                return
            for h in range(16):
                j = h // 2
                g = h // 4
                bank = 4 + h // 8
                oreg = pb[bank][:, (h % 8) * 64:(h % 8 + 1) * 64]
                P.op("pe", "matmul", ["pT%d" % j, "v%d" % prv], ["p%d" % bank], oreg, lhsT=pT[h // 8][:, h % 8, 0, :], rhs=vbd[prv][:, g * 64:(g + 1) * 64],
                     start=True, stop=False)
                P.op("pe", "matmul", ["pT%d" % j, "v%d" % cur], ["p%d" % bank], oreg, lhsT=pT[h // 8][:, h % 8, 1, :], rhs=vbd[cur][:, g * 64:(g + 1) * 64],
                     start=False, stop=True)
            if SWA_CUT <= 7:
                return
            for b2 in range(2):
                P.op("dve", "tensor_tensor", ["p%d" % (4 + b2), "rden"], ["o"], out=o[:, b2 * 512:(b2 + 1) * 512].rearrange("p (h d) -> p h d", h=8),
                     in0=pb[4 + b2][:, :].rearrange("p (h d) -> p h d", h=8), in1=rden[:, b2 * 8:(b2 + 1) * 8].unsqueeze(2).to_broadcast([128, 8, 64]), op=ALU.mult)
            if SWA_CUT <= 8:
                return
            out_proj("o", o, swo_d, 2, xa, xname)

        def peer(xa, xname, l):
            mi = 2 if l == 0 else 8
            gi = 1 if l == 0 else 3
            hTn = modulate(xa, xname, mi, bf16=True)
            qT = SV(0, 0, 2048).rearrange("p (g t) -> p g t", g=16)
            s = SV(1, 0, 2048).rearrange("p (g n) -> p g n", g=16)
            sw = SV(2, 0, 2048).rearrange("p (g n) -> p g n", g=16)
            cand = SV(3, 0, 2048).rearrange("p (h a b) -> p h a b", h=8, a=16)
            cw = SV(4, 0, 2048).rearrange("p (h a b) -> p h a b", h=8, a=16)
            v16 = sm[:, 0:256].rearrange("p (g k) -> p g k", g=16)
            c16 = sm[:, 256:384].rearrange("p (h k) -> p h k", h=8)
            dd = sm[:, 384:512].rearrange("p (h k) -> p h k", h=8)
            Z = sm[:, 512:520]
            lnZ = sm[:, 520:528]
            bia = sm[:, 528:536]
            for bi in range(4):
                nm, wb = wload(pwq_d[l], bi * 512, 512)
                for gl in range(4):
                    g = bi * 4 + gl
                    for kc in range(KC):
                        P.op("pe", "matmul", [nm, "hT%d" % kc], ["p%d" % bi], pb[bi][:, gl * 128:(gl + 1) * 128], lhsT=wb[:, kc, gl * 128:(gl + 1) * 128], rhs=hT[:, kc, :],
                             start=(kc == 0), stop=(kc == KC - 1))
                if bi % 2 == 0:
                    P.op("act", "activation", ["p%d" % bi], ["qT%d" % bi], out=qT[:, bi * 4:bi * 4 + 4, :], in_=pb[bi][:, :].rearrange("p (a b) -> p a b", a=4), func=ACT.Copy)
                else:
                    P.op("dve", "tensor_copy", ["p%d" % bi], ["qT%d" % bi], out=qT[:, bi * 4:bi * 4 + 4, :], in_=pb[bi][:, :].rearrange("p (a b) -> p a b", a=4))
            for g in range(16):
                bank = 4 + g // 4
                P.op("pe", "matmul", ["qT%d" % (g // 4), "const"], ["p%d" % bank], pb[bank][:, (g % 4) * 128:(g % 4 + 1) * 128], lhsT=qT[:, g, :], rhs=skT[:, l, g % 2, :],
                     start=True, stop=True)
            for b4 in range(4):
                bank = 4 + b4
                if b4 % 2 == 0:
                    P.op("act", "activation", ["p%d" % bank], ["s%d" % b4], out=s[:, b4 * 4:b4 * 4 + 4, :], in_=pb[bank][:, :].rearrange("p (a b) -> p a b", a=4), func=ACT.Copy)
                else:
                    P.op("dve", "tensor_copy", ["p%d" % bank], ["s%d" % b4], out=s[:, b4 * 4:b4 * 4 + 4, :], in_=pb[bank][:, :].rearrange("p (a b) -> p a b", a=4))
            snames_all = ["s%d" % b4 for b4 in range(4)]
            for g in range(16):
                P.op("dve", "max", ["s%d" % (g // 4)], ["v16a%d" % g], out=v16[:, g, 0:8], in_=s[:, g, :])
            for g in range(16):
                P.op("dve", "match_replace", ["s%d" % (g // 4), "v16a%d" % g], ["sw%d" % g], out=sw[:, g, :], in_to_replace=v16[:, g, 0:8], in_values=s[:, g, :], imm_value=NEG)
            for g in range(16):
                P.op("dve", "max", ["sw%d" % g], ["v16b%d" % g], out=v16[:, g, 8:16], in_=sw[:, g, :])
            v16n = ["v16a%d" % g for g in range(16)] + ["v16b%d" % g for g in range(16)]
            Ev = sm[:, 536:792].rearrange("p (g k) -> p g k", g=16)
            rZ = sm[:, 520:528]
            E = sw
            swn = ["sw%d" % g for g in range(16)]
            P.op("dve", "tensor_tensor", snames_all + v16n + swn, ["E"], out=E, in0=s, in1=v16[:, :, 0:1].to_broadcast([128, 16, 128]), op=ALU.subtract)
            P.op("act", "activation", ["E"], ["E"], out=E, in_=E, func=ACT.Exp)
            P.op("dve", "tensor_tensor", v16n, ["Ev"], out=Ev, in0=v16, in1=v16[:, :, 0:1].to_broadcast([128, 16, 16]), op=ALU.subtract)
            P.op("act", "activation", ["Ev"], ["Ev"], out=Ev, in_=Ev, func=ACT.Exp)
            E4 = E.rearrange("p (h c) n -> p h c n", c=2)
            Ev4 = Ev.rearrange("p (h c) k -> p h c k", c=2)
            c16n = ["c16a%d" % h for h in range(8)] + ["c16b%d" % h for h in range(8)]
            for rnd in range(2):
                P.op("dve", "tensor_tensor", ["Ev"], ["cand"], out=cand,
                     in0=Ev4[:, :, 0, :].unsqueeze(3).to_broadcast([128, 8, 16, 16]), in1=Ev4[:, :, 1, :].unsqueeze(2).to_broadcast([128, 8, 16, 16]), op=ALU.mult)
                for h in range(8):
                    P.op("dve", "max", ["cand"], ["c16a%d" % h], out=c16[:, h, 0:8], in_=cand[:, h])
                for h in range(8):
                    P.op("dve", "match_replace", ["cand", "c16a%d" % h], ["cw%d" % h], out=cw[:, h], in_to_replace=c16[:, h, 0:8], in_values=cand[:, h], imm_value=-1.0)
                for h in range(8):
                    P.op("dve", "max", ["cw%d" % h], ["c16b%d" % h], out=c16[:, h, 8:16], in_=cw[:, h])
                if rnd == 0:
                    P.op("dve", "tensor_reduce", c16n, ["Z"], out=Z, in_=c16, axis=AX.X, op=ALU.add)
                    P.op("dve", "reciprocal", ["Z"], ["rZ"], out=rZ, in_=Z)
                    P.op("dve", "tensor_tensor", ["E", "rZ"], ["E"], out=E4[:, :, 1, :], in0=E4[:, :, 1, :], in1=rZ.unsqueeze(2).to_broadcast([128, 8, 128]), op=ALU.mult)
                    P.op("dve", "tensor_tensor", ["Ev", "rZ"], ["Ev"], out=Ev4[:, :, 1, :], in0=Ev4[:, :, 1, :], in1=rZ.unsqueeze(2).to_broadcast([128, 8, 16]), op=ALU.mult)
            EEs = [SV(i, 0, 1024).rearrange("p (i j) -> p i j", i=8) for i in (5, 6, 0)]
            gtall = SV(7, 0, 2048).bitcast(BF16)
            Gts = [gtall[:, k * 1024:(k + 1) * 1024] for k in range(4)]
            gk = [0]

            def gbuild_head(n, h):
                k = gk[0] % 3
                k4 = gk[0] % 4
                gk[0] += 1
                EE, Gt = EEs[k], Gts[k4]
                een, gtn = "EE%d" % k, "Gt%d" % k4
                i0 = n * 8
                P.op("dve" if h % 4 == 3 else "pool", "tensor_tensor", ["E"] + c16n, [een], out=EE, in0=E4[:, h, 0, i0:i0 + 8].unsqueeze(2).to_broadcast([128, 8, 128]),
                     in1=E4[:, h, 1, :].unsqueeze(1).to_broadcast([128, 8, 128]), op=ALU.mult)
                P.op("dve", "scalar_tensor_tensor", [een] + c16n, [gtn], out=Gt.rearrange("p (i j) -> p i j", i=8), in0=EE, scalar=c16[:, h, 15:16], in1=EE,
                     op0=ALU.is_ge, op1=ALU.mult)
                pend_acc.append((n, h, gtn, Gt))

            pend_acc = []

            def flush_pe_acc():
                for (n, h, gtn, Gt) in pend_acc:
                    for c in range(2):
                        bank = 2 + 2 * (n % 2) + c
                        P.op("pe", "matmul", [gtn, "identb"], ["p%d" % bank], pb[bank][:, :], lhsT=identb[:], rhs=Gt[:, c * 512:(c + 1) * 512], start=(h == 0), stop=(h == 7))
                del pend_acc[:]

            utv = utb_d[l].rearrange("(kc p) e -> p kc e", p=128)
            chain = [None]
            p1b = pb[1][:, :].bitcast(BF16)

            def tail(prev):
                (ebp, GA, GAT, sn, wnv, vblk, first, last) = prev
                half = p1b[:, (ebp % 2) * 512:(ebp % 2 + 1) * 512]
                for j in range(4):
                    P.op("pe", "transpose", [sn + ".GA", "identb"], ["p1"], out=half[:, j * 128:(j + 1) * 128], in_=GA[:, j * 128:(j + 1) * 128], identity=identb[:])
                P.op("act", "activation", ["p1"], [sn + ".GAT"], out=GAT, in_=half.rearrange("p (a b) -> p a b", a=4), func=ACT.Copy)
                for j in range(4):
                    for db in range(2):
                        P.op("pe", "matmul", [sn + ".GAT", wnv], ["p%d" % (6 + db)], pb[6 + db][:, :], lhsT=GAT[:, j, :], rhs=vblk[:, j, db * 512:(db + 1) * 512],
                             start=(first and j == 0), stop=(last and j == 3))

            for h in range(8):
                gbuild_head(0, h)
                if h % 4 == 3:
                    flush_pe_acc()
            for nb in range(32):
                n = nb // 2
                e0 = nb * 512
                i = wsel[0]
                wsel[0] ^= 1
                wnu = "wbuf%du" % i
                wnv = "wbuf%dv" % i
                wbb = wbuf[i][:].rearrange("p a b -> p (a b)").bitcast(BF16)
                ublk = wbb[:, 0:4096].rearrange("p (kc e) -> p kc e", kc=8)
                vblk = wbb[:, 4096:8192].rearrange("p (j d) -> p j d", j=4)
                extra = ["wbuf%d" % i] if nb < 2 else []
                P.dma(wnu, [], [wnu] + extra, [(ublk, utv[:, :, e0:e0 + 512])])
                P.dma(wnv, [], [wnv] + extra, [(vblk, vb_d[l][e0:e0 + 512, :].rearrange("(j p) d -> p j d", p=128))])
                si = 10 + (nb % 2)
                sn = "slot%d" % si
                Ag = SV(si, 0, 512)
                GA = SV(si, 512, 768).bitcast(BF16)
                GAT = SV(si, 1024, 1280).bitcast(BF16).rearrange("p (a b) -> p a b", a=4)
                for kc in range(KC):
                    P.op("pe", "matmul", [wnu, "hTb"], ["p0"], pb[0][:, :], lhsT=hTb[:, kc, :], rhs=ublk[:, kc, :], start=(kc == 0), stop=(kc == KC - 1))
                flush_pe_acc()
                P.op("act", "activation", ["p0"], [sn + ".Ag"], out=Ag, in_=pb[0][:, :], func=ACT.Gelu)
                gbank = 2 + 2 * (n % 2) + (nb % 2)
                P.op("dve", "tensor_tensor", [sn + ".Ag", "p%d" % gbank], [sn + ".GA"], out=GA, in0=pb[gbank][:, :], in1=Ag, op=ALU.mult)
                if chain[0] is not None:
                    tail(chain[0])
                if n < 15:
                    for h in range(4 * (nb % 2), 4 * (nb % 2) + 4):
                        gbuild_head(n + 1, h)
                chain[0] = (nb, GA, GAT, sn, wnv, vblk, nb == 0, nb == 31)
            tail(chain[0])
            for db in range(2):
                P.op("dve", "tensor_tensor", ["p%d" % (6 + db), "gates"], ["ytmp%d" % db], out=slot[8][:, db * 512:(db + 1) * 512],
                     in0=pb[6 + db][:, :], in1=gates[:, gi, db * 512:(db + 1) * 512], op=ALU.mult)
                P.op("pool", "tensor_tensor", ["ytmp%d" % db, xname], [xname], out=xa[:, db * 512:(db + 1) * 512], in0=xa[:, db * 512:(db + 1) * 512],
                     in1=slot[8][:, db * 512:(db + 1) * 512], op=ALU.add)

        def final_norm(xa, xname, it):
            P.op("act", "activation", [xname], ["junk", "ss0"], out=junk[:], in_=xa, func=ACT.Square, accum_out=ss[:, 0:1])
            P.op("dve", "tensor_scalar", ["ss0"], ["ss1"], out=ss[:, 1:2], in0=ss[:, 0:1], scalar1=1.0 / D, scalar2=EPS, op0=ALU.mult, op1=ALU.add)
            P.op("act", "activation", ["ss1"], ["ss2"], out=ss[:, 2:3], in_=ss[:, 1:2], func=ACT.Sqrt)
            P.op("dve", "reciprocal", ["ss2"], ["ss3"], out=ss[:, 3:4], in_=ss[:, 2:3])
            P.op("dve", "scalar_tensor_tensor", [xname, "ss3", "const"], ["xn"], out=xn[:], in0=xa, scalar=ss[:, 3:4], in1=gfrow[:], op0=ALU.mult, op1=ALU.mult)
            P.dma("yout", ["xn"], [], [(y_d[it * 128:(it + 1) * 128, :], xn[:])])

        stages = os.environ.get("YOCO_STAGES", "gla,peer0,kv,swa,peer1").split(",")
        for it in range(NT):
            xa = xt[it % 2][:]
            xname = "xt%d" % (it % 2)
            P.dma(xname, [], [xname], [(xa, x_d[it * 128:(it + 1) * 128, :])])
            if "gla" in stages:
                gla(xa, xname, it)
                P.barrier()
            if "peer0" in stages:
                peer(xa, xname, 0)
                P.barrier()
            if "kv" in stages:
                kv_phase(xa, xname, it)
                P.barrier()
            if "swa" in stages:
                swa(xa, xname, it)
                P.barrier()
            if "peer1" in stages:
                peer(xa, xname, 1)
                P.barrier()
            final_norm(xa, xname, it)
            P.barrier()
        P.barrier()
        P.emit()
        print("yoco build: ops", P.nops, {e: P.cnt[e] for e in P.ENG}, flush=True)
    return nc


def prep_inputs(inp, NT, nb=8):
    f = np.float32
    S = NT * 128
    t = np.arange(128)
    same = (t[:, None] // 64) == (t[None, :] // 64)
    tri = (same & (t[:, None] <= t[None, :])).astype(f) * f(-1.0 / 16)
    blk = same.astype(f) * f(-1.0 / 16)
    csel = ((t[:, None] // 64) == np.arange(2)[None, :]).astype(f) * f(-1.0 / 16)
    maskT = (same & (t[:, None] <= t[None, :])).astype(f)
    qi = np.arange(128)[:, None]
    mi = np.arange(256)[None, :]
    valid = (mi > qi) & (mi <= qi + 128)
    swam = np.stack([np.where(valid & (mi >= 128), 0.0, NEG), np.where(valid, 0.0, NEG)], axis=1).astype(f)
    invf = (np.float32(500000.0) ** (-(np.arange(0, 16, 2, dtype=np.float32)) / np.float32(16))).astype(f)
    invf = np.ascontiguousarray(np.broadcast_to(invf[None, :], (128, 8)))

    def col(v):
        return np.ascontiguousarray(v.reshape(8, 128).T)

    def row(v):
        return np.ascontiguousarray(np.broadcast_to(v[None, :], (128, v.shape[0])))

    mod_b = inp["mod_b"]
    shared = {
        "invf": invf, "ident": np.eye(128, dtype=f), "tri": tri, "blk": blk, "csel": csel, "maskT": maskT, "swam": swam,
        "modw0": np.ascontiguousarray(inp["mod_w"][0]), "modw1": np.ascontiguousarray(inp["mod_w"][1]),
        "modbcol": np.ascontiguousarray(np.stack([mod_b[l].reshape(48, 128).T for l in range(2)], axis=1)),
        "gaterow": np.ascontiguousarray(np.stack([row(mod_b[0, 2048:3072]), row(mod_b[0, 5120:6144]),
                                                   row(mod_b[1, 2048:3072]), row(mod_b[1, 5120:6144])], axis=1)),
        "kvmodw": np.ascontiguousarray(inp["kv_mod_w"]),
        "kvmodbcol": np.ascontiguousarray(inp["kv_mod_b"].reshape(16, 128).T),
        "ngcol": np.ascontiguousarray(np.stack([col(inp["norm_g"][0, 0]), col(inp["norm_g"][0, 1]), col(inp["norm_g"][1, 0]),
                                                col(inp["norm_g"][1, 1]), col(inp["kv_norm_g"])], axis=1)),
        "gfrow": row(inp["final_norm_g"]),
        "w_in": np.ascontiguousarray(inp["gla_w_in"][0]), "wg2": np.ascontiguousarray(inp["gla_w_g2"][0]),
        "bg2row": row(inp["gla_b_g2"][0]), "gnrow": row(inp["gla_norm_g"][0]), "gwo": np.ascontiguousarray(inp["gla_w_out"][0]),
        "kvw": np.ascontiguousarray(inp["kv_w"]), "swq": np.ascontiguousarray(inp["swa_w_q"][0]),
        "sinkrow": row(inp["swa_sinks"][0]), "swo": np.ascontiguousarray(inp["swa_w_out"][0]),
        "pwq0": np.ascontiguousarray(inp["peer_w_q"][0]), "pwq1": np.ascontiguousarray(inp["peer_w_q"][1]),
        "skT": np.ascontiguousarray(np.transpose(inp["peer_subkeys"], (3, 0, 1, 2))),
        "ut0": np.ascontiguousarray(inp["peer_u"][0].T), "ut1": np.ascontiguousarray(inp["peer_u"][1].T),
        "v0": np.ascontiguousarray(inp["peer_v"][0]), "v1": np.ascontiguousarray(inp["peer_v"][1]),
    }
    maps = []
    for b in range(nb):
        m = dict(shared)
        m["x"] = np.ascontiguousarray(inp["x"][b, :S])
        m["ccol"] = col(inp["c"][b])
        m["pos"] = np.ascontiguousarray(inp["positions"][b, :S].reshape(NT, 128).T.astype(np.int32))
        maps.append(m)
    return maps


_NC_CACHE = {}


def kernel(**inputs):
    inputs = {k: np.asarray(v) for k, v in inputs.items()}
    NT = SEQ // 128
    if NT not in _NC_CACHE:
        _NC_CACHE[NT] = build(NT)
    nc = _NC_CACHE[NT]
    maps = prep_inputs(inputs, NT)
    res = run_bass_kernel_spmd(nc, maps, core_ids=list(range(8)))
    out = np.stack([np.asarray(r["y"]) for r in res.results], axis=0)
    return out.astype(np.float32)
```

```python
import os
from contextlib import ExitStack
import numpy as np
import concourse.bass as bass
import concourse.mybir as mybir
from concourse.bass_utils import run_bass_kernel_spmd

F32 = mybir.dt.float32
BF16 = mybir.dt.bfloat16
I32 = mybir.dt.int32
ACT = mybir.ActivationFunctionType
ALU = mybir.AluOpType
AX = mybir.AxisListType

D = 1024
KC = 8
SEQ = 8192
NEXP = 16384
EPS = 1e-6
NEG = -1e30
ACT_PAR = int(os.environ.get('ACT_PAR', '-1'))
BF16_PROJ = os.environ.get('BF16_PROJ', '1') == '1'
SWA_CUT = int(os.environ.get('SWA_CUT', '99'))
MODEV = os.environ.get('MODEV', 'dve')
GLA_CUT = int(os.environ.get('GLA_CUT', '99'))
PI = float(np.pi)


class Prog:
    ENG = ("pe", "dve", "act", "pool", "sp")

    def __init__(self, nc, es):
        self.nc = nc
        self.es = es
        self.ops = {e: [] for e in self.ENG}
        self.sem = {e: es.enter_context(nc.semaphore("sem_" + e)) for e in self.ENG}
        self.cnt = {e: 0 for e in self.ENG}
        self.dsem = {}
        self.dcnt = {}
        self.lastw = {}
        self.readers = {}
        self.waited = {e: {} for e in self.ENG}
        self.nops = 0

    def _deps(self, eng, r, w):
        deps = {}

        def add(tok):
            if tok is None:
                return
            s, v = tok
            if eng == "pe" and s.name == self.sem["pe"].name:
                return
            if deps.get(s.name, (None, 0))[1] < v:
                deps[s.name] = (s, v)

        for b in r:
            add(self.lastw.get(b))
        for b in w:
            add(self.lastw.get(b))
            for tok in self.readers.get(b, {}).values():
                add(tok)
        out = []
        for key, (s, v) in deps.items():
            if self.waited[eng].get(key, 0) < v:
                self.waited[eng][key] = v
                out.append((s, v))
        return out

    def _commit(self, tok, r, w):
        for b in w:
            self.lastw[b] = tok
            self.readers[b] = {}
        for b in r:
            self.readers.setdefault(b, {})[tok[0].name] = tok

    def op(self, eng, meth, r, w, *a, **k):
        w = list(w) + [b for b in r if len(b) == 2 and b[0] == "p" and b[1].isdigit()]
        waits = self._deps(eng, r, w)
        self.cnt[eng] += 1
        tok = (self.sem[eng], self.cnt[eng])
        self.ops[eng].append((waits, meth, a, k, self.sem[eng], 1))
        self._commit(tok, r, w)
        self.nops += 1

    def dma(self, key, r, w, pairs, eng="sp"):
        if key not in self.dsem:
            self.dsem[key] = self.es.enter_context(self.nc.semaphore("dsem_" + key))
            self.dcnt[key] = 0
        waits = self._deps(eng, r, w)
        for (o, i) in pairs:
            self.dcnt[key] += 16
            self.ops[eng].append((waits, "dma_start", (), dict(out=o, in_=i), self.dsem[key], 16))
            waits = []
            self.nops += 1
        tok = (self.dsem[key], self.dcnt[key])
        self._commit(tok, r, w)

    def barrier(self):
        allw = [(self.sem[e], self.cnt[e]) for e in self.ENG if self.cnt[e] > 0]
        allw += [(s, self.dcnt[k]) for k, s in self.dsem.items()]
        for e in self.ENG:
            ws = []
            for (s, v) in allw:
                if s.name == self.sem[e].name:
                    continue
                if self.waited[e].get(s.name, 0) < v:
                    self.waited[e][s.name] = v
                    ws.append((s, v))
            if ws:
                self.ops[e].append((ws, None, (), {}, None, 0))
        self.lastw = {}
        self.readers = {}

    def emit(self):
        nc = self.nc
        with nc.Block() as block:
            def run(engname):
                def body(engine):
                    for waits, meth, a, k, sem, inc in self.ops[engname]:
                        for (s, v) in waits:
                            engine.wait_ge(s, v)
                        if meth is None:
                            continue
                        getattr(engine, meth)(*a, **k).then_inc(sem, inc)
                return body
            block.tensor(run("pe"))
            block.vector(run("dve"))
            block.scalar(run("act"))
            block.gpsimd(run("pool"))
            block.sync(run("sp"))


def build(NT, dbg=None):
    nc = bass.Bass("TRN2", target_bir_lowering=False)
    S = NT * 128

    def din(name, shape, dt=F32):
        return nc.dram_tensor(name, list(shape), dt, kind="ExternalInput").ap()

    x_d = din("x", [S, D])
    y_d = nc.dram_tensor("y", [S, D], F32, kind="ExternalOutput").ap()
    ccol_d = din("ccol", [128, 8])
    pos_d = din("pos", [128, NT], I32)
    invf_d = din("invf", [128, 8])
    modw_d = [din("modw0", [D, 6 * D]), din("modw1", [D, 6 * D])]
    modbcol_d = din("modbcol", [128, 2, 48])
    gaterow_d = din("gaterow", [128, 4, D])
    kvmodw_d = din("kvmodw", [D, 2 * D])
    kvmodbcol_d = din("kvmodbcol", [128, 16])
    ngcol_d = din("ngcol", [128, 5, 8])
    gfrow_d = din("gfrow", [128, D])
    w_in_d = din("w_in", [D, 3088])
    wg2_d = din("wg2", [16, 512])
    bg2row_d = din("bg2row", [128, 512])
    gnrow_d = din("gnrow", [128, 256])
    gwo_d = din("gwo", [D, D])
    kvw_d = din("kvw", [D, 512])
    swq_d = din("swq", [D, D])
    sinkrow_d = din("sinkrow", [128, 16])
    swo_d = din("swo", [D, D])
    pwq_d = [din("pwq0", [D, 2048]), din("pwq1", [D, 2048])]
    skT_d = din("skT", [128, 2, 2, 128])
    ut_d = [din("ut0", [D, NEXP]), din("ut1", [D, NEXP])]
    v_d = [din("v0", [NEXP, D]), din("v1", [NEXP, D])]
    ident_d = din("ident", [128, 128])
    tri_d = din("tri", [128, 128])
    blk_d = din("blk", [128, 128])
    csel_d = din("csel", [128, 2])
    maskT_d = din("maskT", [128, 128])
    swam_d = din("swam", [128, 2, 256])
    utb_d = [nc.dram_tensor("utb%d" % l, [D, NEXP], BF16, kind="Internal").ap() for l in range(2)]
    vb_d = [nc.dram_tensor("vb%d" % l, [NEXP, D], BF16, kind="Internal").ap() for l in range(2)]
    wb16 = {nm: nc.dram_tensor("b16_" + nm, [D, w], BF16, kind="Internal").ap()
            for nm, w in (("w_in", 3072), ("gwo", D), ("kvw", 512), ("swq", D), ("swo", D))}
    wsrc = {"w_in": w_in_d, "gwo": gwo_d, "kvw": kvw_d, "swq": swq_d, "swo": swo_d}
    dbg_d = None
    if dbg is not None:
        dbg_d = nc.dram_tensor("dbg", [128, 8192], F32, kind="ExternalOutput").ap()

    es = ExitStack()
    with es:
        P = Prog(nc, es)

        def sb(name, shape, dt=F32):
            return es.enter_context(nc.sbuf_tensor("sb_" + name, list(shape), dt))

        def psum(name):
            return es.enter_context(nc.psum_tensor(name, [128, 512], F32))

        ident = sb("ident", [128, 128])
        identb = sb("identb", [128, 128], BF16)
        tri = sb("tri", [128, 128])
        blk = sb("blk", [128, 128])
        csel = sb("csel", [128, 2])
        maskT = sb("maskT", [128, 128])
        swam = sb("swam", [128, 2, 256])
        gates = sb("gates", [128, 4, D])
        gfrow = sb("gfrow", [128, D])
        gnrow = sb("gnrow", [128, 256])
        bg2row = sb("bg2row", [128, 512])
        sinkrow = sb("sinkrow", [128, 16])
        wg2 = sb("wg2", [16, 512])
        skT = sb("skT", [128, 2, 2, 128])
        modc = sb("modc", [128, 10, 8])
        ngcol = sb("ngcol", [128, 5, 8])
        modbcol = sb("modbcol", [128, 2, 48])
        kvmodbcol = sb("kvmodbcol", [128, 16])
        ccol = sb("ccol", [128, 8])
        cact = sb("cact", [128, 8])
        cbc = sb("cbc", [128, 8, 128])
        cosT = sb("cosT", [128, NT, 8])
        sinT = sb("sinT", [128, NT, 8])
        posi = sb("posi", [128, NT], I32)
        invf = sb("invf", [128, 8])
        xt = [sb("xt0", [128, D]), sb("xt1", [128, D])]
        xn = sb("xn", [128, D])
        hT = sb("hT", [128, KC, 128])
        hTb = sb("hTb", [128, KC, 128], BF16)
        junk = sb("junk", [128, D], BF16)
        ss = sb("ss", [128, 8])
        sm = sb("sm", [128, 1024])
        wbuf = [sb("wbuf0", [128, KC, 512]), sb("wbuf1", [128, KC, 512])]
        NSLOT = 12
        slot = [sb("slot%d" % i, [128, 2048]) for i in range(NSLOT)]
        Sst = [sb("Sa", [128, 4, 256]), sb("Sb", [128, 4, 256])]
        kTd = [sb("kTd0", [128, 4, 128]), sb("kTd1", [128, 4, 128])]
        vbd = [sb("vbd0", [128, 256]), sb("vbd1", [128, 256])]
        pb = [psum("p%d" % i) for i in range(8)]

        def SV(i, lo, hi):
            return slot[i][:, lo:hi]

        consts = [(ident, ident_d), (tri, tri_d), (blk, blk_d), (csel, csel_d), (maskT, maskT_d), (swam, swam_d),
                  (gates, gaterow_d), (gfrow, gfrow_d), (gnrow, gnrow_d), (bg2row, bg2row_d), (sinkrow, sinkrow_d),
                  (wg2, wg2_d), (skT, skT_d), (ngcol, ngcol_d), (modbcol, modbcol_d), (kvmodbcol, kvmodbcol_d),
                  (ccol, ccol_d), (posi, pos_d), (invf, invf_d)]
        P.dma("const", [], ["const"], [(t[:], d) for (t, d) in consts])
        P.op("dve", "memset", [], ["Sa"], Sst[0][:], 0.0)
        P.op("dve", "memset", [], ["k1"], kTd[1][:], 0.0)
        P.op("dve", "memset", [], ["v1"], vbd[1][:], 0.0)
        P.op("dve", "tensor_copy", ["const"], ["identb"], out=identb[:], in_=ident[:])
        P.op("act", "activation", ["const"], ["cact"], out=cact[:], in_=ccol[:], func=ACT.Silu)
        P.op("dve", "tensor_copy", ["cact"], ["cbc"], out=cbc[:], in_=cact[:].unsqueeze(2).to_broadcast([128, 8, 128]))
        posf = sm[:, 0:NT]
        ang = slot[0][:, 0:NT * 8].rearrange("p (a b) -> p a b", b=8)
        ang2 = slot[0][:, 1024:1024 + NT * 8].rearrange("p (a b) -> p a b", b=8)
        kf = slot[1][:, 0:NT * 8].rearrange("p (a b) -> p a b", b=8)
        ki = slot[2][:, 0:NT * 8].bitcast(I32).rearrange("p (a b) -> p a b", b=8)
        P.op("dve", "tensor_copy", ["const"], ["posf"], out=posf, in_=posi[:])
        P.op("dve", "tensor_tensor", ["posf", "const"], ["ang"], out=ang, in0=posf.unsqueeze(2).to_broadcast([128, NT, 8]),
             in1=invf[:].unsqueeze(1).to_broadcast([128, NT, 8]), op=ALU.mult)
        P.op("dve", "tensor_scalar_add", ["ang"], ["ang2"], out=ang2, in0=ang, scalar1=PI / 2)
        for (src, nm, dst) in ((ang, "ang", sinT), (ang2, "ang2", cosT)):
            P.op("dve", "tensor_scalar", [nm], ["ki"], out=ki, in0=src, scalar1=float(1.0 / (2 * PI)), scalar2=None, op0=ALU.mult)
            P.op("dve", "tensor_copy", ["ki"], ["kf"], out=kf, in_=ki)
            P.op("dve", "scalar_tensor_tensor", ["kf", nm], [nm], out=src, in0=kf, scalar=float(-2 * PI), in1=src, op0=ALU.mult, op1=ALU.add)
            P.op("dve", "tensor_single_scalar", [nm], ["kf"], out=kf, in_=src, scalar=PI, op=ALU.is_gt)
            P.op("dve", "scalar_tensor_tensor", ["kf", nm], [nm], out=src, in0=kf, scalar=float(-2 * PI), in1=src, op0=ALU.mult, op1=ALU.add)
            P.op("dve", "tensor_single_scalar", [nm], ["kf"], out=kf, in_=src, scalar=-PI, op=ALU.is_lt)
            P.op("dve", "scalar_tensor_tensor", ["kf", nm], [nm], out=src, in0=kf, scalar=float(2 * PI), in1=src, op0=ALU.mult, op1=ALU.add)
            P.op("act", "activation", [nm], [nm + "_out"], out=dst[:], in_=src, func=ACT.Sin)

        wsel = [0]

        def wload(dram_w, c0, ncols):
            i = wsel[0]
            wsel[0] ^= 1
            nm = "wbuf%d" % i
            P.dma(nm, [], [nm], [(wbuf[i][:, :, 0:ncols], dram_w[:, c0:c0 + ncols].rearrange("(kc p) n -> p kc n", p=128))])
            return nm, wbuf[i]

        mcol = sm[:, 64:64 + 96].rearrange("p (a b) -> p a b", b=8)
        for l in range(2):
            for bi in range(12):
                nm, wb = wload(modw_d[l], bi * 512, 512)
                kind = bi // 2
                if kind in (2, 5):
                    gi = l * 2 + (0 if kind == 2 else 1)
                    half = bi % 2
                    for kc in range(KC):
                        P.op("pe", "matmul", [nm, "cbc"], ["p0"], pb[0][:, :], lhsT=cbc[:, kc, :], rhs=wb[:, kc, :], start=(kc == 0), stop=(kc == KC - 1))
                    P.op("dve", "tensor_tensor", ["p0", "const"], ["gates"], out=gates[:, gi, half * 512:(half + 1) * 512], in0=pb[0][:, :],
                         in1=gates[:, gi, half * 512:(half + 1) * 512], op=ALU.add)
                else:
                    for j in range(4):
                        for kc in range(KC):
                            P.op("pe", "matmul", [nm, "cact"], ["p1"], pb[1][:, j:j + 1], lhsT=wb[:, kc, j * 128:(j + 1) * 128], rhs=cact[:, kc:kc + 1],
                                 start=(kc == 0), stop=(kc == KC - 1))
                    vi = {0: 0, 1: 1, 3: 2, 4: 3}[kind]
                    P.op("dve", "tensor_tensor", ["p1", "const"], ["mcol"], out=mcol[:, l * 4 + vi, (bi % 2) * 4:(bi % 2) * 4 + 4], in0=pb[1][:, 0:4],
                         in1=modbcol[:, l, bi * 4:bi * 4 + 4], op=ALU.add)
        for bi in range(4):
            nm, wb = wload(kvmodw_d, bi * 512, 512)
            for j in range(4):
                for kc in range(KC):
                    P.op("pe", "matmul", [nm, "cact"], ["p1"], pb[1][:, j:j + 1], lhsT=wb[:, kc, j * 128:(j + 1) * 128], rhs=cact[:, kc:kc + 1],
                         start=(kc == 0), stop=(kc == KC - 1))
            P.op("dve", "tensor_tensor", ["p1", "const"], ["mcol"], out=mcol[:, 8 + bi // 2, (bi % 2) * 4:(bi % 2) * 4 + 4], in0=pb[1][:, 0:4],
                 in1=kvmodbcol[:, bi * 4:bi * 4 + 4], op=ALU.add)
        for (mi, gi, shi, sci) in ((0, 0, 0, 1), (2, 1, 2, 3), (4, 4, 8, 9), (6, 2, 4, 5), (8, 3, 6, 7)):
            P.op("dve", "scalar_tensor_tensor", ["mcol", "const"], ["modc"], out=modc[:, mi, :], in0=mcol[:, sci, :], scalar=1.0, in1=ngcol[:, gi, :],
                 op0=ALU.add, op1=ALU.mult)
            P.op("dve", "tensor_copy", ["mcol"], ["modc"], out=modc[:, mi + 1, :], in_=mcol[:, shi, :])
        P.barrier()

        cv = [0]

        def convert(src_ap, dst_ap, W=4096):
            i = cv[0] % 3
            cv[0] += 1
            H = W // 2
            P.dma("cin%d" % i, [], ["cin%d" % i], [(slot[2 * i][:, 0:H], src_ap[:, 0:H]), (slot[2 * i + 1][:, 0:H], src_ap[:, H:W])])
            ob = slot[6 + i][:, :].bitcast(BF16)
            eng = ("dve", "act", "pool")[i]
            if eng == "act":
                P.op("act", "activation", ["cin%d" % i], ["cob%d" % i], out=ob[:, 0:H], in_=slot[2 * i][:, 0:H], func=ACT.Copy)
                P.op("act", "activation", ["cin%d" % i], ["cob%d" % i], out=ob[:, H:W], in_=slot[2 * i + 1][:, 0:H], func=ACT.Copy)
            else:
                P.op(eng, "tensor_copy", ["cin%d" % i], ["cob%d" % i], out=ob[:, 0:H], in_=slot[2 * i][:, 0:H])
                P.op(eng, "tensor_copy", ["cin%d" % i], ["cob%d" % i], out=ob[:, H:W], in_=slot[2 * i + 1][:, 0:H])
            P.dma("cout%d" % i, ["cob%d" % i], [], [(dst_ap, ob[:, 0:W])])

        if BF16_PROJ:
            for nm in ("w_in", "gwo", "kvw", "swq", "swo"):
                W = wb16[nm].shape[1]
                for kc in range(KC):
                    convert(wsrc[nm][kc * 128:(kc + 1) * 128, 0:W], wb16[nm][kc * 128:(kc + 1) * 128, :], W=W)

        for l in range(2):
            for kc in range(KC):
                for e4 in range(4):
                    convert(ut_d[l][kc * 128:(kc + 1) * 128, e4 * 4096:(e4 + 1) * 4096],
                            utb_d[l][kc * 128:(kc + 1) * 128, e4 * 4096:(e4 + 1) * 4096])
            for r in range(32):
                convert(v_d[l][r * 512:(r + 1) * 512, :].rearrange("(p j) d -> p (j d)", j=4),
                        vb_d[l][r * 512:(r + 1) * 512, :].rearrange("(p j) d -> p (j d)", j=4))
        P.barrier()

        def modulate(xa, xname, mi, bf16=False):
            P.op("act", "activation", [xname], ["junk", "ss0"], out=junk[:], in_=xa, func=ACT.Square, accum_out=ss[:, 0:1])
            P.op("dve", "tensor_scalar", ["ss0"], ["ss1"], out=ss[:, 1:2], in0=ss[:, 0:1], scalar1=1.0 / D, scalar2=EPS, op0=ALU.mult, op1=ALU.add)
            P.op("act", "activation", ["ss1"], ["ss2"], out=ss[:, 2:3], in_=ss[:, 1:2], func=ACT.Sqrt)
            P.op("dve", "reciprocal", ["ss2"], ["ss3"], out=ss[:, 3:4], in_=ss[:, 2:3])
            P.op("dve", "tensor_scalar", [xname, "ss3"], ["xn"], out=xn[:], in0=xa, scalar1=ss[:, 3:4], scalar2=None, op0=ALU.mult)
            for kc in range(KC):
                bnk = kc // 4
                P.op("pe", "transpose", ["xn", "const"], ["p%d" % bnk], out=pb[bnk][:, (kc % 4) * 128:(kc % 4 + 1) * 128],
                     in_=xn[:, kc * 128:(kc + 1) * 128], identity=ident[:])
            for kc in range(KC):
                bnk = kc // 4
                src = pb[bnk][:, (kc % 4) * 128:(kc % 4 + 1) * 128]
                if MODEV == "none":
                    continue
                if (kc % 2 == 0 and MODEV == "mix") or MODEV == "dve":
                    P.op("dve", "tensor_scalar", ["p%d" % bnk, "modc"], ["hT%d" % kc], out=hT[:, kc, :], in0=src,
                         scalar1=modc[:, mi, kc:kc + 1], scalar2=modc[:, mi + 1, kc:kc + 1], op0=ALU.mult, op1=ALU.add)
                else:
                    P.op("act", "activation", ["p%d" % bnk, "modc"], ["hT%d" % kc], out=hT[:, kc, :], in_=src, func=ACT.Identity,
                         scale=modc[:, mi, kc:kc + 1], bias=modc[:, mi + 1, kc:kc + 1])
            if bf16:
                P.op("pool", "tensor_copy", ["hT%d" % kc for kc in range(KC)], ["hTb"], out=hTb[:], in_=hT[:])
            return ["hT%d" % kc for kc in range(KC)]

        def wload_b(wname, c0, ncols):
            i = wsel[0]
            wsel[0] ^= 1
            nm = "wbuf%d" % i
            wv = wbuf[i][:].rearrange("p a b -> p (a b)").bitcast(BF16)[:, 0:4096].rearrange("p (kc n) -> p kc n", kc=8)
            P.dma(nm, [], [nm], [(wv[:, :, 0:ncols], wb16[wname][:, c0:c0 + ncols].rearrange("(kc p) n -> p kc n", p=128))])
            return nm, wv

        def dense_block(hTn, dram_w, c0, ncols, bank, bname=None):
            if BF16_PROJ and bname is not None:
                nm, wb = wload_b(bname, c0, ncols)
                for kc in range(KC):
                    P.op("pe", "matmul", [nm, "hTb"], ["p%d" % bank], pb[bank][:, 0:ncols], lhsT=hTb[:, kc, :], rhs=wb[:, kc, 0:ncols],
                         start=(kc == 0), stop=(kc == KC - 1))
                return
            nm, wb = wload(dram_w, c0, ncols)
            for kc in range(KC):
                P.op("pe", "matmul", [nm, "hT%d" % kc], ["p%d" % bank], pb[bank][:, 0:ncols], lhsT=hT[:, kc, :], rhs=wb[:, kc, 0:ncols],
                     start=(kc == 0), stop=(kc == KC - 1))

        def out_proj(onm, oap, dram_w, gi, xa, xname, bname=None):
            useb = BF16_PROJ and bname is not None
            if useb:
                oT = SV(5, 1024, 1536).bitcast(BF16).rearrange("p (a b) -> p a b", a=8)
            else:
                oT = SV(5, 1024, 2048).rearrange("p (a b) -> p a b", a=8)
            for kc in range(KC):
                bnk = kc // 4
                P.op("pe", "transpose", [onm, "const"], ["p%d" % bnk], out=pb[bnk][:, (kc % 4) * 128:(kc % 4 + 1) * 128],
                     in_=oap[:, kc * 128:(kc + 1) * 128], identity=ident[:])
            P.op("act", "activation", ["p0"], ["oT0"], out=oT[:, 0:4, :], in_=pb[0][:, :].rearrange("p (a b) -> p a b", a=4), func=ACT.Copy)
            P.op("dve", "tensor_copy", ["p1"], ["oT1"], out=oT[:, 4:8, :], in_=pb[1][:, :].rearrange("p (a b) -> p a b", a=4))
            for half in range(2):
                if useb:
                    nm, wb = wload_b(bname, half * 512, 512)
                else:
                    nm, wb = wload(dram_w, half * 512, 512)
                bank = 2 + half
                for kc in range(KC):
                    P.op("pe", "matmul", [nm, "oT%d" % (kc // 4)], ["p%d" % bank], pb[bank][:, :], lhsT=oT[:, kc, :], rhs=wb[:, kc, 0:512],
                         start=(kc == 0), stop=(kc == KC - 1))
                P.op("dve", "tensor_tensor", ["p%d" % bank, "gates"], ["ytmp%d" % half], out=sm[:, half * 512:(half + 1) * 512], in0=pb[bank][:, :],
                     in1=gates[:, gi, half * 512:(half + 1) * 512], op=ALU.mult)
                P.op("pool", "tensor_tensor", ["ytmp%d" % half, xname], [xname], out=xa[:, half * 512:(half + 1) * 512],
                     in0=xa[:, half * 512:(half + 1) * 512], in1=sm[:, half * 512:(half + 1) * 512], op=ALU.add)

        def gla(xa, xname, it):
            hTn = modulate(xa, xname, 0, bf16=BF16_PROJ)
            if GLA_CUT <= 0:
                return
            qk = SV(0, 0, 1024)
            la = SV(0, 1024, 1536)
            zb = SV(0, 1536, 2048)
            vv = SV(1, 0, 1024)
            og = SV(1, 1024, 2048)
            eb = SV(2, 0, 512)
            enb = SV(2, 512, 1024)
            ebl = SV(2, 1024, 1536)
            scm = SV(2, 1536, 2048).rearrange("p (a b) -> p a b", a=4)
            qt = SV(3, 0, 512)
            kt = SV(3, 512, 1024)
            kdec = SV(3, 1024, 1536)
            glT = slot[3][0:16, 1536:1664]
            dec = SV(3, 1664, 1672)
            qtT0 = SV(4, 0, 512).rearrange("p (a b) -> p a b", a=4)
            qtT1 = SV(4, 512, 1024).rearrange("p (a b) -> p a b", a=4)
            ktT = SV(4, 1024, 1536).rearrange("p (a b) -> p a b", a=4)
            qtT = SV(4, 1536, 2048).rearrange("p (a b) -> p a b", a=4)
            osb = SV(5, 0, 1024)
            dsts = [(qk[:, 0:512], "q"), (qk[:, 512:1024], "k"), (vv[:, 0:512], "v0"), (vv[:, 512:1024], "v1"),
                    (og[:, 0:512], "og0"), (og[:, 512:1024], "og1")]
            for bi, (dst, dn) in enumerate(dsts):
                bank = 2 + (bi % 2)
                dense_block(hTn, w_in_d, bi * 512, 512, bank, bname="w_in")
                if bi % 2 == 0:
                    P.op("act", "activation", ["p%d" % bank], [dn], out=dst, in_=pb[bank][:, :], func=ACT.Copy)
                else:
                    P.op("dve", "tensor_copy", ["p%d" % bank], [dn], out=dst, in_=pb[bank][:, :])
            if GLA_CUT <= 1:
                return
            nm, wb = wload(w_in_d, 3072, 16)
            for kc in range(KC):
                P.op("pe", "matmul", [nm, "hT%d" % kc], ["p4"], pb[4][0:16, 0:128], lhsT=wb[:, kc, 0:16], rhs=hT[:, kc, :], start=(kc == 0), stop=(kc == KC - 1))
            P.op("dve", "tensor_copy", ["p4"], ["glT"], out=glT, in_=pb[4][0:16, 0:128])
            P.op("pe", "matmul", ["glT", "const"], ["p5"], pb[5][:, :], lhsT=glT, rhs=wg2[:, :], start=True, stop=True)
            P.op("dve", "tensor_tensor", ["p5", "const"], ["zb"], out=zb, in0=pb[5][:, :], in1=bg2row[:], op=ALU.add)
            P.op("act", "activation", ["zb"], ["zb"], out=zb, in_=zb, func=ACT.Exp, scale=-1.0)
            P.op("act", "activation", ["zb"], ["la"], out=la, in_=zb, func=ACT.Ln, bias=1.0)
            if GLA_CUT <= 2:
                return
            P.op("pe", "matmul", ["la", "const"], ["p4"], pb[4][:, :], lhsT=tri[:], rhs=la, start=True, stop=True)
            P.op("pe", "matmul", ["la", "const"], ["p5"], pb[5][:, :], lhsT=blk[:], rhs=la, start=True, stop=True)
            for h in range(4):
                P.op("pe", "matmul", ["la", "const"], ["p6"], pb[6][:, 2 * h:2 * h + 2], lhsT=la[:, h * 128:(h + 1) * 128], rhs=csel[:], start=True, stop=True)
            P.op("act", "activation", ["p4"], ["eb"], out=eb, in_=pb[4][:, :], func=ACT.Exp)
            P.op("act", "activation", ["p4"], ["enb"], out=enb, in_=pb[4][:, :], func=ACT.Exp, scale=-1.0)
            P.op("act", "activation", ["p5"], ["ebl"], out=ebl, in_=pb[5][:, :], func=ACT.Exp)
            P.op("act", "activation", ["p6"], ["dec"], out=dec, in_=pb[6][:, 0:8], func=ACT.Exp)
            P.op("dve", "scalar_tensor_tensor", ["q", "eb"], ["qt"], out=qt, in0=qk[:, 0:512], scalar=float(128 ** -0.5), in1=eb, op0=ALU.mult, op1=ALU.mult)
            P.op("dve", "tensor_tensor", ["k", "enb"], ["kt"], out=kt, in0=qk[:, 512:1024], in1=enb, op=ALU.mult)
            P.op("pool", "tensor_tensor", ["kt", "ebl"], ["kdec"], out=kdec, in0=kt, in1=ebl, op=ALU.mult)
            if GLA_CUT <= 3:
                return
            for h in range(4):
                P.op("pe", "transpose", ["qt", "const"], ["p0"], out=pb[0][:, h * 128:(h + 1) * 128], in_=qt[:, h * 128:(h + 1) * 128], identity=ident[:])
            for h in range(4):
                P.op("pe", "transpose", ["kt", "const"], ["p1"], out=pb[1][:, h * 128:(h + 1) * 128], in_=kt[:, h * 128:(h + 1) * 128], identity=ident[:])
            p0v = pb[0][:, :].rearrange("p (a b) -> p a b", a=4)
            P.op("act", "activation", ["p0"], ["qtT"], out=qtT, in_=p0v, func=ACT.Copy)
            P.op("pool", "memset", [], ["qtT0", "qtT1"], slot[4][:, 0:1024], 0.0)
            P.op("dve", "tensor_copy", ["p0"], ["qtT0"], out=qtT0[:, :, 0:64], in_=p0v[:, :, 0:64])
            P.op("dve", "tensor_copy", ["p0"], ["qtT1"], out=qtT1[:, :, 64:128], in_=p0v[:, :, 64:128])
            P.op("act", "activation", ["p1"], ["ktT"], out=ktT, in_=pb[1][:, :].rearrange("p (a b) -> p a b", a=4), func=ACT.Copy)
            if GLA_CUT <= 4:
                return
            for h in range(4):
                P.op("pe", "matmul", ["ktT", "qtT"], ["p4"], pb[4][:, h * 128:(h + 1) * 128], lhsT=ktT[:, h, :], rhs=qtT[:, h, :], start=True, stop=True)
            P.op("dve", "tensor_tensor", ["p4", "const"], ["scm"], out=scm, in0=pb[4][:, :].rearrange("p (a b) -> p a b", a=4),
                 in1=maskT[:].unsqueeze(1).to_broadcast([128, 4, 128]), op=ALU.mult)
            if GLA_CUT <= 5:
                return
            Sa, Sb = Sst[0], Sst[1]
            for h in range(4):
                vh = vv[:, h * 256:(h + 1) * 256]
                vn = "v%d" % (h // 2)
                kvb = 5 + (h % 2)
                ob = 2 + (h // 2)
                oreg = pb[ob][:, (h % 2) * 256:(h % 2 + 1) * 256]
                P.op("pe", "matmul", ["kdec", vn], ["p%d" % kvb], pb[kvb][:, 0:256], lhsT=kdec[0:64, h * 128:(h + 1) * 128], rhs=vv[0:64, h * 256:(h + 1) * 256],
                     start=True, stop=True)
                P.op("dve", "scalar_tensor_tensor", ["Sa", "dec", "p%d" % kvb], ["Sb"], out=Sb[:, h, :], in0=Sa[:, h, :], scalar=dec[:, 2 * h:2 * h + 1],
                     in1=pb[kvb][:, 0:256], op0=ALU.mult, op1=ALU.add)
                P.op("pe", "matmul", ["scm", vn], ["p%d" % ob], oreg, lhsT=scm[:, h, :], rhs=vh, start=True, stop=False)
                P.op("pe", "matmul", ["qtT0", "Sa"], ["p%d" % ob], oreg, lhsT=qtT0[:, h, :], rhs=Sa[:, h, :], start=False, stop=False)
                P.op("pe", "matmul", ["qtT1", "Sb"], ["p%d" % ob], oreg, lhsT=qtT1[:, h, :], rhs=Sb[:, h, :], start=False, stop=True)
                P.op("pe", "matmul", ["kdec", vn], ["p%d" % kvb], pb[kvb][:, 256:512], lhsT=kdec[64:128, h * 128:(h + 1) * 128], rhs=vv[64:128, h * 256:(h + 1) * 256],
                     start=True, stop=True)
                P.op("dve", "scalar_tensor_tensor", ["Sb", "dec", "p%d" % kvb], ["Sa"], out=Sa[:, h, :], in0=Sb[:, h, :], scalar=dec[:, 2 * h + 1:2 * h + 2],
                     in1=pb[kvb][:, 256:512], op0=ALU.mult, op1=ALU.add)
            if GLA_CUT <= 6:
                return
            for h in range(4):
                ob = 2 + (h // 2)
                oreg = pb[ob][:, (h % 2) * 256:(h % 2 + 1) * 256]
                P.op("act", "activation", ["p%d" % ob], ["junk", "oss%d" % h], out=junk[:, 0:256], in_=oreg, func=ACT.Square, accum_out=ss[:, 4 + h:5 + h])
            P.op("dve", "tensor_scalar", ["oss%d" % h for h in range(4)], ["orv"], out=sm[:, 0:4], in0=ss[:, 4:8], scalar1=1.0 / 256, scalar2=EPS, op0=ALU.mult, op1=ALU.add)
            P.op("act", "activation", ["orv"], ["ors"], out=sm[:, 4:8], in_=sm[:, 0:4], func=ACT.Sqrt)
            P.op("dve", "reciprocal", ["ors"], ["orr"], out=sm[:, 8:12], in_=sm[:, 4:8])
            for h in range(4):
                ob = 2 + (h // 2)
                oreg = pb[ob][:, (h % 2) * 256:(h % 2 + 1) * 256]
                P.op("dve", "scalar_tensor_tensor", ["p%d" % ob, "orr", "const"], ["osb"], out=osb[:, h * 256:(h + 1) * 256], in0=oreg, scalar=sm[:, 8 + h:9 + h],
                     in1=gnrow[:], op0=ALU.mult, op1=ALU.mult)
            P.op("act", "activation", ["og0", "og1"], ["ogs"], out=og, in_=og, func=ACT.Silu)
            P.op("dve", "tensor_tensor", ["osb", "ogs"], ["osb"], out=osb, in0=osb, in1=og, op=ALU.mult)
            if GLA_CUT <= 7:
                return
            out_proj("osb", osb, gwo_d, 0, xa, xname, bname="gwo")

        def kv_phase(xa, xname, it):
            cur = it % 2
            hTn = modulate(xa, xname, 4, bf16=BF16_PROJ)
            dense_block(hTn, kvw_d, 0, 512, 2, bname="kvw")
            kdup = SV(6, 0, 512).rearrange("p (g c d) -> p g c d", g=4, c=2)
            tmp = SV(6, 512, 768).rearrange("p (a g d) -> p a g d", a=8, g=4)
            kp = pb[2][:, 0:256].rearrange("p (g d) -> p g d", g=4)
            P.op("act", "activation", ["p2"], ["v%d" % cur], out=vbd[cur][:], in_=pb[2][:, 256:512], func=ACT.Copy)
            P.op("dve", "tensor_copy", ["p2"], ["kdup"], out=kdup[:, :, 0, :], in_=kp)
            cb = cosT[:, it, :].unsqueeze(1).to_broadcast([128, 4, 8])
            sbb = sinT[:, it, :].unsqueeze(1).to_broadcast([128, 4, 8])
            x1 = kdup[:, :, 0, 0:8]
            x2 = kdup[:, :, 0, 8:16]
            P.op("dve", "tensor_tensor", ["kdup"], ["t0"], out=tmp[:, 0], in0=x1, in1=cb, op=ALU.mult)
            P.op("dve", "tensor_tensor", ["kdup"], ["t1"], out=tmp[:, 1], in0=x2, in1=sbb, op=ALU.mult)
            P.op("dve", "tensor_tensor", ["kdup"], ["t2"], out=tmp[:, 2], in0=x2, in1=cb, op=ALU.mult)
            P.op("dve", "tensor_tensor", ["kdup"], ["t3"], out=tmp[:, 3], in0=x1, in1=sbb, op=ALU.mult)
            P.op("dve", "tensor_tensor", ["t0", "t1"], ["kdup"], out=x1, in0=tmp[:, 0], in1=tmp[:, 1], op=ALU.subtract)
            P.op("dve", "tensor_tensor", ["t2", "t3"], ["kdup"], out=x2, in0=tmp[:, 2], in1=tmp[:, 3], op=ALU.add)
            P.op("dve", "tensor_copy", ["kdup"], ["kdup"], out=kdup[:, :, 1, :], in_=kdup[:, :, 0, :])
            kflat = SV(6, 0, 512)
            for g in range(4):
                P.op("pe", "transpose", ["kdup", "const"], ["p0"], out=pb[0][:, g * 128:(g + 1) * 128], in_=kflat[:, g * 128:(g + 1) * 128], identity=ident[:])
            P.op("act", "activation", ["p0"], ["k%d" % cur], out=kTd[cur][:], in_=pb[0][:, :].rearrange("p (a b) -> p a b", a=4), func=ACT.Copy)

        def swa(xa, xname, it):
            cur = it % 2
            prv = 1 - cur
            hTn = modulate(xa, xname, 6, bf16=BF16_PROJ)
            q = SV(0, 0, 1024)
            q3 = q.rearrange("p (h d) -> p h d", h=16)
            qT = SV(0, 1024, 2048).rearrange("p (a b) -> p a b", a=8)
            sc = [SV(1, 0, 2048).rearrange("p (h m) -> p h m", h=8), SV(2, 0, 2048).rearrange("p (h m) -> p h m", h=8)]
            pT = [SV(3, 0, 2048).rearrange("p (h c m) -> p h c m", h=8, c=2), SV(4, 0, 2048).rearrange("p (h c m) -> p h c m", h=8, c=2)]
            o = SV(5, 0, 1024)
            tmp = SV(6, 1024, 2048).rearrange("p (a h d) -> p a h d", a=4, h=16)
            for half in range(2):
                dense_block(hTn, swq_d, half * 512, 512, 2 + half, bname="swq")
                if half == 0:
                    P.op("act", "activation", ["p2"], ["q"], out=q[:, 0:512], in_=pb[2][:, :], func=ACT.Copy)
                else:
                    P.op("dve", "tensor_copy", ["p3"], ["q"], out=q[:, 512:1024], in_=pb[3][:, :])
            if SWA_CUT <= 1:
                return
            cb = cosT[:, it, :].unsqueeze(1).to_broadcast([128, 16, 8])
            sbb = sinT[:, it, :].unsqueeze(1).to_broadcast([128, 16, 8])
            x1 = q3[:, :, 0:8]
            x2 = q3[:, :, 8:16]
            tv = SV(6, 1024, 1536).rearrange("p (a h d) -> p a h d", a=4, h=16)
            P.op("dve", "tensor_tensor", ["q"], ["t0"], out=tv[:, 0], in0=x1, in1=cb, op=ALU.mult)
            P.op("dve", "tensor_tensor", ["q"], ["t1"], out=tv[:, 1], in0=x2, in1=sbb, op=ALU.mult)
            P.op("dve", "tensor_tensor", ["q"], ["t2"], out=tv[:, 2], in0=x2, in1=cb, op=ALU.mult)
            P.op("dve", "tensor_tensor", ["q"], ["t3"], out=tv[:, 3], in0=x1, in1=sbb, op=ALU.mult)
            P.op("dve", "tensor_tensor", ["t0", "t1"], ["q"], out=x1, in0=tv[:, 0], in1=tv[:, 1], op=ALU.subtract)
            P.op("dve", "tensor_tensor", ["t2", "t3"], ["q"], out=x2, in0=tv[:, 2], in1=tv[:, 3], op=ALU.add)
            if SWA_CUT <= 2:
                return
            for j in range(8):
                bnk = j // 4
                P.op("pe", "transpose", ["q", "const"], ["p%d" % bnk], out=pb[bnk][:, (j % 4) * 128:(j % 4 + 1) * 128], in_=q[:, j * 128:(j + 1) * 128], identity=ident[:])
            P.op("act", "activation", ["p0"], ["qT0"], out=qT[:, 0:4, :], in_=pb[0][:, :].rearrange("p (a b) -> p a b", a=4), func=ACT.Copy)
            P.op("dve", "tensor_copy", ["p1"], ["qT1"], out=qT[:, 4:8, :], in_=pb[1][:, :].rearrange("p (a b) -> p a b", a=4))
            if SWA_CUT <= 3:
                return
            mk = swam[:, 0 if it == 0 else 1, :]
            for grp in range(4):
                bankE = 4 + 2 * (grp % 2)
                bankO = bankE + 1
                for pj in range(2):
                    j = 2 * grp + pj
                    for hh in range(2):
                        h = 2 * j + hh
                        g = h // 4
                        base = 64 * hh
                        bank = bankE if hh == 0 else bankO
                        col = pj * 256
                        P.op("pe", "matmul", ["qT%d" % (j // 4), "k%d" % prv], ["p%d" % bank], pb[bank][:, col:col + 128],
                             lhsT=qT[base:base + 64, j, :], rhs=kTd[prv][base:base + 64, g, :], start=True, stop=True)
                        P.op("pe", "matmul", ["qT%d" % (j // 4), "k%d" % cur], ["p%d" % bank], pb[bank][:, col + 128:col + 256],
                             lhsT=qT[base:base + 64, j, :], rhs=kTd[cur][base:base + 64, g, :], start=True, stop=True)
                for hh in range(2):
                    bank = bankE if hh == 0 else bankO
                    for pj in range(2):
                        j = 2 * grp + pj
                        h = 2 * j + hh
                        P.op("dve", "scalar_tensor_tensor", ["p%d" % bank, "const"], ["sc%d" % j], out=sc[h // 8][:, h % 8, :],
                             in0=pb[bank][:, pj * 256:(pj + 1) * 256], scalar=0.125, in1=mk, op0=ALU.mult, op1=ALU.add)
            if SWA_CUT <= 4:
                return
            rmax = sm[:, 0:16]
            mm = sm[:, 16:32]
            negm = sm[:, 32:48]
            rs = sm[:, 48:64]
            sk = sm[:, 64:80]
            den = sm[:, 80:96]
            rden = sm[:, 96:112]
            for half in range(2):
                P.op("dve", "tensor_reduce", ["sc%d" % j for j in range(half * 4, half * 4 + 4)], ["rmax%d" % half], out=rmax[:, half * 8:(half + 1) * 8],
                     in_=sc[half], axis=AX.X, op=ALU.max)
            P.op("dve", "tensor_tensor", ["rmax0", "rmax1", "const"], ["mm"], out=mm, in0=rmax, in1=sinkrow[:], op=ALU.max)
            P.op("dve", "tensor_scalar", ["mm"], ["negm"], out=negm, in0=mm, scalar1=-1.0, scalar2=None, op0=ALU.mult)
            P.op("dve", "tensor_tensor", ["mm", "const"], ["sk"], out=sk, in0=sinkrow[:], in1=mm, op=ALU.subtract)
            P.op("act", "activation", ["sk"], ["sk"], out=sk, in_=sk, func=ACT.Exp)
            for h in range(16):
                j = h // 2
                sl = sc[h // 8][:, h % 8, :]
                P.op("act", "activation", ["sc%d" % j, "negm"], ["sc%d" % j, "rs%d" % h], out=sl, in_=sl, func=ACT.Exp, bias=negm[:, h:h + 1], scale=1.0,
                     accum_out=rs[:, h:h + 1])
            P.op("dve", "tensor_tensor", ["rs%d" % h for h in range(16)] + ["sk"], ["den"], out=den, in0=rs, in1=sk, op=ALU.add)
            P.op("dve", "reciprocal", ["den"], ["rden"], out=rden, in_=den)
            if SWA_CUT <= 5:
                return
            for j in range(8):
                bank = j % 4
                for hh in range(2):
                    h = 2 * j + hh
                    for part in range(2):
                        P.op("pe", "transpose", ["sc%d" % j, "const"], ["p%d" % bank], out=pb[bank][:, (hh * 2 + part) * 128:(hh * 2 + part + 1) * 128],
                             in_=sc[h // 8][:, h % 8, part * 128:(part + 1) * 128], identity=ident[:])
                dstv = pT[j // 4][:, 2 * (j % 4):2 * (j % 4) + 2, :, :]
                srcv = pb[bank][:, :].rearrange("p (h c m) -> p h c m", h=2, c=2)
                if j % 2 == 0:
                    P.op("act", "activation", ["p%d" % bank], ["pT%d" % j], out=dstv, in_=srcv, func=ACT.Copy)
                else:
                    P.op("dve", "tensor_copy", ["p%d" % bank], ["pT%d" % j], out=dstv, in_=srcv)
            if SWA_CUT <= 6:
                return
            for h in range(16):
                j = h // 2
                g = h // 4
                bank = 4 + h // 8
                oreg = pb[bank][:, (h % 8) * 64:(h % 8 + 1) * 64]
                P.op("pe", "matmul", ["pT%d" % j, "v%d" % prv], ["p%d" % bank], oreg, lhsT=pT[h // 8][:, h % 8, 0, :], rhs=vbd[prv][:, g * 64:(g + 1) * 64],
                     start=True, stop=False)
                P.op("pe", "matmul", ["pT%d" % j, "v%d" % cur], ["p%d" % bank], oreg, lhsT=pT[h // 8][:, h % 8, 1, :], rhs=vbd[cur][:, g * 64:(g + 1) * 64],
                     start=False, stop=True)
            if SWA_CUT <= 7:
                return
            for b2 in range(2):
                P.op("dve", "tensor_tensor", ["p%d" % (4 + b2), "rden"], ["o"], out=o[:, b2 * 512:(b2 + 1) * 512].rearrange("p (h d) -> p h d", h=8),
                     in0=pb[4 + b2][:, :].rearrange("p (h d) -> p h d", h=8), in1=rden[:, b2 * 8:(b2 + 1) * 8].unsqueeze(2).to_broadcast([128, 8, 64]), op=ALU.mult)
            if SWA_CUT <= 8:
                return
            out_proj("o", o, swo_d, 2, xa, xname, bname="swo")

        def peer(xa, xname, l):
            mi = 2 if l == 0 else 8
            gi = 1 if l == 0 else 3
            hTn = modulate(xa, xname, mi, bf16=True)
            qT = SV(0, 0, 2048).rearrange("p (g t) -> p g t", g=16)
            s = SV(1, 0, 2048).rearrange("p (g n) -> p g n", g=16)
            sw = SV(2, 0, 2048).rearrange("p (g n) -> p g n", g=16)
            cand = SV(3, 0, 2048).rearrange("p (h a b) -> p h a b", h=8, a=16)
            cw = SV(4, 0, 2048).rearrange("p (h a b) -> p h a b", h=8, a=16)
            v16 = sm[:, 0:256].rearrange("p (g k) -> p g k", g=16)
            c16 = sm[:, 256:384].rearrange("p (h k) -> p h k", h=8)
            dd = sm[:, 384:512].rearrange("p (h k) -> p h k", h=8)
            Z = sm[:, 512:520]
            lnZ = sm[:, 520:528]
            bia = sm[:, 528:536]
            for bi in range(4):
                nm, wb = wload(pwq_d[l], bi * 512, 512)
                for gl in range(4):
                    g = bi * 4 + gl
                    for kc in range(KC):
                        P.op("pe", "matmul", [nm, "hT%d" % kc], ["p%d" % bi], pb[bi][:, gl * 128:(gl + 1) * 128], lhsT=wb[:, kc, gl * 128:(gl + 1) * 128], rhs=hT[:, kc, :],
                             start=(kc == 0), stop=(kc == KC - 1))
                if bi % 2 == 0:
                    P.op("act", "activation", ["p%d" % bi], ["qT%d" % bi], out=qT[:, bi * 4:bi * 4 + 4, :], in_=pb[bi][:, :].rearrange("p (a b) -> p a b", a=4), func=ACT.Copy)
                else:
                    P.op("dve", "tensor_copy", ["p%d" % bi], ["qT%d" % bi], out=qT[:, bi * 4:bi * 4 + 4, :], in_=pb[bi][:, :].rearrange("p (a b) -> p a b", a=4))
            for g in range(16):
                bank = 4 + g // 4
                P.op("pe", "matmul", ["qT%d" % (g // 4), "const"], ["p%d" % bank], pb[bank][:, (g % 4) * 128:(g % 4 + 1) * 128], lhsT=qT[:, g, :], rhs=skT[:, l, g % 2, :],
                     start=True, stop=True)
            for b4 in range(4):
                bank = 4 + b4
                if b4 % 2 == 0:
                    P.op("act", "activation", ["p%d" % bank], ["s%d" % b4], out=s[:, b4 * 4:b4 * 4 + 4, :], in_=pb[bank][:, :].rearrange("p (a b) -> p a b", a=4), func=ACT.Copy)
                else:
                    P.op("dve", "tensor_copy", ["p%d" % bank], ["s%d" % b4], out=s[:, b4 * 4:b4 * 4 + 4, :], in_=pb[bank][:, :].rearrange("p (a b) -> p a b", a=4))
            snames_all = ["s%d" % b4 for b4 in range(4)]
            for g in range(16):
                P.op("dve", "max", ["s%d" % (g // 4)], ["v16a%d" % g], out=v16[:, g, 0:8], in_=s[:, g, :])
            for g in range(16):
                P.op("dve", "match_replace", ["s%d" % (g // 4), "v16a%d" % g], ["sw%d" % g], out=sw[:, g, :], in_to_replace=v16[:, g, 0:8], in_values=s[:, g, :], imm_value=NEG)
            for g in range(16):
                P.op("dve", "max", ["sw%d" % g], ["v16b%d" % g], out=v16[:, g, 8:16], in_=sw[:, g, :])
            v16n = ["v16a%d" % g for g in range(16)] + ["v16b%d" % g for g in range(16)]
            Ev = sm[:, 536:792].rearrange("p (g k) -> p g k", g=16)
            rZ = sm[:, 520:528]
            E = sw
            swn = ["sw%d" % g for g in range(16)]
            P.op("dve", "tensor_tensor", snames_all + v16n + swn, ["E"], out=E, in0=s, in1=v16[:, :, 0:1].to_broadcast([128, 16, 128]), op=ALU.subtract)
            P.op("act", "activation", ["E"], ["E"], out=E, in_=E, func=ACT.Exp)
            P.op("dve", "tensor_tensor", v16n, ["Ev"], out=Ev, in0=v16, in1=v16[:, :, 0:1].to_broadcast([128, 16, 16]), op=ALU.subtract)
            P.op("act", "activation", ["Ev"], ["Ev"], out=Ev, in_=Ev, func=ACT.Exp)
            E4 = E.rearrange("p (h c) n -> p h c n", c=2)
            Ev4 = Ev.rearrange("p (h c) k -> p h c k", c=2)
            c16n = ["c16a%d" % h for h in range(8)] + ["c16b%d" % h for h in range(8)]
            for rnd in range(2):
                P.op("dve", "tensor_tensor", ["Ev"], ["cand"], out=cand,
                     in0=Ev4[:, :, 0, :].unsqueeze(3).to_broadcast([128, 8, 16, 16]), in1=Ev4[:, :, 1, :].unsqueeze(2).to_broadcast([128, 8, 16, 16]), op=ALU.mult)
                for h in range(8):
                    P.op("dve", "max", ["cand"], ["c16a%d" % h], out=c16[:, h, 0:8], in_=cand[:, h])
                for h in range(8):
                    P.op("dve", "match_replace", ["cand", "c16a%d" % h], ["cw%d" % h], out=cw[:, h], in_to_replace=c16[:, h, 0:8], in_values=cand[:, h], imm_value=-1.0)
                for h in range(8):
                    P.op("dve", "max", ["cw%d" % h], ["c16b%d" % h], out=c16[:, h, 8:16], in_=cw[:, h])
                if rnd == 0:
                    P.op("dve", "tensor_reduce", c16n, ["Z"], out=Z, in_=c16, axis=AX.X, op=ALU.add)
                    P.op("dve", "reciprocal", ["Z"], ["rZ"], out=rZ, in_=Z)
                    P.op("dve", "tensor_tensor", ["E", "rZ"], ["E"], out=E4[:, :, 1, :], in0=E4[:, :, 1, :], in1=rZ.unsqueeze(2).to_broadcast([128, 8, 128]), op=ALU.mult)
                    P.op("dve", "tensor_tensor", ["Ev", "rZ"], ["Ev"], out=Ev4[:, :, 1, :], in0=Ev4[:, :, 1, :], in1=rZ.unsqueeze(2).to_broadcast([128, 8, 16]), op=ALU.mult)
            EEs = [SV(i, 0, 1024).rearrange("p (i j) -> p i j", i=8) for i in (5, 6, 0)]
            gtall = SV(7, 0, 2048).bitcast(BF16)
            Gts = [gtall[:, k * 1024:(k + 1) * 1024] for k in range(4)]
            gk = [0]

            def gbuild_head(n, h):
                k = gk[0] % 3
                k4 = gk[0] % 4
                gk[0] += 1
                EE, Gt = EEs[k], Gts[k4]
                een, gtn = "EE%d" % k, "Gt%d" % k4
                eenl = [een + ".%d" % ii for ii in range(8)]
                i0 = n * 8
                if h % 2 == ACT_PAR:
                    for ii in range(8):
                        P.op("act", "activation", ["E"] + c16n, [eenl[ii]], out=EE[:, ii, :], in_=E4[:, h, 1, :], func=ACT.Copy, scale=E4[:, h, 0, i0 + ii:i0 + ii + 1])
                else:
                    P.op("dve" if h % 4 == 3 else "pool", "tensor_tensor", ["E"] + c16n, eenl, out=EE, in0=E4[:, h, 0, i0:i0 + 8].unsqueeze(2).to_broadcast([128, 8, 128]),
                         in1=E4[:, h, 1, :].unsqueeze(1).to_broadcast([128, 8, 128]), op=ALU.mult)
                P.op("dve", "scalar_tensor_tensor", eenl + c16n, [gtn], out=Gt.rearrange("p (i j) -> p i j", i=8), in0=EE, scalar=c16[:, h, 15:16], in1=EE,
                     op0=ALU.is_ge, op1=ALU.mult)
                pend_acc.append((n, h, gtn, Gt))

            pend_acc = []

            def flush_pe_acc():
                for (n, h, gtn, Gt) in pend_acc:
                    for c in range(2):
                        bank = 2 + 2 * (n % 2) + c
                        P.op("pe", "matmul", [gtn, "identb"], ["p%d" % bank], pb[bank][:, :], lhsT=identb[:], rhs=Gt[:, c * 512:(c + 1) * 512], start=(h == 0), stop=(h == 7))
                del pend_acc[:]

            utv = utb_d[l].rearrange("(kc p) e -> p kc e", p=128)
            chain = [None]
            p1b = pb[1][:, :].bitcast(BF16)

            def tail(prev):
                (ebp, GA, GAT, sn, wnv, vblk, first, last) = prev
                half = p1b[:, (ebp % 2) * 512:(ebp % 2 + 1) * 512]
                for j in range(4):
                    P.op("pe", "transpose", [sn + ".GA", "identb"], ["p1"], out=half[:, j * 128:(j + 1) * 128], in_=GA[:, j * 128:(j + 1) * 128], identity=identb[:])
                P.op("act", "activation", ["p1"], [sn + ".GAT"], out=GAT, in_=half.rearrange("p (a b) -> p a b", a=4), func=ACT.Copy)
                for j in range(4):
                    for db in range(2):
                        P.op("pe", "matmul", [sn + ".GAT", wnv], ["p%d" % (6 + db)], pb[6 + db][:, :], lhsT=GAT[:, j, :], rhs=vblk[:, j, db * 512:(db + 1) * 512],
                             start=(first and j == 0), stop=(last and j == 3))

            for h in range(8):
                gbuild_head(0, h)
                if h % 4 == 3:
                    flush_pe_acc()
            for nb in range(32):
                n = nb // 2
                e0 = nb * 512
                i = wsel[0]
                wsel[0] ^= 1
                wnu = "wbuf%du" % i
                wnv = "wbuf%dv" % i
                wbb = wbuf[i][:].rearrange("p a b -> p (a b)").bitcast(BF16)
                ublk = wbb[:, 0:4096].rearrange("p (kc e) -> p kc e", kc=8)
                vblk = wbb[:, 4096:8192].rearrange("p (j d) -> p j d", j=4)
                extra = ["wbuf%d" % i] if nb < 2 else []
                P.dma(wnu, [], [wnu] + extra, [(ublk, utv[:, :, e0:e0 + 512])])
                P.dma(wnv, [], [wnv] + extra, [(vblk, vb_d[l][e0:e0 + 512, :].rearrange("(j p) d -> p j d", p=128))])
                si = 10 + (nb % 2)
                sn = "slot%d" % si
                Ag = SV(si, 0, 512)
                GA = SV(si, 512, 768).bitcast(BF16)
                GAT = SV(si, 1024, 1280).bitcast(BF16).rearrange("p (a b) -> p a b", a=4)
                for kc in range(KC):
                    P.op("pe", "matmul", [wnu, "hTb"], ["p0"], pb[0][:, :], lhsT=hTb[:, kc, :], rhs=ublk[:, kc, :], start=(kc == 0), stop=(kc == KC - 1))
                flush_pe_acc()
                P.op("act", "activation", ["p0"], [sn + ".Ag"], out=Ag, in_=pb[0][:, :], func=ACT.Gelu)
                gbank = 2 + 2 * (n % 2) + (nb % 2)
                P.op("dve", "tensor_tensor", [sn + ".Ag", "p%d" % gbank], [sn + ".GA"], out=GA, in0=pb[gbank][:, :], in1=Ag, op=ALU.mult)
                if chain[0] is not None:
                    tail(chain[0])
                if n < 15:
                    for h in range(4 * (nb % 2), 4 * (nb % 2) + 4):
                        gbuild_head(n + 1, h)
                chain[0] = (nb, GA, GAT, sn, wnv, vblk, nb == 0, nb == 31)
            tail(chain[0])
            for db in range(2):
                P.op("dve", "tensor_tensor", ["p%d" % (6 + db), "gates"], ["ytmp%d" % db], out=slot[8][:, db * 512:(db + 1) * 512],
                     in0=pb[6 + db][:, :], in1=gates[:, gi, db * 512:(db + 1) * 512], op=ALU.mult)
                P.op("pool", "tensor_tensor", ["ytmp%d" % db, xname], [xname], out=xa[:, db * 512:(db + 1) * 512], in0=xa[:, db * 512:(db + 1) * 512],
                     in1=slot[8][:, db * 512:(db + 1) * 512], op=ALU.add)

        def final_norm(xa, xname, it):
            P.op("act", "activation", [xname], ["junk", "ss0"], out=junk[:], in_=xa, func=ACT.Square, accum_out=ss[:, 0:1])
            P.op("dve", "tensor_scalar", ["ss0"], ["ss1"], out=ss[:, 1:2], in0=ss[:, 0:1], scalar1=1.0 / D, scalar2=EPS, op0=ALU.mult, op1=ALU.add)
            P.op("act", "activation", ["ss1"], ["ss2"], out=ss[:, 2:3], in_=ss[:, 1:2], func=ACT.Sqrt)
            P.op("dve", "reciprocal", ["ss2"], ["ss3"], out=ss[:, 3:4], in_=ss[:, 2:3])
            P.op("dve", "scalar_tensor_tensor", [xname, "ss3", "const"], ["xn"], out=xn[:], in0=xa, scalar=ss[:, 3:4], in1=gfrow[:], op0=ALU.mult, op1=ALU.mult)
            P.dma("yout", ["xn"], [], [(y_d[it * 128:(it + 1) * 128, :], xn[:])])

        stages = os.environ.get("YOCO_STAGES", "gla,peer0,kv,swa,peer1").split(",")
        for it in range(NT):
            xa = xt[it % 2][:]
            xname = "xt%d" % (it % 2)
            P.dma(xname, [], [xname], [(xa, x_d[it * 128:(it + 1) * 128, :])])
            if "gla" in stages:
                gla(xa, xname, it)
                P.barrier()
            if "peer0" in stages:
                peer(xa, xname, 0)
                P.barrier()
            if "kv" in stages:
                kv_phase(xa, xname, it)
                P.barrier()
            if "swa" in stages:
                swa(xa, xname, it)
                P.barrier()
            if "peer1" in stages:
                peer(xa, xname, 1)
                P.barrier()
            final_norm(xa, xname, it)
            P.barrier()
        P.barrier()
        P.emit()
        print("yoco build: ops", P.nops, {e: P.cnt[e] for e in P.ENG}, flush=True)
    return nc


def prep_inputs(inp, NT, nb=8):
    f = np.float32
    S = NT * 128
    t = np.arange(128)
    same = (t[:, None] // 64) == (t[None, :] // 64)
    tri = (same & (t[:, None] <= t[None, :])).astype(f) * f(-1.0 / 16)
    blk = same.astype(f) * f(-1.0 / 16)
    csel = ((t[:, None] // 64) == np.arange(2)[None, :]).astype(f) * f(-1.0 / 16)
    maskT = (same & (t[:, None] <= t[None, :])).astype(f)
    qi = np.arange(128)[:, None]
    mi = np.arange(256)[None, :]
    valid = (mi > qi) & (mi <= qi + 128)
    swam = np.stack([np.where(valid & (mi >= 128), 0.0, NEG), np.where(valid, 0.0, NEG)], axis=1).astype(f)
    invf = (np.float32(500000.0) ** (-(np.arange(0, 16, 2, dtype=np.float32)) / np.float32(16))).astype(f)
    invf = np.ascontiguousarray(np.broadcast_to(invf[None, :], (128, 8)))

    def col(v):
        return np.ascontiguousarray(v.reshape(8, 128).T)

    def row(v):
        return np.ascontiguousarray(np.broadcast_to(v[None, :], (128, v.shape[0])))

    mod_b = inp["mod_b"]
    shared = {
        "invf": invf, "ident": np.eye(128, dtype=f), "tri": tri, "blk": blk, "csel": csel, "maskT": maskT, "swam": swam,
        "modw0": np.ascontiguousarray(inp["mod_w"][0]), "modw1": np.ascontiguousarray(inp["mod_w"][1]),
        "modbcol": np.ascontiguousarray(np.stack([mod_b[l].reshape(48, 128).T for l in range(2)], axis=1)),
        "gaterow": np.ascontiguousarray(np.stack([row(mod_b[0, 2048:3072]), row(mod_b[0, 5120:6144]),
                                                   row(mod_b[1, 2048:3072]), row(mod_b[1, 5120:6144])], axis=1)),
        "kvmodw": np.ascontiguousarray(inp["kv_mod_w"]),
        "kvmodbcol": np.ascontiguousarray(inp["kv_mod_b"].reshape(16, 128).T),
        "ngcol": np.ascontiguousarray(np.stack([col(inp["norm_g"][0, 0]), col(inp["norm_g"][0, 1]), col(inp["norm_g"][1, 0]),
                                                col(inp["norm_g"][1, 1]), col(inp["kv_norm_g"])], axis=1)),
        "gfrow": row(inp["final_norm_g"]),
        "w_in": np.ascontiguousarray(inp["gla_w_in"][0]), "wg2": np.ascontiguousarray(inp["gla_w_g2"][0]),
        "bg2row": row(inp["gla_b_g2"][0]), "gnrow": row(inp["gla_norm_g"][0]), "gwo": np.ascontiguousarray(inp["gla_w_out"][0]),
        "kvw": np.ascontiguousarray(inp["kv_w"]), "swq": np.ascontiguousarray(inp["swa_w_q"][0]),
        "sinkrow": row(inp["swa_sinks"][0]), "swo": np.ascontiguousarray(inp["swa_w_out"][0]),
        "pwq0": np.ascontiguousarray(inp["peer_w_q"][0]), "pwq1": np.ascontiguousarray(inp["peer_w_q"][1]),
        "skT": np.ascontiguousarray(np.transpose(inp["peer_subkeys"], (3, 0, 1, 2))),
        "ut0": np.ascontiguousarray(inp["peer_u"][0].T), "ut1": np.ascontiguousarray(inp["peer_u"][1].T),
        "v0": np.ascontiguousarray(inp["peer_v"][0]), "v1": np.ascontiguousarray(inp["peer_v"][1]),
    }
    maps = []
    for b in range(nb):
        m = dict(shared)
        m["x"] = np.ascontiguousarray(inp["x"][b, :S])
        m["ccol"] = col(inp["c"][b])
        m["pos"] = np.ascontiguousarray(inp["positions"][b, :S].reshape(NT, 128).T.astype(np.int32))
        maps.append(m)
    return maps


_NC_CACHE = {}


def kernel(**inputs):
    inputs = {k: np.asarray(v) for k, v in inputs.items()}
    NT = SEQ // 128
    if NT not in _NC_CACHE:
        _NC_CACHE[NT] = build(NT)
    nc = _NC_CACHE[NT]
    maps = prep_inputs(inputs, NT)
    res = run_bass_kernel_spmd(nc, maps, core_ids=list(range(8)))
    out = np.stack([np.asarray(r["y"]) for r in res.results], axis=0)
    return out.astype(np.float32)
```

```python
import os
from contextlib import ExitStack
import numpy as np
import concourse.bass as bass
import concourse.mybir as mybir
from concourse.bass_utils import run_bass_kernel_spmd

F32 = mybir.dt.float32
BF16 = mybir.dt.bfloat16
I32 = mybir.dt.int32
ACT = mybir.ActivationFunctionType
ALU = mybir.AluOpType
AX = mybir.AxisListType

D = 1024
KC = 8
SEQ = 8192
NEXP = 16384
EPS = 1e-6
NEG = -1e30
ACT_PAR = int(os.environ.get('ACT_PAR', '-1'))
BF16_PROJ = os.environ.get('BF16_PROJ', '1') == '1'
BF16_PQ = os.environ.get('BF16_PQ', '1') == '1'
SWA_CUT = int(os.environ.get('SWA_CUT', '99'))
MODEV = os.environ.get('MODEV', 'dve')
GLA_CUT = int(os.environ.get('GLA_CUT', '99'))
PI = float(np.pi)


class Prog:
    ENG = ("pe", "dve", "act", "pool", "sp")

    def __init__(self, nc, es):
        self.nc = nc
        self.es = es
        self.ops = {e: [] for e in self.ENG}
        self.sem = {e: es.enter_context(nc.semaphore("sem_" + e)) for e in self.ENG}
        self.cnt = {e: 0 for e in self.ENG}
        self.dsem = {}
        self.dcnt = {}
        self.lastw = {}
        self.readers = {}
        self.waited = {e: {} for e in self.ENG}
        self.nops = 0

    def _deps(self, eng, r, w):
        deps = {}

        def add(tok):
            if tok is None:
                return
            s, v = tok
            if eng == "pe" and s.name == self.sem["pe"].name:
                return
            if deps.get(s.name, (None, 0))[1] < v:
                deps[s.name] = (s, v)

        for b in r:
            add(self.lastw.get(b))
        for b in w:
            add(self.lastw.get(b))
            for tok in self.readers.get(b, {}).values():
                add(tok)
        out = []
        for key, (s, v) in deps.items():
            if self.waited[eng].get(key, 0) < v:
                self.waited[eng][key] = v
                out.append((s, v))
        return out

    def _commit(self, tok, r, w):
        for b in w:
            self.lastw[b] = tok
            self.readers[b] = {}
        for b in r:
            self.readers.setdefault(b, {})[tok[0].name] = tok

    def op(self, eng, meth, r, w, *a, **k):
        w = list(w) + [b for b in r if len(b) == 2 and b[0] == "p" and b[1].isdigit()]
        waits = self._deps(eng, r, w)
        self.cnt[eng] += 1
        tok = (self.sem[eng], self.cnt[eng])
        self.ops[eng].append((waits, meth, a, k, self.sem[eng], 1))
        self._commit(tok, r, w)
        self.nops += 1

    def dma(self, key, r, w, pairs, eng="sp"):
        if key not in self.dsem:
            self.dsem[key] = self.es.enter_context(self.nc.semaphore("dsem_" + key))
            self.dcnt[key] = 0
        waits = self._deps(eng, r, w)
        for (o, i) in pairs:
            self.dcnt[key] += 16
            self.ops[eng].append((waits, "dma_start", (), dict(out=o, in_=i), self.dsem[key], 16))
            waits = []
            self.nops += 1
        tok = (self.dsem[key], self.dcnt[key])
        self._commit(tok, r, w)

    def barrier(self):
        allw = [(self.sem[e], self.cnt[e]) for e in self.ENG if self.cnt[e] > 0]
        allw += [(s, self.dcnt[k]) for k, s in self.dsem.items()]
        for e in self.ENG:
            ws = []
            for (s, v) in allw:
                if s.name == self.sem[e].name:
                    continue
                if self.waited[e].get(s.name, 0) < v:
                    self.waited[e][s.name] = v
                    ws.append((s, v))
            if ws:
                self.ops[e].append((ws, None, (), {}, None, 0))
        self.lastw = {}
        self.readers = {}

    def emit(self):
        nc = self.nc
        with nc.Block() as block:
            def run(engname):
                def body(engine):
                    for waits, meth, a, k, sem, inc in self.ops[engname]:
                        for (s, v) in waits:
                            engine.wait_ge(s, v)
                        if meth is None:
                            continue
                        getattr(engine, meth)(*a, **k).then_inc(sem, inc)
                return body
            block.tensor(run("pe"))
            block.vector(run("dve"))
            block.scalar(run("act"))
            block.gpsimd(run("pool"))
            block.sync(run("sp"))


def build(NT, dbg=None):
    nc = bass.Bass("TRN2", target_bir_lowering=False)
    S = NT * 128

    def din(name, shape, dt=F32):
        return nc.dram_tensor(name, list(shape), dt, kind="ExternalInput").ap()

    x_d = din("x", [S, D])
    y_d = nc.dram_tensor("y", [S, D], F32, kind="ExternalOutput").ap()
    ccol_d = din("ccol", [128, 8])
    pos_d = din("pos", [128, NT], I32)
    invf_d = din("invf", [128, 8])
    modw_d = [din("modw0", [D, 6 * D]), din("modw1", [D, 6 * D])]
    modbcol_d = din("modbcol", [128, 2, 48])
    gaterow_d = din("gaterow", [128, 4, D])
    kvmodw_d = din("kvmodw", [D, 2 * D])
    kvmodbcol_d = din("kvmodbcol", [128, 16])
    ngcol_d = din("ngcol", [128, 5, 8])
    gfrow_d = din("gfrow", [128, D])
    w_in_d = din("w_in", [D, 3088])
    wg2_d = din("wg2", [16, 512])
    bg2row_d = din("bg2row", [128, 512])
    gnrow_d = din("gnrow", [128, 256])
    gwo_d = din("gwo", [D, D])
    kvw_d = din("kvw", [D, 512])
    swq_d = din("swq", [D, D])
    sinkrow_d = din("sinkrow", [128, 16])
    swo_d = din("swo", [D, D])
    pwq_d = [din("pwq0", [D, 2048]), din("pwq1", [D, 2048])]
    skT_d = din("skT", [128, 2, 2, 128])
    ut_d = [din("ut0", [D, NEXP]), din("ut1", [D, NEXP])]
    v_d = [din("v0", [NEXP, D]), din("v1", [NEXP, D])]
    ident_d = din("ident", [128, 128])
    tri_d = din("tri", [128, 128])
    blk_d = din("blk", [128, 128])
    csel_d = din("csel", [128, 2])
    maskT_d = din("maskT", [128, 128])
    swam_d = din("swam", [128, 2, 256])
    utb_d = [nc.dram_tensor("utb%d" % l, [D, NEXP], BF16, kind="Internal").ap() for l in range(2)]
    vb_d = [nc.dram_tensor("vb%d" % l, [NEXP, D], BF16, kind="Internal").ap() for l in range(2)]
    wb16 = {nm: nc.dram_tensor("b16_" + nm, [D, w], BF16, kind="Internal").ap()
            for nm, w in (("w_in", 3072), ("gwo", D), ("kvw", 512), ("swq", D), ("swo", D), ("pwq0", 2048), ("pwq1", 2048))}
    wsrc = {"w_in": w_in_d, "gwo": gwo_d, "kvw": kvw_d, "swq": swq_d, "swo": swo_d, "pwq0": pwq_d[0], "pwq1": pwq_d[1]}
    dbg_d = None
    if dbg is not None:
        dbg_d = nc.dram_tensor("dbg", [128, 8192], F32, kind="ExternalOutput").ap()

    es = ExitStack()
    with es:
        P = Prog(nc, es)

        def sb(name, shape, dt=F32):
            return es.enter_context(nc.sbuf_tensor("sb_" + name, list(shape), dt))

        def psum(name):
            return es.enter_context(nc.psum_tensor(name, [128, 512], F32))

        ident = sb("ident", [128, 128])
        identb = sb("identb", [128, 128], BF16)
        tri = sb("tri", [128, 128])
        blk = sb("blk", [128, 128])
        csel = sb("csel", [128, 2])
        maskT = sb("maskT", [128, 128])
        swam = sb("swam", [128, 2, 256])
        gates = sb("gates", [128, 4, D])
        gfrow = sb("gfrow", [128, D])
        gnrow = sb("gnrow", [128, 256])
        bg2row = sb("bg2row", [128, 512])
        sinkrow = sb("sinkrow", [128, 16])
        wg2 = sb("wg2", [16, 512])
        skT = sb("skT", [128, 2, 2, 128])
        modc = sb("modc", [128, 10, 8])
        ngcol = sb("ngcol", [128, 5, 8])
        modbcol = sb("modbcol", [128, 2, 48])
        kvmodbcol = sb("kvmodbcol", [128, 16])
        ccol = sb("ccol", [128, 8])
        cact = sb("cact", [128, 8])
        cbc = sb("cbc", [128, 8, 128])
        cosT = sb("cosT", [128, NT, 8])
        sinT = sb("sinT", [128, NT, 8])
        posi = sb("posi", [128, NT], I32)
        invf = sb("invf", [128, 8])
        xt = [sb("xt0", [128, D]), sb("xt1", [128, D])]
        xn = sb("xn", [128, D])
        hT = sb("hT", [128, KC, 128])
        hTb = sb("hTb", [128, KC, 128], BF16)
        junk = sb("junk", [128, D], BF16)
        ss = sb("ss", [128, 8])
        sm = sb("sm", [128, 1024])
        wbuf = [sb("wbuf0", [128, KC, 512]), sb("wbuf1", [128, KC, 512])]
        NSLOT = 12
        slot = [sb("slot%d" % i, [128, 2048]) for i in range(NSLOT)]
        Sst = [sb("Sa", [128, 4, 256]), sb("Sb", [128, 4, 256])]
        kTd = [sb("kTd0", [128, 4, 128]), sb("kTd1", [128, 4, 128])]
        vbd = [sb("vbd0", [128, 256]), sb("vbd1", [128, 256])]
        pb = [psum("p%d" % i) for i in range(8)]

        def SV(i, lo, hi):
            return slot[i][:, lo:hi]

        consts = [(ident, ident_d), (tri, tri_d), (blk, blk_d), (csel, csel_d), (maskT, maskT_d), (swam, swam_d),
                  (gates, gaterow_d), (gfrow, gfrow_d), (gnrow, gnrow_d), (bg2row, bg2row_d), (sinkrow, sinkrow_d),
                  (wg2, wg2_d), (skT, skT_d), (ngcol, ngcol_d), (modbcol, modbcol_d), (kvmodbcol, kvmodbcol_d),
                  (ccol, ccol_d), (posi, pos_d), (invf, invf_d)]
        P.dma("const", [], ["const"], [(t[:], d) for (t, d) in consts])
        P.op("dve", "memset", [], ["Sa"], Sst[0][:], 0.0)
        P.op("dve", "memset", [], ["k1"], kTd[1][:], 0.0)
        P.op("dve", "memset", [], ["v1"], vbd[1][:], 0.0)
        P.op("dve", "tensor_copy", ["const"], ["identb"], out=identb[:], in_=ident[:])
        P.op("act", "activation", ["const"], ["cact"], out=cact[:], in_=ccol[:], func=ACT.Silu)
        P.op("dve", "tensor_copy", ["cact"], ["cbc"], out=cbc[:], in_=cact[:].unsqueeze(2).to_broadcast([128, 8, 128]))
        posf = sm[:, 0:NT]
        ang = slot[0][:, 0:NT * 8].rearrange("p (a b) -> p a b", b=8)
        ang2 = slot[0][:, 1024:1024 + NT * 8].rearrange("p (a b) -> p a b", b=8)
        kf = slot[1][:, 0:NT * 8].rearrange("p (a b) -> p a b", b=8)
        ki = slot[2][:, 0:NT * 8].bitcast(I32).rearrange("p (a b) -> p a b", b=8)
        P.op("dve", "tensor_copy", ["const"], ["posf"], out=posf, in_=posi[:])
        P.op("dve", "tensor_tensor", ["posf", "const"], ["ang"], out=ang, in0=posf.unsqueeze(2).to_broadcast([128, NT, 8]),
             in1=invf[:].unsqueeze(1).to_broadcast([128, NT, 8]), op=ALU.mult)
        P.op("dve", "tensor_scalar_add", ["ang"], ["ang2"], out=ang2, in0=ang, scalar1=PI / 2)
        for (src, nm, dst) in ((ang, "ang", sinT), (ang2, "ang2", cosT)):
            P.op("dve", "tensor_scalar", [nm], ["ki"], out=ki, in0=src, scalar1=float(1.0 / (2 * PI)), scalar2=None, op0=ALU.mult)
            P.op("dve", "tensor_copy", ["ki"], ["kf"], out=kf, in_=ki)
            P.op("dve", "scalar_tensor_tensor", ["kf", nm], [nm], out=src, in0=kf, scalar=float(-2 * PI), in1=src, op0=ALU.mult, op1=ALU.add)
            P.op("dve", "tensor_single_scalar", [nm], ["kf"], out=kf, in_=src, scalar=PI, op=ALU.is_gt)
            P.op("dve", "scalar_tensor_tensor", ["kf", nm], [nm], out=src, in0=kf, scalar=float(-2 * PI), in1=src, op0=ALU.mult, op1=ALU.add)
            P.op("dve", "tensor_single_scalar", [nm], ["kf"], out=kf, in_=src, scalar=-PI, op=ALU.is_lt)
            P.op("dve", "scalar_tensor_tensor", ["kf", nm], [nm], out=src, in0=kf, scalar=float(2 * PI), in1=src, op0=ALU.mult, op1=ALU.add)
            P.op("act", "activation", [nm], [nm + "_out"], out=dst[:], in_=src, func=ACT.Sin)

        wsel = [0]

        def wload(dram_w, c0, ncols):
            i = wsel[0]
            wsel[0] ^= 1
            nm = "wbuf%d" % i
            P.dma(nm, [], [nm], [(wbuf[i][:, :, 0:ncols], dram_w[:, c0:c0 + ncols].rearrange("(kc p) n -> p kc n", p=128))])
            return nm, wbuf[i]

        mcol = sm[:, 64:64 + 96].rearrange("p (a b) -> p a b", b=8)
        for l in range(2):
            for bi in range(12):
                nm, wb = wload(modw_d[l], bi * 512, 512)
                kind = bi // 2
                if kind in (2, 5):
                    gi = l * 2 + (0 if kind == 2 else 1)
                    half = bi % 2
                    for kc in range(KC):
                        P.op("pe", "matmul", [nm, "cbc"], ["p0"], pb[0][:, :], lhsT=cbc[:, kc, :], rhs=wb[:, kc, :], start=(kc == 0), stop=(kc == KC - 1))
                    P.op("dve", "tensor_tensor", ["p0", "const"], ["gates"], out=gates[:, gi, half * 512:(half + 1) * 512], in0=pb[0][:, :],
                         in1=gates[:, gi, half * 512:(half + 1) * 512], op=ALU.add)
                else:
                    for j in range(4):
                        for kc in range(KC):
                            P.op("pe", "matmul", [nm, "cact"], ["p1"], pb[1][:, j:j + 1], lhsT=wb[:, kc, j * 128:(j + 1) * 128], rhs=cact[:, kc:kc + 1],
                                 start=(kc == 0), stop=(kc == KC - 1))
                    vi = {0: 0, 1: 1, 3: 2, 4: 3}[kind]
                    P.op("dve", "tensor_tensor", ["p1", "const"], ["mcol"], out=mcol[:, l * 4 + vi, (bi % 2) * 4:(bi % 2) * 4 + 4], in0=pb[1][:, 0:4],
                         in1=modbcol[:, l, bi * 4:bi * 4 + 4], op=ALU.add)
        for bi in range(4):
            nm, wb = wload(kvmodw_d, bi * 512, 512)
            for j in range(4):
                for kc in range(KC):
                    P.op("pe", "matmul", [nm, "cact"], ["p1"], pb[1][:, j:j + 1], lhsT=wb[:, kc, j * 128:(j + 1) * 128], rhs=cact[:, kc:kc + 1],
                         start=(kc == 0), stop=(kc == KC - 1))
            P.op("dve", "tensor_tensor", ["p1", "const"], ["mcol"], out=mcol[:, 8 + bi // 2, (bi % 2) * 4:(bi % 2) * 4 + 4], in0=pb[1][:, 0:4],
                 in1=kvmodbcol[:, bi * 4:bi * 4 + 4], op=ALU.add)
        for (mi, gi, shi, sci) in ((0, 0, 0, 1), (2, 1, 2, 3), (4, 4, 8, 9), (6, 2, 4, 5), (8, 3, 6, 7)):
            P.op("dve", "scalar_tensor_tensor", ["mcol", "const"], ["modc"], out=modc[:, mi, :], in0=mcol[:, sci, :], scalar=1.0, in1=ngcol[:, gi, :],
                 op0=ALU.add, op1=ALU.mult)
            P.op("dve", "tensor_copy", ["mcol"], ["modc"], out=modc[:, mi + 1, :], in_=mcol[:, shi, :])
        P.barrier()

        cv = [0]

        def convert(src_ap, dst_ap, W=4096):
            i = cv[0] % 3
            cv[0] += 1
            H = W // 2
            P.dma("cin%d" % i, [], ["cin%d" % i], [(slot[2 * i][:, 0:H], src_ap[:, 0:H]), (slot[2 * i + 1][:, 0:H], src_ap[:, H:W])])
            ob = slot[6 + i][:, :].bitcast(BF16)
            eng = ("dve", "act", "pool")[i]
            if eng == "act":
                P.op("act", "activation", ["cin%d" % i], ["cob%d" % i], out=ob[:, 0:H], in_=slot[2 * i][:, 0:H], func=ACT.Copy)
                P.op("act", "activation", ["cin%d" % i], ["cob%d" % i], out=ob[:, H:W], in_=slot[2 * i + 1][:, 0:H], func=ACT.Copy)
            else:
                P.op(eng, "tensor_copy", ["cin%d" % i], ["cob%d" % i], out=ob[:, 0:H], in_=slot[2 * i][:, 0:H])
                P.op(eng, "tensor_copy", ["cin%d" % i], ["cob%d" % i], out=ob[:, H:W], in_=slot[2 * i + 1][:, 0:H])
            P.dma("cout%d" % i, ["cob%d" % i], [], [(dst_ap, ob[:, 0:W])])

        if BF16_PROJ:
            for nm in ("w_in", "gwo", "kvw", "swq", "swo") + (("pwq0", "pwq1") if BF16_PQ else ()):
                W = wb16[nm].shape[1]
                for kc in range(KC):
                    convert(wsrc[nm][kc * 128:(kc + 1) * 128, 0:W], wb16[nm][kc * 128:(kc + 1) * 128, :], W=W)

        for l in range(2):
            for kc in range(KC):
                for e4 in range(4):
                    convert(ut_d[l][kc * 128:(kc + 1) * 128, e4 * 4096:(e4 + 1) * 4096],
                            utb_d[l][kc * 128:(kc + 1) * 128, e4 * 4096:(e4 + 1) * 4096])
            for r in range(32):
                convert(v_d[l][r * 512:(r + 1) * 512, :].rearrange("(p j) d -> p (j d)", j=4),
                        vb_d[l][r * 512:(r + 1) * 512, :].rearrange("(p j) d -> p (j d)", j=4))
        P.barrier()

        def modulate(xa, xname, mi, bf16=False):
            P.op("act", "activation", [xname], ["junk", "ss0"], out=junk[:], in_=xa, func=ACT.Square, accum_out=ss[:, 0:1])
            P.op("dve", "tensor_scalar", ["ss0"], ["ss1"], out=ss[:, 1:2], in0=ss[:, 0:1], scalar1=1.0 / D, scalar2=EPS, op0=ALU.mult, op1=ALU.add)
            P.op("act", "activation", ["ss1"], ["ss2"], out=ss[:, 2:3], in_=ss[:, 1:2], func=ACT.Sqrt)
            P.op("dve", "reciprocal", ["ss2"], ["ss3"], out=ss[:, 3:4], in_=ss[:, 2:3])
            P.op("dve", "tensor_scalar", [xname, "ss3"], ["xn"], out=xn[:], in0=xa, scalar1=ss[:, 3:4], scalar2=None, op0=ALU.mult)
            for kc in range(KC):
                bnk = kc // 4
                P.op("pe", "transpose", ["xn", "const"], ["p%d" % bnk], out=pb[bnk][:, (kc % 4) * 128:(kc % 4 + 1) * 128],
                     in_=xn[:, kc * 128:(kc + 1) * 128], identity=ident[:])
            for kc in range(KC):
                bnk = kc // 4
                src = pb[bnk][:, (kc % 4) * 128:(kc % 4 + 1) * 128]
                if MODEV == "none":
                    continue
                if (kc % 2 == 0 and MODEV == "mix") or MODEV == "dve":
                    P.op("dve", "tensor_scalar", ["p%d" % bnk, "modc"], ["hT%d" % kc], out=hT[:, kc, :], in0=src,
                         scalar1=modc[:, mi, kc:kc + 1], scalar2=modc[:, mi + 1, kc:kc + 1], op0=ALU.mult, op1=ALU.add)
                else:
                    P.op("act", "activation", ["p%d" % bnk, "modc"], ["hT%d" % kc], out=hT[:, kc, :], in_=src, func=ACT.Identity,
                         scale=modc[:, mi, kc:kc + 1], bias=modc[:, mi + 1, kc:kc + 1])
            if bf16:
                P.op("pool", "tensor_copy", ["hT%d" % kc for kc in range(KC)], ["hTb"], out=hTb[:], in_=hT[:])
            return ["hT%d" % kc for kc in range(KC)]

        def wload_b(wname, c0, ncols):
            i = wsel[0]
            wsel[0] ^= 1
            nm = "wbuf%d" % i
            wv = wbuf[i][:].rearrange("p a b -> p (a b)").bitcast(BF16)[:, 0:4096].rearrange("p (kc n) -> p kc n", kc=8)
            P.dma(nm, [], [nm], [(wv[:, :, 0:ncols], wb16[wname][:, c0:c0 + ncols].rearrange("(kc p) n -> p kc n", p=128))])
            return nm, wv

        def dense_block(hTn, dram_w, c0, ncols, bank, bname=None):
            if BF16_PROJ and bname is not None:
                nm, wb = wload_b(bname, c0, ncols)
                for kc in range(KC):
                    P.op("pe", "matmul", [nm, "hTb"], ["p%d" % bank], pb[bank][:, 0:ncols], lhsT=hTb[:, kc, :], rhs=wb[:, kc, 0:ncols],
                         start=(kc == 0), stop=(kc == KC - 1))
                return
            nm, wb = wload(dram_w, c0, ncols)
            for kc in range(KC):
                P.op("pe", "matmul", [nm, "hT%d" % kc], ["p%d" % bank], pb[bank][:, 0:ncols], lhsT=hT[:, kc, :], rhs=wb[:, kc, 0:ncols],
                     start=(kc == 0), stop=(kc == KC - 1))

        def out_proj(onm, oap, dram_w, gi, xa, xname, bname=None):
            useb = BF16_PROJ and bname is not None
            if useb:
                oT = SV(5, 1024, 1536).bitcast(BF16).rearrange("p (a b) -> p a b", a=8)
            else:
                oT = SV(5, 1024, 2048).rearrange("p (a b) -> p a b", a=8)
            for kc in range(KC):
                bnk = kc // 4
                P.op("pe", "transpose", [onm, "const"], ["p%d" % bnk], out=pb[bnk][:, (kc % 4) * 128:(kc % 4 + 1) * 128],
                     in_=oap[:, kc * 128:(kc + 1) * 128], identity=ident[:])
            P.op("act", "activation", ["p0"], ["oT0"], out=oT[:, 0:4, :], in_=pb[0][:, :].rearrange("p (a b) -> p a b", a=4), func=ACT.Copy)
            P.op("dve", "tensor_copy", ["p1"], ["oT1"], out=oT[:, 4:8, :], in_=pb[1][:, :].rearrange("p (a b) -> p a b", a=4))
            for half in range(2):
                if useb:
                    nm, wb = wload_b(bname, half * 512, 512)
                else:
                    nm, wb = wload(dram_w, half * 512, 512)
                bank = 2 + half
                for kc in range(KC):
                    P.op("pe", "matmul", [nm, "oT%d" % (kc // 4)], ["p%d" % bank], pb[bank][:, :], lhsT=oT[:, kc, :], rhs=wb[:, kc, 0:512],
                         start=(kc == 0), stop=(kc == KC - 1))
                P.op("dve", "tensor_tensor", ["p%d" % bank, "gates"], ["ytmp%d" % half], out=sm[:, half * 512:(half + 1) * 512], in0=pb[bank][:, :],
                     in1=gates[:, gi, half * 512:(half + 1) * 512], op=ALU.mult)
                P.op("pool", "tensor_tensor", ["ytmp%d" % half, xname], [xname], out=xa[:, half * 512:(half + 1) * 512],
                     in0=xa[:, half * 512:(half + 1) * 512], in1=sm[:, half * 512:(half + 1) * 512], op=ALU.add)

        def gla(xa, xname, it):
            hTn = modulate(xa, xname, 0, bf16=BF16_PROJ)
            if GLA_CUT <= 0:
                return
            qk = SV(0, 0, 1024)
            la = SV(0, 1024, 1536)
            zb = SV(0, 1536, 2048)
            vv = SV(1, 0, 1024)
            og = SV(1, 1024, 2048)
            eb = SV(2, 0, 512)
            enb = SV(2, 512, 1024)
            ebl = SV(2, 1024, 1536)
            scm = SV(2, 1536, 2048).rearrange("p (a b) -> p a b", a=4)
            qt = SV(3, 0, 512)
            kt = SV(3, 512, 1024)
            kdec = SV(3, 1024, 1536)
            glT = slot[3][0:16, 1536:1664]
            dec = SV(3, 1664, 1672)
            qtT0 = SV(4, 0, 512).rearrange("p (a b) -> p a b", a=4)
            qtT1 = SV(4, 512, 1024).rearrange("p (a b) -> p a b", a=4)
            ktT = SV(4, 1024, 1536).rearrange("p (a b) -> p a b", a=4)
            qtT = SV(4, 1536, 2048).rearrange("p (a b) -> p a b", a=4)
            osb = SV(5, 0, 1024)
            dsts = [(qk[:, 0:512], "q"), (qk[:, 512:1024], "k"), (vv[:, 0:512], "v0"), (vv[:, 512:1024], "v1"),
                    (og[:, 0:512], "og0"), (og[:, 512:1024], "og1")]
            for bi, (dst, dn) in enumerate(dsts):
                bank = 2 + (bi % 2)
                dense_block(hTn, w_in_d, bi * 512, 512, bank, bname="w_in")
                if bi % 2 == 0:
                    P.op("act", "activation", ["p%d" % bank], [dn], out=dst, in_=pb[bank][:, :], func=ACT.Copy)
                else:
                    P.op("dve", "tensor_copy", ["p%d" % bank], [dn], out=dst, in_=pb[bank][:, :])
            if GLA_CUT <= 1:
                return
            nm, wb = wload(w_in_d, 3072, 16)
            for kc in range(KC):
                P.op("pe", "matmul", [nm, "hT%d" % kc], ["p4"], pb[4][0:16, 0:128], lhsT=wb[:, kc, 0:16], rhs=hT[:, kc, :], start=(kc == 0), stop=(kc == KC - 1))
            P.op("dve", "tensor_copy", ["p4"], ["glT"], out=glT, in_=pb[4][0:16, 0:128])
            P.op("pe", "matmul", ["glT", "const"], ["p5"], pb[5][:, :], lhsT=glT, rhs=wg2[:, :], start=True, stop=True)
            P.op("dve", "tensor_tensor", ["p5", "const"], ["zb"], out=zb, in0=pb[5][:, :], in1=bg2row[:], op=ALU.add)
            P.op("act", "activation", ["zb"], ["zb"], out=zb, in_=zb, func=ACT.Exp, scale=-1.0)
            P.op("act", "activation", ["zb"], ["la"], out=la, in_=zb, func=ACT.Ln, bias=1.0)
            if GLA_CUT <= 2:
                return
            P.op("pe", "matmul", ["la", "const"], ["p4"], pb[4][:, :], lhsT=tri[:], rhs=la, start=True, stop=True)
            P.op("pe", "matmul", ["la", "const"], ["p5"], pb[5][:, :], lhsT=blk[:], rhs=la, start=True, stop=True)
            for h in range(4):
                P.op("pe", "matmul", ["la", "const"], ["p6"], pb[6][:, 2 * h:2 * h + 2], lhsT=la[:, h * 128:(h + 1) * 128], rhs=csel[:], start=True, stop=True)
            P.op("act", "activation", ["p4"], ["eb"], out=eb, in_=pb[4][:, :], func=ACT.Exp)
            P.op("act", "activation", ["p4"], ["enb"], out=enb, in_=pb[4][:, :], func=ACT.Exp, scale=-1.0)
            P.op("act", "activation", ["p5"], ["ebl"], out=ebl, in_=pb[5][:, :], func=ACT.Exp)
            P.op("act", "activation", ["p6"], ["dec"], out=dec, in_=pb[6][:, 0:8], func=ACT.Exp)
            P.op("dve", "scalar_tensor_tensor", ["q", "eb"], ["qt"], out=qt, in0=qk[:, 0:512], scalar=float(128 ** -0.5), in1=eb, op0=ALU.mult, op1=ALU.mult)
            P.op("dve", "tensor_tensor", ["k", "enb"], ["kt"], out=kt, in0=qk[:, 512:1024], in1=enb, op=ALU.mult)
            P.op("pool", "tensor_tensor", ["kt", "ebl"], ["kdec"], out=kdec, in0=kt, in1=ebl, op=ALU.mult)
            if GLA_CUT <= 3:
                return
            for h in range(4):
                P.op("pe", "transpose", ["qt", "const"], ["p0"], out=pb[0][:, h * 128:(h + 1) * 128], in_=qt[:, h * 128:(h + 1) * 128], identity=ident[:])
            for h in range(4):
                P.op("pe", "transpose", ["kt", "const"], ["p1"], out=pb[1][:, h * 128:(h + 1) * 128], in_=kt[:, h * 128:(h + 1) * 128], identity=ident[:])
            p0v = pb[0][:, :].rearrange("p (a b) -> p a b", a=4)
            P.op("act", "activation", ["p0"], ["qtT"], out=qtT, in_=p0v, func=ACT.Copy)
            P.op("pool", "memset", [], ["qtT0", "qtT1"], slot[4][:, 0:1024], 0.0)
            P.op("dve", "tensor_copy", ["p0"], ["qtT0"], out=qtT0[:, :, 0:64], in_=p0v[:, :, 0:64])
            P.op("dve", "tensor_copy", ["p0"], ["qtT1"], out=qtT1[:, :, 64:128], in_=p0v[:, :, 64:128])
            P.op("act", "activation", ["p1"], ["ktT"], out=ktT, in_=pb[1][:, :].rearrange("p (a b) -> p a b", a=4), func=ACT.Copy)
            if GLA_CUT <= 4:
                return
            for h in range(4):
                P.op("pe", "matmul", ["ktT", "qtT"], ["p4"], pb[4][:, h * 128:(h + 1) * 128], lhsT=ktT[:, h, :], rhs=qtT[:, h, :], start=True, stop=True)
            P.op("dve", "tensor_tensor", ["p4", "const"], ["scm"], out=scm, in0=pb[4][:, :].rearrange("p (a b) -> p a b", a=4),
                 in1=maskT[:].unsqueeze(1).to_broadcast([128, 4, 128]), op=ALU.mult)
            if GLA_CUT <= 5:
                return
            Sa, Sb = Sst[0], Sst[1]
            for h in range(4):
                vh = vv[:, h * 256:(h + 1) * 256]
                vn = "v%d" % (h // 2)
                kvb = 5 + (h % 2)
                ob = 2 + (h // 2)
                oreg = pb[ob][:, (h % 2) * 256:(h % 2 + 1) * 256]
                P.op("pe", "matmul", ["kdec", vn], ["p%d" % kvb], pb[kvb][:, 0:256], lhsT=kdec[0:64, h * 128:(h + 1) * 128], rhs=vv[0:64, h * 256:(h + 1) * 256],
                     start=True, stop=True)
                P.op("dve", "scalar_tensor_tensor", ["Sa", "dec", "p%d" % kvb], ["Sb"], out=Sb[:, h, :], in0=Sa[:, h, :], scalar=dec[:, 2 * h:2 * h + 1],
                     in1=pb[kvb][:, 0:256], op0=ALU.mult, op1=ALU.add)
                P.op("pe", "matmul", ["scm", vn], ["p%d" % ob], oreg, lhsT=scm[:, h, :], rhs=vh, start=True, stop=False)
                P.op("pe", "matmul", ["qtT0", "Sa"], ["p%d" % ob], oreg, lhsT=qtT0[:, h, :], rhs=Sa[:, h, :], start=False, stop=False)
                P.op("pe", "matmul", ["qtT1", "Sb"], ["p%d" % ob], oreg, lhsT=qtT1[:, h, :], rhs=Sb[:, h, :], start=False, stop=True)
                P.op("pe", "matmul", ["kdec", vn], ["p%d" % kvb], pb[kvb][:, 256:512], lhsT=kdec[64:128, h * 128:(h + 1) * 128], rhs=vv[64:128, h * 256:(h + 1) * 256],
                     start=True, stop=True)
                P.op("dve", "scalar_tensor_tensor", ["Sb", "dec", "p%d" % kvb], ["Sa"], out=Sa[:, h, :], in0=Sb[:, h, :], scalar=dec[:, 2 * h + 1:2 * h + 2],
                     in1=pb[kvb][:, 256:512], op0=ALU.mult, op1=ALU.add)
            if GLA_CUT <= 6:
                return
            for h in range(4):
                ob = 2 + (h // 2)
                oreg = pb[ob][:, (h % 2) * 256:(h % 2 + 1) * 256]
                P.op("act", "activation", ["p%d" % ob], ["junk", "oss%d" % h], out=junk[:, 0:256], in_=oreg, func=ACT.Square, accum_out=ss[:, 4 + h:5 + h])
            P.op("dve", "tensor_scalar", ["oss%d" % h for h in range(4)], ["orv"], out=sm[:, 0:4], in0=ss[:, 4:8], scalar1=1.0 / 256, scalar2=EPS, op0=ALU.mult, op1=ALU.add)
            P.op("act", "activation", ["orv"], ["ors"], out=sm[:, 4:8], in_=sm[:, 0:4], func=ACT.Sqrt)
            P.op("dve", "reciprocal", ["ors"], ["orr"], out=sm[:, 8:12], in_=sm[:, 4:8])
            for h in range(4):
                ob = 2 + (h // 2)
                oreg = pb[ob][:, (h % 2) * 256:(h % 2 + 1) * 256]
                P.op("dve", "scalar_tensor_tensor", ["p%d" % ob, "orr", "const"], ["osb"], out=osb[:, h * 256:(h + 1) * 256], in0=oreg, scalar=sm[:, 8 + h:9 + h],
                     in1=gnrow[:], op0=ALU.mult, op1=ALU.mult)
            P.op("act", "activation", ["og0", "og1"], ["ogs"], out=og, in_=og, func=ACT.Silu)
            P.op("dve", "tensor_tensor", ["osb", "ogs"], ["osb"], out=osb, in0=osb, in1=og, op=ALU.mult)
            if GLA_CUT <= 7:
                return
            out_proj("osb", osb, gwo_d, 0, xa, xname, bname="gwo")

        def kv_phase(xa, xname, it):
            cur = it % 2
            hTn = modulate(xa, xname, 4, bf16=BF16_PROJ)
            dense_block(hTn, kvw_d, 0, 512, 2, bname="kvw")
            kdup = SV(6, 0, 512).rearrange("p (g c d) -> p g c d", g=4, c=2)
            tmp = SV(6, 512, 768).rearrange("p (a g d) -> p a g d", a=8, g=4)
            kp = pb[2][:, 0:256].rearrange("p (g d) -> p g d", g=4)
            P.op("act", "activation", ["p2"], ["v%d" % cur], out=vbd[cur][:], in_=pb[2][:, 256:512], func=ACT.Copy)
            P.op("dve", "tensor_copy", ["p2"], ["kdup"], out=kdup[:, :, 0, :], in_=kp)
            cb = cosT[:, it, :].unsqueeze(1).to_broadcast([128, 4, 8])
            sbb = sinT[:, it, :].unsqueeze(1).to_broadcast([128, 4, 8])
            x1 = kdup[:, :, 0, 0:8]
            x2 = kdup[:, :, 0, 8:16]
            P.op("dve", "tensor_tensor", ["kdup"], ["t0"], out=tmp[:, 0], in0=x1, in1=cb, op=ALU.mult)
            P.op("dve", "tensor_tensor", ["kdup"], ["t1"], out=tmp[:, 1], in0=x2, in1=sbb, op=ALU.mult)
            P.op("dve", "tensor_tensor", ["kdup"], ["t2"], out=tmp[:, 2], in0=x2, in1=cb, op=ALU.mult)
            P.op("dve", "tensor_tensor", ["kdup"], ["t3"], out=tmp[:, 3], in0=x1, in1=sbb, op=ALU.mult)
            P.op("dve", "tensor_tensor", ["t0", "t1"], ["kdup"], out=x1, in0=tmp[:, 0], in1=tmp[:, 1], op=ALU.subtract)
            P.op("dve", "tensor_tensor", ["t2", "t3"], ["kdup"], out=x2, in0=tmp[:, 2], in1=tmp[:, 3], op=ALU.add)
            P.op("dve", "tensor_copy", ["kdup"], ["kdup"], out=kdup[:, :, 1, :], in_=kdup[:, :, 0, :])
            kflat = SV(6, 0, 512)
            for g in range(4):
                P.op("pe", "transpose", ["kdup", "const"], ["p0"], out=pb[0][:, g * 128:(g + 1) * 128], in_=kflat[:, g * 128:(g + 1) * 128], identity=ident[:])
            P.op("act", "activation", ["p0"], ["k%d" % cur], out=kTd[cur][:], in_=pb[0][:, :].rearrange("p (a b) -> p a b", a=4), func=ACT.Copy)

        def swa(xa, xname, it):
            cur = it % 2
            prv = 1 - cur
            hTn = modulate(xa, xname, 6, bf16=BF16_PROJ)
            q = SV(0, 0, 1024)
            q3 = q.rearrange("p (h d) -> p h d", h=16)
            qT = SV(0, 1024, 2048).rearrange("p (a b) -> p a b", a=8)
            sc = [SV(1, 0, 2048).rearrange("p (h m) -> p h m", h=8), SV(2, 0, 2048).rearrange("p (h m) -> p h m", h=8)]
            pT = [SV(3, 0, 2048).rearrange("p (h c m) -> p h c m", h=8, c=2), SV(4, 0, 2048).rearrange("p (h c m) -> p h c m", h=8, c=2)]
            o = SV(5, 0, 1024)
            tmp = SV(6, 1024, 2048).rearrange("p (a h d) -> p a h d", a=4, h=16)
            for half in range(2):
                dense_block(hTn, swq_d, half * 512, 512, 2 + half, bname="swq")
                if half == 0:
                    P.op("act", "activation", ["p2"], ["q"], out=q[:, 0:512], in_=pb[2][:, :], func=ACT.Copy)
                else:
                    P.op("dve", "tensor_copy", ["p3"], ["q"], out=q[:, 512:1024], in_=pb[3][:, :])
            if SWA_CUT <= 1:
                return
            cb = cosT[:, it, :].unsqueeze(1).to_broadcast([128, 16, 8])
            sbb = sinT[:, it, :].unsqueeze(1).to_broadcast([128, 16, 8])
            x1 = q3[:, :, 0:8]
            x2 = q3[:, :, 8:16]
            tv = SV(6, 1024, 1536).rearrange("p (a h d) -> p a h d", a=4, h=16)
            P.op("dve", "tensor_tensor", ["q"], ["t0"], out=tv[:, 0], in0=x1, in1=cb, op=ALU.mult)
            P.op("dve", "tensor_tensor", ["q"], ["t1"], out=tv[:, 1], in0=x2, in1=sbb, op=ALU.mult)
            P.op("dve", "tensor_tensor", ["q"], ["t2"], out=tv[:, 2], in0=x2, in1=cb, op=ALU.mult)
            P.op("dve", "tensor_tensor", ["q"], ["t3"], out=tv[:, 3], in0=x1, in1=sbb, op=ALU.mult)
            P.op("dve", "tensor_tensor", ["t0", "t1"], ["q"], out=x1, in0=tv[:, 0], in1=tv[:, 1], op=ALU.subtract)
            P.op("dve", "tensor_tensor", ["t2", "t3"], ["q"], out=x2, in0=tv[:, 2], in1=tv[:, 3], op=ALU.add)
            if SWA_CUT <= 2:
                return
            for j in range(8):
                bnk = j // 4
                P.op("pe", "transpose", ["q", "const"], ["p%d" % bnk], out=pb[bnk][:, (j % 4) * 128:(j % 4 + 1) * 128], in_=q[:, j * 128:(j + 1) * 128], identity=ident[:])
            P.op("act", "activation", ["p0"], ["qT0"], out=qT[:, 0:4, :], in_=pb[0][:, :].rearrange("p (a b) -> p a b", a=4), func=ACT.Copy)
            P.op("dve", "tensor_copy", ["p1"], ["qT1"], out=qT[:, 4:8, :], in_=pb[1][:, :].rearrange("p (a b) -> p a b", a=4))
            if SWA_CUT <= 3:
                return
            mk = swam[:, 0 if it == 0 else 1, :]
            for grp in range(4):
                bankE = 4 + 2 * (grp % 2)
                bankO = bankE + 1
                for pj in range(2):
                    j = 2 * grp + pj
                    for hh in range(2):
                        h = 2 * j + hh
                        g = h // 4
                        base = 64 * hh
                        bank = bankE if hh == 0 else bankO
                        col = pj * 256
                        P.op("pe", "matmul", ["qT%d" % (j // 4), "k%d" % prv], ["p%d" % bank], pb[bank][:, col:col + 128],
                             lhsT=qT[base:base + 64, j, :], rhs=kTd[prv][base:base + 64, g, :], start=True, stop=True)
                        P.op("pe", "matmul", ["qT%d" % (j // 4), "k%d" % cur], ["p%d" % bank], pb[bank][:, col + 128:col + 256],
                             lhsT=qT[base:base + 64, j, :], rhs=kTd[cur][base:base + 64, g, :], start=True, stop=True)
                for hh in range(2):
                    bank = bankE if hh == 0 else bankO
                    for pj in range(2):
                        j = 2 * grp + pj
                        h = 2 * j + hh
                        P.op("dve", "scalar_tensor_tensor", ["p%d" % bank, "const"], ["sc%d" % j], out=sc[h // 8][:, h % 8, :],
                             in0=pb[bank][:, pj * 256:(pj + 1) * 256], scalar=0.125, in1=mk, op0=ALU.mult, op1=ALU.add)
            if SWA_CUT <= 4:
                return
            rmax = sm[:, 0:16]
            mm = sm[:, 16:32]
            negm = sm[:, 32:48]
            rs = sm[:, 48:64]
            sk = sm[:, 64:80]
            den = sm[:, 80:96]
            rden = sm[:, 96:112]
            for half in range(2):
                P.op("dve", "tensor_reduce", ["sc%d" % j for j in range(half * 4, half * 4 + 4)], ["rmax%d" % half], out=rmax[:, half * 8:(half + 1) * 8],
                     in_=sc[half], axis=AX.X, op=ALU.max)
            P.op("dve", "tensor_tensor", ["rmax0", "rmax1", "const"], ["mm"], out=mm, in0=rmax, in1=sinkrow[:], op=ALU.max)
            P.op("dve", "tensor_scalar", ["mm"], ["negm"], out=negm, in0=mm, scalar1=-1.0, scalar2=None, op0=ALU.mult)
            P.op("dve", "tensor_tensor", ["mm", "const"], ["sk"], out=sk, in0=sinkrow[:], in1=mm, op=ALU.subtract)
            P.op("act", "activation", ["sk"], ["sk"], out=sk, in_=sk, func=ACT.Exp)
            for h in range(16):
                j = h // 2
                sl = sc[h // 8][:, h % 8, :]
                P.op("act", "activation", ["sc%d" % j, "negm"], ["sc%d" % j, "rs%d" % h], out=sl, in_=sl, func=ACT.Exp, bias=negm[:, h:h + 1], scale=1.0,
                     accum_out=rs[:, h:h + 1])
            P.op("dve", "tensor_tensor", ["rs%d" % h for h in range(16)] + ["sk"], ["den"], out=den, in0=rs, in1=sk, op=ALU.add)
            P.op("dve", "reciprocal", ["den"], ["rden"], out=rden, in_=den)
            if SWA_CUT <= 5:
                return
            for j in range(8):
                bank = j % 4
                for hh in range(2):
                    h = 2 * j + hh
                    for part in range(2):
                        P.op("pe", "transpose", ["sc%d" % j, "const"], ["p%d" % bank], out=pb[bank][:, (hh * 2 + part) * 128:(hh * 2 + part + 1) * 128],
                             in_=sc[h // 8][:, h % 8, part * 128:(part + 1) * 128], identity=ident[:])
                dstv = pT[j // 4][:, 2 * (j % 4):2 * (j % 4) + 2, :, :]
                srcv = pb[bank][:, :].rearrange("p (h c m) -> p h c m", h=2, c=2)
                if j % 2 == 0:
                    P.op("act", "activation", ["p%d" % bank], ["pT%d" % j], out=dstv, in_=srcv, func=ACT.Copy)
                else:
                    P.op("dve", "tensor_copy", ["p%d" % bank], ["pT%d" % j], out=dstv, in_=srcv)
            if SWA_CUT <= 6:
                return
            for h in range(16):
                j = h // 2
                g = h // 4
                bank = 4 + h // 8
                oreg = pb[bank][:, (h % 8) * 64:(h % 8 + 1) * 64]
                P.op("pe", "matmul", ["pT%d" % j, "v%d" % prv], ["p%d" % bank], oreg, lhsT=pT[h // 8][:, h % 8, 0, :], rhs=vbd[prv][:, g * 64:(g + 1) * 64],
                     start=True, stop=False)
                P.op("pe", "matmul", ["pT%d" % j, "v%d" % cur], ["p%d" % bank], oreg, lhsT=pT[h // 8][:, h % 8, 1, :], rhs=vbd[cur][:, g * 64:(g + 1) * 64],
                     start=False, stop=True)
            if SWA_CUT <= 7:
                return
            for b2 in range(2):
                P.op("dve", "tensor_tensor", ["p%d" % (4 + b2), "rden"], ["o"], out=o[:, b2 * 512:(b2 + 1) * 512].rearrange("p (h d) -> p h d", h=8),
                     in0=pb[4 + b2][:, :].rearrange("p (h d) -> p h d", h=8), in1=rden[:, b2 * 8:(b2 + 1) * 8].unsqueeze(2).to_broadcast([128, 8, 64]), op=ALU.mult)
            if SWA_CUT <= 8:
                return
            out_proj("o", o, swo_d, 2, xa, xname, bname="swo")

        def peer(xa, xname, l):
            mi = 2 if l == 0 else 8
            gi = 1 if l == 0 else 3
            hTn = modulate(xa, xname, mi, bf16=True)
            qT = SV(0, 0, 2048).rearrange("p (g t) -> p g t", g=16)
            s = SV(1, 0, 2048).rearrange("p (g n) -> p g n", g=16)
            sw = SV(2, 0, 2048).rearrange("p (g n) -> p g n", g=16)
            cand = SV(3, 0, 2048).rearrange("p (h a b) -> p h a b", h=8, a=16)
            cw = SV(4, 0, 2048).rearrange("p (h a b) -> p h a b", h=8, a=16)
            v16 = sm[:, 0:256].rearrange("p (g k) -> p g k", g=16)
            c16 = sm[:, 256:384].rearrange("p (h k) -> p h k", h=8)
            dd = sm[:, 384:512].rearrange("p (h k) -> p h k", h=8)
            Z = sm[:, 512:520]
            lnZ = sm[:, 520:528]
            bia = sm[:, 528:536]
            for bi in range(4):
                if BF16_PROJ and BF16_PQ:
                    nm, wb = wload_b("pwq%d" % l, bi * 512, 512)
                    rsrc, rn = hTb, ["hTb"] * KC
                else:
                    nm, wb = wload(pwq_d[l], bi * 512, 512)
                    rsrc, rn = hT, ["hT%d" % kc for kc in range(KC)]
                for gl in range(4):
                    g = bi * 4 + gl
                    for kc in range(KC):
                        P.op("pe", "matmul", [nm, rn[kc]], ["p%d" % bi], pb[bi][:, gl * 128:(gl + 1) * 128], lhsT=wb[:, kc, gl * 128:(gl + 1) * 128], rhs=rsrc[:, kc, :],
                             start=(kc == 0), stop=(kc == KC - 1))
                if bi % 2 == 0:
                    P.op("act", "activation", ["p%d" % bi], ["qT%d" % bi], out=qT[:, bi * 4:bi * 4 + 4, :], in_=pb[bi][:, :].rearrange("p (a b) -> p a b", a=4), func=ACT.Copy)
                else:
                    P.op("dve", "tensor_copy", ["p%d" % bi], ["qT%d" % bi], out=qT[:, bi * 4:bi * 4 + 4, :], in_=pb[bi][:, :].rearrange("p (a b) -> p a b", a=4))
            for g in range(16):
                bank = 4 + g // 4
                P.op("pe", "matmul", ["qT%d" % (g // 4), "const"], ["p%d" % bank], pb[bank][:, (g % 4) * 128:(g % 4 + 1) * 128], lhsT=qT[:, g, :], rhs=skT[:, l, g % 2, :],
                     start=True, stop=True)
            for b4 in range(4):
                bank = 4 + b4
                if b4 % 2 == 0:
                    P.op("act", "activation", ["p%d" % bank], ["s%d" % b4], out=s[:, b4 * 4:b4 * 4 + 4, :], in_=pb[bank][:, :].rearrange("p (a b) -> p a b", a=4), func=ACT.Copy)
                else:
                    P.op("dve", "tensor_copy", ["p%d" % bank], ["s%d" % b4], out=s[:, b4 * 4:b4 * 4 + 4, :], in_=pb[bank][:, :].rearrange("p (a b) -> p a b", a=4))
            snames_all = ["s%d" % b4 for b4 in range(4)]
            for g in range(16):
                P.op("dve", "max", ["s%d" % (g // 4)], ["v16a%d" % g], out=v16[:, g, 0:8], in_=s[:, g, :])
            for g in range(16):
                P.op("dve", "match_replace", ["s%d" % (g // 4), "v16a%d" % g], ["sw%d" % g], out=sw[:, g, :], in_to_replace=v16[:, g, 0:8], in_values=s[:, g, :], imm_value=NEG)
            for g in range(16):
                P.op("dve", "max", ["sw%d" % g], ["v16b%d" % g], out=v16[:, g, 8:16], in_=sw[:, g, :])
            v16n = ["v16a%d" % g for g in range(16)] + ["v16b%d" % g for g in range(16)]
            Ev = sm[:, 536:792].rearrange("p (g k) -> p g k", g=16)
            rZ = sm[:, 520:528]
            E = sw
            swn = ["sw%d" % g for g in range(16)]
            P.op("dve", "tensor_tensor", snames_all + v16n + swn, ["E"], out=E, in0=s, in1=v16[:, :, 0:1].to_broadcast([128, 16, 128]), op=ALU.subtract)
            P.op("act", "activation", ["E"], ["E"], out=E, in_=E, func=ACT.Exp)
            P.op("dve", "tensor_tensor", v16n, ["Ev"], out=Ev, in0=v16, in1=v16[:, :, 0:1].to_broadcast([128, 16, 16]), op=ALU.subtract)
            P.op("act", "activation", ["Ev"], ["Ev"], out=Ev, in_=Ev, func=ACT.Exp)
            E4 = E.rearrange("p (h c) n -> p h c n", c=2)
            Ev4 = Ev.rearrange("p (h c) k -> p h c k", c=2)
            c16n = ["c16a%d" % h for h in range(8)] + ["c16b%d" % h for h in range(8)]
            for rnd in range(2):
                P.op("dve", "tensor_tensor", ["Ev"], ["cand"], out=cand,
                     in0=Ev4[:, :, 0, :].unsqueeze(3).to_broadcast([128, 8, 16, 16]), in1=Ev4[:, :, 1, :].unsqueeze(2).to_broadcast([128, 8, 16, 16]), op=ALU.mult)
                for h in range(8):
                    P.op("dve", "max", ["cand"], ["c16a%d" % h], out=c16[:, h, 0:8], in_=cand[:, h])
                for h in range(8):
                    P.op("dve", "match_replace", ["cand", "c16a%d" % h], ["cw%d" % h], out=cw[:, h], in_to_replace=c16[:, h, 0:8], in_values=cand[:, h], imm_value=-1.0)
                for h in range(8):
                    P.op("dve", "max", ["cw%d" % h], ["c16b%d" % h], out=c16[:, h, 8:16], in_=cw[:, h])
                if rnd == 0:
                    P.op("dve", "tensor_reduce", c16n, ["Z"], out=Z, in_=c16, axis=AX.X, op=ALU.add)
                    P.op("dve", "reciprocal", ["Z"], ["rZ"], out=rZ, in_=Z)
                    P.op("dve", "tensor_tensor", ["E", "rZ"], ["E"], out=E4[:, :, 1, :], in0=E4[:, :, 1, :], in1=rZ.unsqueeze(2).to_broadcast([128, 8, 128]), op=ALU.mult)
                    P.op("dve", "tensor_tensor", ["Ev", "rZ"], ["Ev"], out=Ev4[:, :, 1, :], in0=Ev4[:, :, 1, :], in1=rZ.unsqueeze(2).to_broadcast([128, 8, 16]), op=ALU.mult)
            EEs = [SV(i, 0, 1024).rearrange("p (i j) -> p i j", i=8) for i in (5, 6, 0)]
            gtall = SV(7, 0, 2048).bitcast(BF16)
            Gts = [gtall[:, k * 1024:(k + 1) * 1024] for k in range(4)]
            gk = [0]

            def gbuild_head(n, h):
                k = gk[0] % 3
                k4 = gk[0] % 4
                gk[0] += 1
                EE, Gt = EEs[k], Gts[k4]
                een, gtn = "EE%d" % k, "Gt%d" % k4
                eenl = [een + ".%d" % ii for ii in range(8)]
                i0 = n * 8
                if h % 2 == ACT_PAR:
                    for ii in range(8):
                        P.op("act", "activation", ["E"] + c16n, [eenl[ii]], out=EE[:, ii, :], in_=E4[:, h, 1, :], func=ACT.Copy, scale=E4[:, h, 0, i0 + ii:i0 + ii + 1])
                else:
                    P.op("dve" if h % 4 == 3 else "pool", "tensor_tensor", ["E"] + c16n, eenl, out=EE, in0=E4[:, h, 0, i0:i0 + 8].unsqueeze(2).to_broadcast([128, 8, 128]),
                         in1=E4[:, h, 1, :].unsqueeze(1).to_broadcast([128, 8, 128]), op=ALU.mult)
                P.op("dve", "scalar_tensor_tensor", eenl + c16n, [gtn], out=Gt.rearrange("p (i j) -> p i j", i=8), in0=EE, scalar=c16[:, h, 15:16], in1=EE,
                     op0=ALU.is_ge, op1=ALU.mult)
                pend_acc.append((n, h, gtn, Gt))

            pend_acc = []

            def flush_pe_acc():
                for (n, h, gtn, Gt) in pend_acc:
                    for c in range(2):
                        bank = 2 + 2 * (n % 2) + c
                        P.op("pe", "matmul", [gtn, "identb"], ["p%d" % bank], pb[bank][:, :], lhsT=identb[:], rhs=Gt[:, c * 512:(c + 1) * 512], start=(h == 0), stop=(h == 7))
                del pend_acc[:]

            utv = utb_d[l].rearrange("(kc p) e -> p kc e", p=128)
            chain = [None]
            p1b = pb[1][:, :].bitcast(BF16)

            def tail(prev):
                (ebp, GA, GAT, sn, wnv, vblk, first, last) = prev
                half = p1b[:, (ebp % 2) * 512:(ebp % 2 + 1) * 512]
                for j in range(4):
                    P.op("pe", "transpose", [sn + ".GA", "identb"], ["p1"], out=half[:, j * 128:(j + 1) * 128], in_=GA[:, j * 128:(j + 1) * 128], identity=identb[:])
                P.op("act", "activation", ["p1"], [sn + ".GAT"], out=GAT, in_=half.rearrange("p (a b) -> p a b", a=4), func=ACT.Copy)
                for j in range(4):
                    for db in range(2):
                        P.op("pe", "matmul", [sn + ".GAT", wnv], ["p%d" % (6 + db)], pb[6 + db][:, :], lhsT=GAT[:, j, :], rhs=vblk[:, j, db * 512:(db + 1) * 512],
                             start=(first and j == 0), stop=(last and j == 3))

            for h in range(8):
                gbuild_head(0, h)
                if h % 4 == 3:
                    flush_pe_acc()
            for nb in range(32):
                n = nb // 2
                e0 = nb * 512
                i = wsel[0]
                wsel[0] ^= 1
                wnu = "wbuf%du" % i
                wnv = "wbuf%dv" % i
                wbb = wbuf[i][:].rearrange("p a b -> p (a b)").bitcast(BF16)
                ublk = wbb[:, 0:4096].rearrange("p (kc e) -> p kc e", kc=8)
                vblk = wbb[:, 4096:8192].rearrange("p (j d) -> p j d", j=4)
                extra = ["wbuf%d" % i] if nb < 2 else []
                P.dma(wnu, [], [wnu] + extra, [(ublk, utv[:, :, e0:e0 + 512])])
                P.dma(wnv, [], [wnv] + extra, [(vblk, vb_d[l][e0:e0 + 512, :].rearrange("(j p) d -> p j d", p=128))])
                si = 10 + (nb % 2)
                sn = "slot%d" % si
                Ag = SV(si, 0, 512)
                GA = SV(si, 512, 768).bitcast(BF16)
                GAT = SV(si, 1024, 1280).bitcast(BF16).rearrange("p (a b) -> p a b", a=4)
                for kc in range(KC):
                    P.op("pe", "matmul", [wnu, "hTb"], ["p0"], pb[0][:, :], lhsT=hTb[:, kc, :], rhs=ublk[:, kc, :], start=(kc == 0), stop=(kc == KC - 1))
                flush_pe_acc()
                P.op("act", "activation", ["p0"], [sn + ".Ag"], out=Ag, in_=pb[0][:, :], func=ACT.Gelu)
                gbank = 2 + 2 * (n % 2) + (nb % 2)
                P.op("dve", "tensor_tensor", [sn + ".Ag", "p%d" % gbank], [sn + ".GA"], out=GA, in0=pb[gbank][:, :], in1=Ag, op=ALU.mult)
                if chain[0] is not None:
                    tail(chain[0])
                if n < 15:
                    for h in range(4 * (nb % 2), 4 * (nb % 2) + 4):
                        gbuild_head(n + 1, h)
                chain[0] = (nb, GA, GAT, sn, wnv, vblk, nb == 0, nb == 31)
            tail(chain[0])
            for db in range(2):
                P.op("dve", "tensor_tensor", ["p%d" % (6 + db), "gates"], ["ytmp%d" % db], out=slot[8][:, db * 512:(db + 1) * 512],
                     in0=pb[6 + db][:, :], in1=gates[:, gi, db * 512:(db + 1) * 512], op=ALU.mult)
                P.op("pool", "tensor_tensor", ["ytmp%d" % db, xname], [xname], out=xa[:, db * 512:(db + 1) * 512], in0=xa[:, db * 512:(db + 1) * 512],
                     in1=slot[8][:, db * 512:(db + 1) * 512], op=ALU.add)

        def final_norm(xa, xname, it):
            P.op("act", "activation", [xname], ["junk", "ss0"], out=junk[:], in_=xa, func=ACT.Square, accum_out=ss[:, 0:1])
            P.op("dve", "tensor_scalar", ["ss0"], ["ss1"], out=ss[:, 1:2], in0=ss[:, 0:1], scalar1=1.0 / D, scalar2=EPS, op0=ALU.mult, op1=ALU.add)
            P.op("act", "activation", ["ss1"], ["ss2"], out=ss[:, 2:3], in_=ss[:, 1:2], func=ACT.Sqrt)
            P.op("dve", "reciprocal", ["ss2"], ["ss3"], out=ss[:, 3:4], in_=ss[:, 2:3])
            P.op("dve", "scalar_tensor_tensor", [xname, "ss3", "const"], ["xn"], out=xn[:], in0=xa, scalar=ss[:, 3:4], in1=gfrow[:], op0=ALU.mult, op1=ALU.mult)
            P.dma("yout", ["xn"], [], [(y_d[it * 128:(it + 1) * 128, :], xn[:])])

        stages = os.environ.get("YOCO_STAGES", "gla,peer0,kv,swa,peer1").split(",")
        for it in range(NT):
            xa = xt[it % 2][:]
            xname = "xt%d" % (it % 2)
            P.dma(xname, [], [xname], [(xa, x_d[it * 128:(it + 1) * 128, :])])
            if "gla" in stages:
                gla(xa, xname, it)
                P.barrier()
            if "peer0" in stages:
                peer(xa, xname, 0)
                P.barrier()
            if "kv" in stages:
                kv_phase(xa, xname, it)
                P.barrier()
            if "swa" in stages:
                swa(xa, xname, it)
                P.barrier()
            if "peer1" in stages:
                peer(xa, xname, 1)
                P.barrier()
            final_norm(xa, xname, it)
            P.barrier()
        P.barrier()
        P.emit()
        print("yoco build: ops", P.nops, {e: P.cnt[e] for e in P.ENG}, flush=True)
    return nc


def prep_inputs(inp, NT, nb=8):
    f = np.float32
    S = NT * 128
    t = np.arange(128)
    same = (t[:, None] // 64) == (t[None, :] // 64)
    tri = (same & (t[:, None] <= t[None, :])).astype(f) * f(-1.0 / 16)
    blk = same.astype(f) * f(-1.0 / 16)
    csel = ((t[:, None] // 64) == np.arange(2)[None, :]).astype(f) * f(-1.0 / 16)
    maskT = (same & (t[:, None] <= t[None, :])).astype(f)
    qi = np.arange(128)[:, None]
    mi = np.arange(256)[None, :]
    valid = (mi > qi) & (mi <= qi + 128)
    swam = np.stack([np.where(valid & (mi >= 128), 0.0, NEG), np.where(valid, 0.0, NEG)], axis=1).astype(f)
    invf = (np.float32(500000.0) ** (-(np.arange(0, 16, 2, dtype=np.float32)) / np.float32(16))).astype(f)
    invf = np.ascontiguousarray(np.broadcast_to(invf[None, :], (128, 8)))

    def col(v):
        return np.ascontiguousarray(v.reshape(8, 128).T)

    def row(v):
        return np.ascontiguousarray(np.broadcast_to(v[None, :], (128, v.shape[0])))

    mod_b = inp["mod_b"]
    shared = {
        "invf": invf, "ident": np.eye(128, dtype=f), "tri": tri, "blk": blk, "csel": csel, "maskT": maskT, "swam": swam,
        "modw0": np.ascontiguousarray(inp["mod_w"][0]), "modw1": np.ascontiguousarray(inp["mod_w"][1]),
        "modbcol": np.ascontiguousarray(np.stack([mod_b[l].reshape(48, 128).T for l in range(2)], axis=1)),
        "gaterow": np.ascontiguousarray(np.stack([row(mod_b[0, 2048:3072]), row(mod_b[0, 5120:6144]),
                                                   row(mod_b[1, 2048:3072]), row(mod_b[1, 5120:6144])], axis=1)),
        "kvmodw": np.ascontiguousarray(inp["kv_mod_w"]),
        "kvmodbcol": np.ascontiguousarray(inp["kv_mod_b"].reshape(16, 128).T),
        "ngcol": np.ascontiguousarray(np.stack([col(inp["norm_g"][0, 0]), col(inp["norm_g"][0, 1]), col(inp["norm_g"][1, 0]),
                                                col(inp["norm_g"][1, 1]), col(inp["kv_norm_g"])], axis=1)),
        "gfrow": row(inp["final_norm_g"]),
        "w_in": np.ascontiguousarray(inp["gla_w_in"][0]), "wg2": np.ascontiguousarray(inp["gla_w_g2"][0]),
        "bg2row": row(inp["gla_b_g2"][0]), "gnrow": row(inp["gla_norm_g"][0]), "gwo": np.ascontiguousarray(inp["gla_w_out"][0]),
        "kvw": np.ascontiguousarray(inp["kv_w"]), "swq": np.ascontiguousarray(inp["swa_w_q"][0]),
        "sinkrow": row(inp["swa_sinks"][0]), "swo": np.ascontiguousarray(inp["swa_w_out"][0]),
        "pwq0": np.ascontiguousarray(inp["peer_w_q"][0]), "pwq1": np.ascontiguousarray(inp["peer_w_q"][1]),
        "skT": np.ascontiguousarray(np.transpose(inp["peer_subkeys"], (3, 0, 1, 2))),
        "ut0": np.ascontiguousarray(inp["peer_u"][0].T), "ut1": np.ascontiguousarray(inp["peer_u"][1].T),
        "v0": np.ascontiguousarray(inp["peer_v"][0]), "v1": np.ascontiguousarray(inp["peer_v"][1]),
    }
    maps = []
    for b in range(nb):
        m = dict(shared)
        m["x"] = np.ascontiguousarray(inp["x"][b, :S])
        m["ccol"] = col(inp["c"][b])
        m["pos"] = np.ascontiguousarray(inp["positions"][b, :S].reshape(NT, 128).T.astype(np.int32))
        maps.append(m)
    return maps


_NC_CACHE = {}


def kernel(**inputs):
    inputs = {k: np.asarray(v) for k, v in inputs.items()}
    NT = SEQ // 128
    if NT not in _NC_CACHE:
        _NC_CACHE[NT] = build(NT)
    nc = _NC_CACHE[NT]
    maps = prep_inputs(inputs, NT)
    res = run_bass_kernel_spmd(nc, maps, core_ids=list(range(8)))
    out = np.stack([np.asarray(r["y"]) for r in res.results], axis=0)
    return out.astype(np.float32)
```

```python
import os
from contextlib import ExitStack
import numpy as np
import concourse.bass as bass
import concourse.mybir as mybir
from concourse.bass_utils import run_bass_kernel_spmd

F32 = mybir.dt.float32
BF16 = mybir.dt.bfloat16
I32 = mybir.dt.int32
ACT = mybir.ActivationFunctionType
ALU = mybir.AluOpType
AX = mybir.AxisListType

D = 1024
KC = 8
SEQ = 8192
NEXP = 16384
EPS = 1e-6
NEG = -1e30
ACT_PAR = int(os.environ.get('ACT_PAR', '-1'))
BF16_PROJ = os.environ.get('BF16_PROJ', '1') == '1'
BF16_PQ = os.environ.get('BF16_PQ', '1') == '1'
SWA_CUT = int(os.environ.get('SWA_CUT', '99'))
MODEV = os.environ.get('MODEV', 'dve')
GLA_CUT = int(os.environ.get('GLA_CUT', '99'))
PI = float(np.pi)


class Prog:
    ENG = ("pe", "dve", "act", "pool", "sp")

    def __init__(self, nc, es):
        self.nc = nc
        self.es = es
        self.ops = {e: [] for e in self.ENG}
        self.sem = {e: es.enter_context(nc.semaphore("sem_" + e)) for e in self.ENG}
        self.cnt = {e: 0 for e in self.ENG}
        self.dsem = {}
        self.dcnt = {}
        self.lastw = {}
        self.readers = {}
        self.waited = {e: {} for e in self.ENG}
        self.nops = 0

    def _deps(self, eng, r, w):
        deps = {}

        def add(tok):
            if tok is None:
                return
            s, v = tok
            if eng == "pe" and s.name == self.sem["pe"].name:
                return
            if deps.get(s.name, (None, 0))[1] < v:
                deps[s.name] = (s, v)

        for b in r:
            add(self.lastw.get(b))
        for b in w:
            add(self.lastw.get(b))
            for tok in self.readers.get(b, {}).values():
                add(tok)
        out = []
        for key, (s, v) in deps.items():
            if self.waited[eng].get(key, 0) < v:
                self.waited[eng][key] = v
                out.append((s, v))
        return out

    def _commit(self, tok, r, w):
        for b in w:
            self.lastw[b] = tok
            self.readers[b] = {}
        for b in r:
            self.readers.setdefault(b, {})[tok[0].name] = tok

    def op(self, eng, meth, r, w, *a, **k):
        w = list(w) + [b for b in r if len(b) == 2 and b[0] == "p" and b[1].isdigit()]
        waits = self._deps(eng, r, w)
        self.cnt[eng] += 1
        tok = (self.sem[eng], self.cnt[eng])
        self.ops[eng].append((waits, meth, a, k, self.sem[eng], 1))
        self._commit(tok, r, w)
        self.nops += 1

    def dma(self, key, r, w, pairs, eng="sp"):
        if key not in self.dsem:
            self.dsem[key] = self.es.enter_context(self.nc.semaphore("dsem_" + key))
            self.dcnt[key] = 0
        waits = self._deps(eng, r, w)
        for (o, i) in pairs:
            self.dcnt[key] += 16
            self.ops[eng].append((waits, "dma_start", (), dict(out=o, in_=i), self.dsem[key], 16))
            waits = []
            self.nops += 1
        tok = (self.dsem[key], self.dcnt[key])
        self._commit(tok, r, w)

    def barrier(self):
        allw = [(self.sem[e], self.cnt[e]) for e in self.ENG if self.cnt[e] > 0]
        allw += [(s, self.dcnt[k]) for k, s in self.dsem.items()]
        for e in self.ENG:
            ws = []
            for (s, v) in allw:
                if s.name == self.sem[e].name:
                    continue
                if self.waited[e].get(s.name, 0) < v:
                    self.waited[e][s.name] = v
                    ws.append((s, v))
            if ws:
                self.ops[e].append((ws, None, (), {}, None, 0))
        self.lastw = {}
        self.readers = {}

    def emit(self):
        nc = self.nc
        with nc.Block() as block:
            def run(engname):
                def body(engine):
                    for waits, meth, a, k, sem, inc in self.ops[engname]:
                        for (s, v) in waits:
                            engine.wait_ge(s, v)
                        if meth is None:
                            continue
                        getattr(engine, meth)(*a, **k).then_inc(sem, inc)
                return body
            block.tensor(run("pe"))
            block.vector(run("dve"))
            block.scalar(run("act"))
            block.gpsimd(run("pool"))
            block.sync(run("sp"))


def build(NT, dbg=None):
    nc = bass.Bass("TRN2", target_bir_lowering=False)
    S = NT * 128

    def din(name, shape, dt=F32):
        return nc.dram_tensor(name, list(shape), dt, kind="ExternalInput").ap()

    x_d = din("x", [S, D])
    y_d = nc.dram_tensor("y", [S, D], F32, kind="ExternalOutput").ap()
    ccol_d = din("ccol", [128, 8])
    pos_d = din("pos", [128, NT], I32)
    invf_d = din("invf", [128, 8])
    modw_d = [din("modw0", [D, 6 * D]), din("modw1", [D, 6 * D])]
    modbcol_d = din("modbcol", [128, 2, 48])
    gaterow_d = din("gaterow", [128, 4, D])
    kvmodw_d = din("kvmodw", [D, 2 * D])
    kvmodbcol_d = din("kvmodbcol", [128, 16])
    ngcol_d = din("ngcol", [128, 5, 8])
    gfrow_d = din("gfrow", [128, D])
    w_in_d = din("w_in", [D, 3088])
    wg2_d = din("wg2", [16, 512])
    bg2row_d = din("bg2row", [128, 512])
    gnrow_d = din("gnrow", [128, 256])
    gwo_d = din("gwo", [D, D])
    kvw_d = din("kvw", [D, 512])
    swq_d = din("swq", [D, D])
    sinkrow_d = din("sinkrow", [128, 16])
    swo_d = din("swo", [D, D])
    pwq_d = [din("pwq0", [D, 2048]), din("pwq1", [D, 2048])]
    skT_d = din("skT", [128, 2, 2, 128])
    ut_d = [din("ut0", [D, NEXP]), din("ut1", [D, NEXP])]
    v_d = [din("v0", [NEXP, D]), din("v1", [NEXP, D])]
    ident_d = din("ident", [128, 128])
    tri_d = din("tri", [128, 128])
    blk_d = din("blk", [128, 128])
    csel_d = din("csel", [128, 2])
    maskT_d = din("maskT", [128, 128])
    swam_d = din("swam", [128, 2, 256])
    utb_d = [nc.dram_tensor("utb%d" % l, [D, NEXP], BF16, kind="Internal").ap() for l in range(2)]
    vb_d = [nc.dram_tensor("vb%d" % l, [NEXP, D], BF16, kind="Internal").ap() for l in range(2)]
    wb16 = {nm: nc.dram_tensor("b16_" + nm, [D, w], BF16, kind="Internal").ap()
            for nm, w in (("w_in", 3072), ("gwo", D), ("kvw", 512), ("swq", D), ("swo", D), ("pwq0", 2048), ("pwq1", 2048))}
    wsrc = {"w_in": w_in_d, "gwo": gwo_d, "kvw": kvw_d, "swq": swq_d, "swo": swo_d, "pwq0": pwq_d[0], "pwq1": pwq_d[1]}
    dbg_d = None
    if dbg is not None:
        dbg_d = nc.dram_tensor("dbg", [128, 8192], F32, kind="ExternalOutput").ap()

    es = ExitStack()
    with es:
        P = Prog(nc, es)

        def sb(name, shape, dt=F32):
            return es.enter_context(nc.sbuf_tensor("sb_" + name, list(shape), dt))

        def psum(name):
            return es.enter_context(nc.psum_tensor(name, [128, 512], F32))

        ident = sb("ident", [128, 128])
        identb = sb("identb", [128, 128], BF16)
        tri = sb("tri", [128, 128])
        blk = sb("blk", [128, 128])
        csel = sb("csel", [128, 2])
        maskT = sb("maskT", [128, 128])
        swam = sb("swam", [128, 2, 256])
        gates = sb("gates", [128, 4, D])
        gfrow = sb("gfrow", [128, D])
        gnrow = sb("gnrow", [128, 256])
        bg2row = sb("bg2row", [128, 512])
        sinkrow = sb("sinkrow", [128, 16])
        wg2 = sb("wg2", [16, 512])
        skT = sb("skT", [128, 2, 2, 128])
        modc = sb("modc", [128, 10, 8])
        ngcol = sb("ngcol", [128, 5, 8])
        modbcol = sb("modbcol", [128, 2, 48])
        kvmodbcol = sb("kvmodbcol", [128, 16])
        ccol = sb("ccol", [128, 8])
        cact = sb("cact", [128, 8])
        cbc = sb("cbc", [128, 8, 128])
        cosT = sb("cosT", [128, NT, 8])
        sinT = sb("sinT", [128, NT, 8])
        posi = sb("posi", [128, NT], I32)
        invf = sb("invf", [128, 8])
        xt = [sb("xt0", [128, D]), sb("xt1", [128, D])]
        xn = sb("xn", [128, D])
        hT = sb("hT", [128, KC, 128])
        hTb = sb("hTb", [128, KC, 128], BF16)
        junk = sb("junk", [128, D], BF16)
        ss = sb("ss", [128, 8])
        sm = sb("sm", [128, 1024])
        wbuf = [sb("wbuf0", [128, KC, 512]), sb("wbuf1", [128, KC, 512])]
        NSLOT = 12
        slot = [sb("slot%d" % i, [128, 2048]) for i in range(NSLOT)]
        Sst = [sb("Sa", [128, 4, 256]), sb("Sb", [128, 4, 256])]
        kTd = [sb("kTd0", [128, 4, 128]), sb("kTd1", [128, 4, 128])]
        vbd = [sb("vbd0", [128, 256]), sb("vbd1", [128, 256])]
        pb = [psum("p%d" % i) for i in range(8)]

        def SV(i, lo, hi):
            return slot[i][:, lo:hi]

        consts = [(ident, ident_d), (tri, tri_d), (blk, blk_d), (csel, csel_d), (maskT, maskT_d), (swam, swam_d),
                  (gates, gaterow_d), (gfrow, gfrow_d), (gnrow, gnrow_d), (bg2row, bg2row_d), (sinkrow, sinkrow_d),
                  (wg2, wg2_d), (skT, skT_d), (ngcol, ngcol_d), (modbcol, modbcol_d), (kvmodbcol, kvmodbcol_d),
                  (ccol, ccol_d), (posi, pos_d), (invf, invf_d)]
        P.dma("const", [], ["const"], [(t[:], d) for (t, d) in consts])
        P.op("dve", "memset", [], ["Sa"], Sst[0][:], 0.0)
        P.op("dve", "memset", [], ["k1"], kTd[1][:], 0.0)
        P.op("dve", "memset", [], ["v1"], vbd[1][:], 0.0)
        P.op("dve", "tensor_copy", ["const"], ["identb"], out=identb[:], in_=ident[:])
        P.op("act", "activation", ["const"], ["cact"], out=cact[:], in_=ccol[:], func=ACT.Silu)
        P.op("dve", "tensor_copy", ["cact"], ["cbc"], out=cbc[:], in_=cact[:].unsqueeze(2).to_broadcast([128, 8, 128]))
        posf = sm[:, 0:NT]
        ang = slot[0][:, 0:NT * 8].rearrange("p (a b) -> p a b", b=8)
        ang2 = slot[0][:, 1024:1024 + NT * 8].rearrange("p (a b) -> p a b", b=8)
        kf = slot[1][:, 0:NT * 8].rearrange("p (a b) -> p a b", b=8)
        ki = slot[2][:, 0:NT * 8].bitcast(I32).rearrange("p (a b) -> p a b", b=8)
        P.op("dve", "tensor_copy", ["const"], ["posf"], out=posf, in_=posi[:])
        P.op("dve", "tensor_tensor", ["posf", "const"], ["ang"], out=ang, in0=posf.unsqueeze(2).to_broadcast([128, NT, 8]),
             in1=invf[:].unsqueeze(1).to_broadcast([128, NT, 8]), op=ALU.mult)
        P.op("dve", "tensor_scalar_add", ["ang"], ["ang2"], out=ang2, in0=ang, scalar1=PI / 2)
        for (src, nm, dst) in ((ang, "ang", sinT), (ang2, "ang2", cosT)):
            P.op("dve", "tensor_scalar", [nm], ["ki"], out=ki, in0=src, scalar1=float(1.0 / (2 * PI)), scalar2=None, op0=ALU.mult)
            P.op("dve", "tensor_copy", ["ki"], ["kf"], out=kf, in_=ki)
            P.op("dve", "scalar_tensor_tensor", ["kf", nm], [nm], out=src, in0=kf, scalar=float(-2 * PI), in1=src, op0=ALU.mult, op1=ALU.add)
            P.op("dve", "tensor_single_scalar", [nm], ["kf"], out=kf, in_=src, scalar=PI, op=ALU.is_gt)
            P.op("dve", "scalar_tensor_tensor", ["kf", nm], [nm], out=src, in0=kf, scalar=float(-2 * PI), in1=src, op0=ALU.mult, op1=ALU.add)
            P.op("dve", "tensor_single_scalar", [nm], ["kf"], out=kf, in_=src, scalar=-PI, op=ALU.is_lt)
            P.op("dve", "scalar_tensor_tensor", ["kf", nm], [nm], out=src, in0=kf, scalar=float(2 * PI), in1=src, op0=ALU.mult, op1=ALU.add)
            P.op("act", "activation", [nm], [nm + "_out"], out=dst[:], in_=src, func=ACT.Sin)

        wsel = [0]

        def wload(dram_w, c0, ncols):
            i = wsel[0]
            wsel[0] ^= 1
            nm = "wbuf%d" % i
            P.dma(nm, [], [nm], [(wbuf[i][:, :, 0:ncols], dram_w[:, c0:c0 + ncols].rearrange("(kc p) n -> p kc n", p=128))])
            return nm, wbuf[i]

        mcol = sm[:, 64:64 + 96].rearrange("p (a b) -> p a b", b=8)
        for l in range(2):
            for bi in range(12):
                nm, wb = wload(modw_d[l], bi * 512, 512)
                kind = bi // 2
                if kind in (2, 5):
                    gi = l * 2 + (0 if kind == 2 else 1)
                    half = bi % 2
                    for kc in range(KC):
                        P.op("pe", "matmul", [nm, "cbc"], ["p0"], pb[0][:, :], lhsT=cbc[:, kc, :], rhs=wb[:, kc, :], start=(kc == 0), stop=(kc == KC - 1))
                    P.op("dve", "tensor_tensor", ["p0", "const"], ["gates"], out=gates[:, gi, half * 512:(half + 1) * 512], in0=pb[0][:, :],
                         in1=gates[:, gi, half * 512:(half + 1) * 512], op=ALU.add)
                else:
                    for j in range(4):
                        for kc in range(KC):
                            P.op("pe", "matmul", [nm, "cact"], ["p1"], pb[1][:, j:j + 1], lhsT=wb[:, kc, j * 128:(j + 1) * 128], rhs=cact[:, kc:kc + 1],
                                 start=(kc == 0), stop=(kc == KC - 1))
                    vi = {0: 0, 1: 1, 3: 2, 4: 3}[kind]
                    P.op("dve", "tensor_tensor", ["p1", "const"], ["mcol"], out=mcol[:, l * 4 + vi, (bi % 2) * 4:(bi % 2) * 4 + 4], in0=pb[1][:, 0:4],
                         in1=modbcol[:, l, bi * 4:bi * 4 + 4], op=ALU.add)
        for bi in range(4):
            nm, wb = wload(kvmodw_d, bi * 512, 512)
            for j in range(4):
                for kc in range(KC):
                    P.op("pe", "matmul", [nm, "cact"], ["p1"], pb[1][:, j:j + 1], lhsT=wb[:, kc, j * 128:(j + 1) * 128], rhs=cact[:, kc:kc + 1],
                         start=(kc == 0), stop=(kc == KC - 1))
            P.op("dve", "tensor_tensor", ["p1", "const"], ["mcol"], out=mcol[:, 8 + bi // 2, (bi % 2) * 4:(bi % 2) * 4 + 4], in0=pb[1][:, 0:4],
                 in1=kvmodbcol[:, bi * 4:bi * 4 + 4], op=ALU.add)
        for (mi, gi, shi, sci) in ((0, 0, 0, 1), (2, 1, 2, 3), (4, 4, 8, 9), (6, 2, 4, 5), (8, 3, 6, 7)):
            P.op("dve", "scalar_tensor_tensor", ["mcol", "const"], ["modc"], out=modc[:, mi, :], in0=mcol[:, sci, :], scalar=1.0, in1=ngcol[:, gi, :],
                 op0=ALU.add, op1=ALU.mult)
            P.op("dve", "tensor_copy", ["mcol"], ["modc"], out=modc[:, mi + 1, :], in_=mcol[:, shi, :])
        P.barrier()

        cv = [0]

        def convert(src_ap, dst_ap, W=4096):
            i = cv[0] % 3
            cv[0] += 1
            H = W // 2
            P.dma("cin%d" % i, [], ["cin%d" % i], [(slot[2 * i][:, 0:H], src_ap[:, 0:H]), (slot[2 * i + 1][:, 0:H], src_ap[:, H:W])])
            ob = slot[6 + i][:, :].bitcast(BF16)
            eng = ("dve", "act", "pool")[i]
            if eng == "act":
                P.op("act", "activation", ["cin%d" % i], ["cob%d" % i], out=ob[:, 0:H], in_=slot[2 * i][:, 0:H], func=ACT.Copy)
                P.op("act", "activation", ["cin%d" % i], ["cob%d" % i], out=ob[:, H:W], in_=slot[2 * i + 1][:, 0:H], func=ACT.Copy)
            else:
                P.op(eng, "tensor_copy", ["cin%d" % i], ["cob%d" % i], out=ob[:, 0:H], in_=slot[2 * i][:, 0:H])
                P.op(eng, "tensor_copy", ["cin%d" % i], ["cob%d" % i], out=ob[:, H:W], in_=slot[2 * i + 1][:, 0:H])
            P.dma("cout%d" % i, ["cob%d" % i], [], [(dst_ap, ob[:, 0:W])])

        if BF16_PROJ:
            for nm in ("w_in", "gwo", "kvw", "swq", "swo") + (("pwq0", "pwq1") if BF16_PQ else ()):
                W = wb16[nm].shape[1]
                for kc in range(KC):
                    convert(wsrc[nm][kc * 128:(kc + 1) * 128, 0:W], wb16[nm][kc * 128:(kc + 1) * 128, :], W=W)

        for l in range(2):
            for kc in range(KC):
                for e4 in range(4):
                    convert(ut_d[l][kc * 128:(kc + 1) * 128, e4 * 4096:(e4 + 1) * 4096],
                            utb_d[l][kc * 128:(kc + 1) * 128, e4 * 4096:(e4 + 1) * 4096])
            for r in range(32):
                convert(v_d[l][r * 512:(r + 1) * 512, :].rearrange("(p j) d -> p (j d)", j=4),
                        vb_d[l][r * 512:(r + 1) * 512, :].rearrange("(p j) d -> p (j d)", j=4))
        P.barrier()

        def modulate(xa, xname, mi, bf16=False, need32=True):
            P.op("act", "activation", [xname], ["junk", "ss0"], out=junk[:], in_=xa, func=ACT.Square, accum_out=ss[:, 0:1])
            P.op("dve", "tensor_scalar", ["ss0"], ["ss1"], out=ss[:, 1:2], in0=ss[:, 0:1], scalar1=1.0 / D, scalar2=EPS, op0=ALU.mult, op1=ALU.add)
            P.op("act", "activation", ["ss1"], ["ss2"], out=ss[:, 2:3], in_=ss[:, 1:2], func=ACT.Sqrt)
            P.op("dve", "reciprocal", ["ss2"], ["ss3"], out=ss[:, 3:4], in_=ss[:, 2:3])
            P.op("dve", "tensor_scalar", [xname, "ss3"], ["xn"], out=xn[:], in0=xa, scalar1=ss[:, 3:4], scalar2=None, op0=ALU.mult)
            for kc in range(KC):
                bnk = kc // 4
                P.op("pe", "transpose", ["xn", "const"], ["p%d" % bnk], out=pb[bnk][:, (kc % 4) * 128:(kc % 4 + 1) * 128],
                     in_=xn[:, kc * 128:(kc + 1) * 128], identity=ident[:])
            direct = bf16 and not need32
            for kc in range(KC):
                bnk = kc // 4
                src = pb[bnk][:, (kc % 4) * 128:(kc % 4 + 1) * 128]
                dst = hTb[:, kc, :] if direct else hT[:, kc, :]
                dn = "hTb" if direct else "hT%d" % kc
                P.op("dve", "tensor_scalar", ["p%d" % bnk, "modc"], [dn], out=dst, in0=src,
                     scalar1=modc[:, mi, kc:kc + 1], scalar2=modc[:, mi + 1, kc:kc + 1], op0=ALU.mult, op1=ALU.add)
            if bf16 and need32:
                P.op("act", "activation", ["hT%d" % kc for kc in range(4)], ["hTb"], out=hTb[:, 0:4, :], in_=hT[:, 0:4, :], func=ACT.Copy)
                P.op("act", "activation", ["hT%d" % kc for kc in range(4, 8)], ["hTb"], out=hTb[:, 4:8, :], in_=hT[:, 4:8, :], func=ACT.Copy)
            return ["hT%d" % kc for kc in range(KC)]

        def wload_b(wname, c0, ncols):
            i = wsel[0]
            wsel[0] ^= 1
            nm = "wbuf%d" % i
            wv = wbuf[i][:].rearrange("p a b -> p (a b)").bitcast(BF16)[:, 0:4096].rearrange("p (kc n) -> p kc n", kc=8)
            P.dma(nm, [], [nm], [(wv[:, :, 0:ncols], wb16[wname][:, c0:c0 + ncols].rearrange("(kc p) n -> p kc n", p=128))])
            return nm, wv

        def dense_block(hTn, dram_w, c0, ncols, bank, bname=None):
            if BF16_PROJ and bname is not None:
                nm, wb = wload_b(bname, c0, ncols)
                for kc in range(KC):
                    P.op("pe", "matmul", [nm, "hTb"], ["p%d" % bank], pb[bank][:, 0:ncols], lhsT=hTb[:, kc, :], rhs=wb[:, kc, 0:ncols],
                         start=(kc == 0), stop=(kc == KC - 1))
                return
            nm, wb = wload(dram_w, c0, ncols)
            for kc in range(KC):
                P.op("pe", "matmul", [nm, "hT%d" % kc], ["p%d" % bank], pb[bank][:, 0:ncols], lhsT=hT[:, kc, :], rhs=wb[:, kc, 0:ncols],
                     start=(kc == 0), stop=(kc == KC - 1))

        def out_proj(onm, oap, dram_w, gi, xa, xname, bname=None):
            useb = BF16_PROJ and bname is not None
            if useb:
                oT = SV(5, 1024, 1536).bitcast(BF16).rearrange("p (a b) -> p a b", a=8)
            else:
                oT = SV(5, 1024, 2048).rearrange("p (a b) -> p a b", a=8)
            for kc in range(KC):
                bnk = kc // 4
                P.op("pe", "transpose", [onm, "const"], ["p%d" % bnk], out=pb[bnk][:, (kc % 4) * 128:(kc % 4 + 1) * 128],
                     in_=oap[:, kc * 128:(kc + 1) * 128], identity=ident[:])
            P.op("act", "activation", ["p0"], ["oT0"], out=oT[:, 0:4, :], in_=pb[0][:, :].rearrange("p (a b) -> p a b", a=4), func=ACT.Copy)
            P.op("dve", "tensor_copy", ["p1"], ["oT1"], out=oT[:, 4:8, :], in_=pb[1][:, :].rearrange("p (a b) -> p a b", a=4))
            for half in range(2):
                if useb:
                    nm, wb = wload_b(bname, half * 512, 512)
                else:
                    nm, wb = wload(dram_w, half * 512, 512)
                bank = 2 + half
                for kc in range(KC):
                    P.op("pe", "matmul", [nm, "oT%d" % (kc // 4)], ["p%d" % bank], pb[bank][:, :], lhsT=oT[:, kc, :], rhs=wb[:, kc, 0:512],
                         start=(kc == 0), stop=(kc == KC - 1))
                P.op("dve", "tensor_tensor", ["p%d" % bank, "gates"], ["ytmp%d" % half], out=sm[:, half * 512:(half + 1) * 512], in0=pb[bank][:, :],
                     in1=gates[:, gi, half * 512:(half + 1) * 512], op=ALU.mult)
                P.op("pool", "tensor_tensor", ["ytmp%d" % half, xname], [xname], out=xa[:, half * 512:(half + 1) * 512],
                     in0=xa[:, half * 512:(half + 1) * 512], in1=sm[:, half * 512:(half + 1) * 512], op=ALU.add)

        def gla(xa, xname, it):
            hTn = modulate(xa, xname, 0, bf16=BF16_PROJ)
            if GLA_CUT <= 0:
                return
            qk = SV(0, 0, 1024)
            la = SV(0, 1024, 1536)
            zb = SV(0, 1536, 2048)
            vv = SV(1, 0, 1024)
            og = SV(1, 1024, 2048)
            eb = SV(2, 0, 512)
            enb = SV(2, 512, 1024)
            ebl = SV(2, 1024, 1536)
            scm = SV(2, 1536, 2048).rearrange("p (a b) -> p a b", a=4)
            qt = SV(3, 0, 512)
            kt = SV(3, 512, 1024)
            kdec = SV(3, 1024, 1536)
            glT = slot[3][0:16, 1536:1664]
            dec = SV(3, 1664, 1672)
            qtT0 = SV(4, 0, 512).rearrange("p (a b) -> p a b", a=4)
            qtT1 = SV(4, 512, 1024).rearrange("p (a b) -> p a b", a=4)
            ktT = SV(4, 1024, 1536).rearrange("p (a b) -> p a b", a=4)
            qtT = SV(4, 1536, 2048).rearrange("p (a b) -> p a b", a=4)
            osb = SV(5, 0, 1024)
            dsts = [(qk[:, 0:512], "q"), (qk[:, 512:1024], "k"), (vv[:, 0:512], "v0"), (vv[:, 512:1024], "v1"),
                    (og[:, 0:512], "og0"), (og[:, 512:1024], "og1")]
            for bi, (dst, dn) in enumerate(dsts):
                bank = 2 + (bi % 2)
                dense_block(hTn, w_in_d, bi * 512, 512, bank, bname="w_in")
                if bi % 2 == 0:
                    P.op("act", "activation", ["p%d" % bank], [dn], out=dst, in_=pb[bank][:, :], func=ACT.Copy)
                else:
                    P.op("dve", "tensor_copy", ["p%d" % bank], [dn], out=dst, in_=pb[bank][:, :])
            if GLA_CUT <= 1:
                return
            nm, wb = wload(w_in_d, 3072, 16)
            for kc in range(KC):
                P.op("pe", "matmul", [nm, "hT%d" % kc], ["p4"], pb[4][0:16, 0:128], lhsT=wb[:, kc, 0:16], rhs=hT[:, kc, :], start=(kc == 0), stop=(kc == KC - 1))
            P.op("dve", "tensor_copy", ["p4"], ["glT"], out=glT, in_=pb[4][0:16, 0:128])
            P.op("pe", "matmul", ["glT", "const"], ["p5"], pb[5][:, :], lhsT=glT, rhs=wg2[:, :], start=True, stop=True)
            P.op("dve", "tensor_tensor", ["p5", "const"], ["zb"], out=zb, in0=pb[5][:, :], in1=bg2row[:], op=ALU.add)
            P.op("act", "activation", ["zb"], ["zb"], out=zb, in_=zb, func=ACT.Exp, scale=-1.0)
            P.op("act", "activation", ["zb"], ["la"], out=la, in_=zb, func=ACT.Ln, bias=1.0)
            if GLA_CUT <= 2:
                return
            P.op("pe", "matmul", ["la", "const"], ["p4"], pb[4][:, :], lhsT=tri[:], rhs=la, start=True, stop=True)
            P.op("pe", "matmul", ["la", "const"], ["p5"], pb[5][:, :], lhsT=blk[:], rhs=la, start=True, stop=True)
            for h in range(4):
                P.op("pe", "matmul", ["la", "const"], ["p6"], pb[6][:, 2 * h:2 * h + 2], lhsT=la[:, h * 128:(h + 1) * 128], rhs=csel[:], start=True, stop=True)
            P.op("act", "activation", ["p4"], ["eb"], out=eb, in_=pb[4][:, :], func=ACT.Exp)
            P.op("act", "activation", ["p4"], ["enb"], out=enb, in_=pb[4][:, :], func=ACT.Exp, scale=-1.0)
            P.op("act", "activation", ["p5"], ["ebl"], out=ebl, in_=pb[5][:, :], func=ACT.Exp)
            P.op("act", "activation", ["p6"], ["dec"], out=dec, in_=pb[6][:, 0:8], func=ACT.Exp)
            P.op("dve", "scalar_tensor_tensor", ["q", "eb"], ["qt"], out=qt, in0=qk[:, 0:512], scalar=float(128 ** -0.5), in1=eb, op0=ALU.mult, op1=ALU.mult)
            P.op("dve", "tensor_tensor", ["k", "enb"], ["kt"], out=kt, in0=qk[:, 512:1024], in1=enb, op=ALU.mult)
            P.op("pool", "tensor_tensor", ["kt", "ebl"], ["kdec"], out=kdec, in0=kt, in1=ebl, op=ALU.mult)
            if GLA_CUT <= 3:
                return
            for h in range(4):
                P.op("pe", "transpose", ["qt", "const"], ["p0"], out=pb[0][:, h * 128:(h + 1) * 128], in_=qt[:, h * 128:(h + 1) * 128], identity=ident[:])
            for h in range(4):
                P.op("pe", "transpose", ["kt", "const"], ["p1"], out=pb[1][:, h * 128:(h + 1) * 128], in_=kt[:, h * 128:(h + 1) * 128], identity=ident[:])
            p0v = pb[0][:, :].rearrange("p (a b) -> p a b", a=4)
            P.op("act", "activation", ["p0"], ["qtT"], out=qtT, in_=p0v, func=ACT.Copy)
            P.op("pool", "memset", [], ["qtT0", "qtT1"], slot[4][:, 0:1024], 0.0)
            P.op("dve", "tensor_copy", ["p0"], ["qtT0"], out=qtT0[:, :, 0:64], in_=p0v[:, :, 0:64])
            P.op("dve", "tensor_copy", ["p0"], ["qtT1"], out=qtT1[:, :, 64:128], in_=p0v[:, :, 64:128])
            P.op("act", "activation", ["p1"], ["ktT"], out=ktT, in_=pb[1][:, :].rearrange("p (a b) -> p a b", a=4), func=ACT.Copy)
            if GLA_CUT <= 4:
                return
            for h in range(4):
                P.op("pe", "matmul", ["ktT", "qtT"], ["p4"], pb[4][:, h * 128:(h + 1) * 128], lhsT=ktT[:, h, :], rhs=qtT[:, h, :], start=True, stop=True)
            P.op("dve", "tensor_tensor", ["p4", "const"], ["scm"], out=scm, in0=pb[4][:, :].rearrange("p (a b) -> p a b", a=4),
                 in1=maskT[:].unsqueeze(1).to_broadcast([128, 4, 128]), op=ALU.mult)
            if GLA_CUT <= 5:
                return
            Sa, Sb = Sst[0], Sst[1]
            for h in range(4):
                vh = vv[:, h * 256:(h + 1) * 256]
                vn = "v%d" % (h // 2)
                kvb = 5 + (h % 2)
                ob = 2 + (h // 2)
                oreg = pb[ob][:, (h % 2) * 256:(h % 2 + 1) * 256]
                P.op("pe", "matmul", ["kdec", vn], ["p%d" % kvb], pb[kvb][:, 0:256], lhsT=kdec[0:64, h * 128:(h + 1) * 128], rhs=vv[0:64, h * 256:(h + 1) * 256],
                     start=True, stop=True)
                P.op("dve", "scalar_tensor_tensor", ["Sa", "dec", "p%d" % kvb], ["Sb"], out=Sb[:, h, :], in0=Sa[:, h, :], scalar=dec[:, 2 * h:2 * h + 1],
                     in1=pb[kvb][:, 0:256], op0=ALU.mult, op1=ALU.add)
                P.op("pe", "matmul", ["scm", vn], ["p%d" % ob], oreg, lhsT=scm[:, h, :], rhs=vh, start=True, stop=False)
                P.op("pe", "matmul", ["qtT0", "Sa"], ["p%d" % ob], oreg, lhsT=qtT0[:, h, :], rhs=Sa[:, h, :], start=False, stop=False)
                P.op("pe", "matmul", ["qtT1", "Sb"], ["p%d" % ob], oreg, lhsT=qtT1[:, h, :], rhs=Sb[:, h, :], start=False, stop=True)
                P.op("pe", "matmul", ["kdec", vn], ["p%d" % kvb], pb[kvb][:, 256:512], lhsT=kdec[64:128, h * 128:(h + 1) * 128], rhs=vv[64:128, h * 256:(h + 1) * 256],
                     start=True, stop=True)
                P.op("dve", "scalar_tensor_tensor", ["Sb", "dec", "p%d" % kvb], ["Sa"], out=Sa[:, h, :], in0=Sb[:, h, :], scalar=dec[:, 2 * h + 1:2 * h + 2],
                     in1=pb[kvb][:, 256:512], op0=ALU.mult, op1=ALU.add)
            if GLA_CUT <= 6:
                return
            for h in range(4):
                ob = 2 + (h // 2)
                oreg = pb[ob][:, (h % 2) * 256:(h % 2 + 1) * 256]
                P.op("act", "activation", ["p%d" % ob], ["junk", "oss%d" % h], out=junk[:, 0:256], in_=oreg, func=ACT.Square, accum_out=ss[:, 4 + h:5 + h])
            P.op("dve", "tensor_scalar", ["oss%d" % h for h in range(4)], ["orv"], out=sm[:, 0:4], in0=ss[:, 4:8], scalar1=1.0 / 256, scalar2=EPS, op0=ALU.mult, op1=ALU.add)
            P.op("act", "activation", ["orv"], ["ors"], out=sm[:, 4:8], in_=sm[:, 0:4], func=ACT.Sqrt)
            P.op("dve", "reciprocal", ["ors"], ["orr"], out=sm[:, 8:12], in_=sm[:, 4:8])
            for h in range(4):
                ob = 2 + (h // 2)
                oreg = pb[ob][:, (h % 2) * 256:(h % 2 + 1) * 256]
                P.op("dve", "scalar_tensor_tensor", ["p%d" % ob, "orr", "const"], ["osb"], out=osb[:, h * 256:(h + 1) * 256], in0=oreg, scalar=sm[:, 8 + h:9 + h],
                     in1=gnrow[:], op0=ALU.mult, op1=ALU.mult)
            P.op("act", "activation", ["og0", "og1"], ["ogs"], out=og, in_=og, func=ACT.Silu)
            P.op("dve", "tensor_tensor", ["osb", "ogs"], ["osb"], out=osb, in0=osb, in1=og, op=ALU.mult)
            if GLA_CUT <= 7:
                return
            out_proj("osb", osb, gwo_d, 0, xa, xname, bname="gwo")

        def kv_phase(xa, xname, it):
            cur = it % 2
            hTn = modulate(xa, xname, 4, bf16=BF16_PROJ, need32=not BF16_PROJ)
            dense_block(hTn, kvw_d, 0, 512, 2, bname="kvw")
            kdup = SV(6, 0, 512).rearrange("p (g c d) -> p g c d", g=4, c=2)
            tmp = SV(6, 512, 768).rearrange("p (a g d) -> p a g d", a=8, g=4)
            kp = pb[2][:, 0:256].rearrange("p (g d) -> p g d", g=4)
            P.op("act", "activation", ["p2"], ["v%d" % cur], out=vbd[cur][:], in_=pb[2][:, 256:512], func=ACT.Copy)
            P.op("dve", "tensor_copy", ["p2"], ["kdup"], out=kdup[:, :, 0, :], in_=kp)
            cb = cosT[:, it, :].unsqueeze(1).to_broadcast([128, 4, 8])
            sbb = sinT[:, it, :].unsqueeze(1).to_broadcast([128, 4, 8])
            x1 = kdup[:, :, 0, 0:8]
            x2 = kdup[:, :, 0, 8:16]
            P.op("dve", "tensor_tensor", ["kdup"], ["t0"], out=tmp[:, 0], in0=x1, in1=cb, op=ALU.mult)
            P.op("dve", "tensor_tensor", ["kdup"], ["t1"], out=tmp[:, 1], in0=x2, in1=sbb, op=ALU.mult)
            P.op("dve", "tensor_tensor", ["kdup"], ["t2"], out=tmp[:, 2], in0=x2, in1=cb, op=ALU.mult)
            P.op("dve", "tensor_tensor", ["kdup"], ["t3"], out=tmp[:, 3], in0=x1, in1=sbb, op=ALU.mult)
            P.op("dve", "tensor_tensor", ["t0", "t1"], ["kdup"], out=x1, in0=tmp[:, 0], in1=tmp[:, 1], op=ALU.subtract)
            P.op("dve", "tensor_tensor", ["t2", "t3"], ["kdup"], out=x2, in0=tmp[:, 2], in1=tmp[:, 3], op=ALU.add)
            P.op("dve", "tensor_copy", ["kdup"], ["kdup"], out=kdup[:, :, 1, :], in_=kdup[:, :, 0, :])
            kflat = SV(6, 0, 512)
            for g in range(4):
                P.op("pe", "transpose", ["kdup", "const"], ["p0"], out=pb[0][:, g * 128:(g + 1) * 128], in_=kflat[:, g * 128:(g + 1) * 128], identity=ident[:])
            P.op("act", "activation", ["p0"], ["k%d" % cur], out=kTd[cur][:], in_=pb[0][:, :].rearrange("p (a b) -> p a b", a=4), func=ACT.Copy)

        def swa(xa, xname, it):
            cur = it % 2
            prv = 1 - cur
            hTn = modulate(xa, xname, 6, bf16=BF16_PROJ, need32=not BF16_PROJ)
            q = SV(0, 0, 1024)
            q3 = q.rearrange("p (h d) -> p h d", h=16)
            qT = SV(0, 1024, 2048).rearrange("p (a b) -> p a b", a=8)
            sc = [SV(1, 0, 2048).rearrange("p (h m) -> p h m", h=8), SV(2, 0, 2048).rearrange("p (h m) -> p h m", h=8)]
            pT = [SV(3, 0, 2048).rearrange("p (h c m) -> p h c m", h=8, c=2), SV(4, 0, 2048).rearrange("p (h c m) -> p h c m", h=8, c=2)]
            o = SV(5, 0, 1024)
            tmp = SV(6, 1024, 2048).rearrange("p (a h d) -> p a h d", a=4, h=16)
            for half in range(2):
                dense_block(hTn, swq_d, half * 512, 512, 2 + half, bname="swq")
                if half == 0:
                    P.op("act", "activation", ["p2"], ["q"], out=q[:, 0:512], in_=pb[2][:, :], func=ACT.Copy)
                else:
                    P.op("dve", "tensor_copy", ["p3"], ["q"], out=q[:, 512:1024], in_=pb[3][:, :])
            if SWA_CUT <= 1:
                return
            cb = cosT[:, it, :].unsqueeze(1).to_broadcast([128, 16, 8])
            sbb = sinT[:, it, :].unsqueeze(1).to_broadcast([128, 16, 8])
            x1 = q3[:, :, 0:8]
            x2 = q3[:, :, 8:16]
            tv = SV(6, 1024, 1536).rearrange("p (a h d) -> p a h d", a=4, h=16)
            P.op("dve", "tensor_tensor", ["q"], ["t0"], out=tv[:, 0], in0=x1, in1=cb, op=ALU.mult)
            P.op("dve", "tensor_tensor", ["q"], ["t1"], out=tv[:, 1], in0=x2, in1=sbb, op=ALU.mult)
            P.op("dve", "tensor_tensor", ["q"], ["t2"], out=tv[:, 2], in0=x2, in1=cb, op=ALU.mult)
            P.op("dve", "tensor_tensor", ["q"], ["t3"], out=tv[:, 3], in0=x1, in1=sbb, op=ALU.mult)
            P.op("dve", "tensor_tensor", ["t0", "t1"], ["q"], out=x1, in0=tv[:, 0], in1=tv[:, 1], op=ALU.subtract)
            P.op("dve", "tensor_tensor", ["t2", "t3"], ["q"], out=x2, in0=tv[:, 2], in1=tv[:, 3], op=ALU.add)
            if SWA_CUT <= 2:
                return
            for j in range(8):
                bnk = j // 4
                P.op("pe", "transpose", ["q", "const"], ["p%d" % bnk], out=pb[bnk][:, (j % 4) * 128:(j % 4 + 1) * 128], in_=q[:, j * 128:(j + 1) * 128], identity=ident[:])
            P.op("act", "activation", ["p0"], ["qT0"], out=qT[:, 0:4, :], in_=pb[0][:, :].rearrange("p (a b) -> p a b", a=4), func=ACT.Copy)
            P.op("dve", "tensor_copy", ["p1"], ["qT1"], out=qT[:, 4:8, :], in_=pb[1][:, :].rearrange("p (a b) -> p a b", a=4))
            if SWA_CUT <= 3:
                return
            mk = swam[:, 0 if it == 0 else 1, :]
            for grp in range(4):
                bankE = 4 + 2 * (grp % 2)
                bankO = bankE + 1
                for pj in range(2):
                    j = 2 * grp + pj
                    for hh in range(2):
                        h = 2 * j + hh
                        g = h // 4
                        base = 64 * hh
                        bank = bankE if hh == 0 else bankO
                        col = pj * 256
                        P.op("pe", "matmul", ["qT%d" % (j // 4), "k%d" % prv], ["p%d" % bank], pb[bank][:, col:col + 128],
                             lhsT=qT[base:base + 64, j, :], rhs=kTd[prv][base:base + 64, g, :], start=True, stop=True)
                        P.op("pe", "matmul", ["qT%d" % (j // 4), "k%d" % cur], ["p%d" % bank], pb[bank][:, col + 128:col + 256],
                             lhsT=qT[base:base + 64, j, :], rhs=kTd[cur][base:base + 64, g, :], start=True, stop=True)
                for hh in range(2):
                    bank = bankE if hh == 0 else bankO
                    for pj in range(2):
                        j = 2 * grp + pj
                        h = 2 * j + hh
                        P.op("dve", "scalar_tensor_tensor", ["p%d" % bank, "const"], ["sc%d" % j], out=sc[h // 8][:, h % 8, :],
                             in0=pb[bank][:, pj * 256:(pj + 1) * 256], scalar=0.125, in1=mk, op0=ALU.mult, op1=ALU.add)
            if SWA_CUT <= 4:
                return
            rmax = sm[:, 0:16]
            mm = sm[:, 16:32]
            negm = sm[:, 32:48]
            rs = sm[:, 48:64]
            sk = sm[:, 64:80]
            den = sm[:, 80:96]
            rden = sm[:, 96:112]
            for half in range(2):
                P.op("dve", "tensor_reduce", ["sc%d" % j for j in range(half * 4, half * 4 + 4)], ["rmax%d" % half], out=rmax[:, half * 8:(half + 1) * 8],
                     in_=sc[half], axis=AX.X, op=ALU.max)
            P.op("dve", "tensor_tensor", ["rmax0", "rmax1", "const"], ["mm"], out=mm, in0=rmax, in1=sinkrow[:], op=ALU.max)
            P.op("dve", "tensor_scalar", ["mm"], ["negm"], out=negm, in0=mm, scalar1=-1.0, scalar2=None, op0=ALU.mult)
            P.op("dve", "tensor_tensor", ["mm", "const"], ["sk"], out=sk, in0=sinkrow[:], in1=mm, op=ALU.subtract)
            P.op("act", "activation", ["sk"], ["sk"], out=sk, in_=sk, func=ACT.Exp)
            for h in range(16):
                j = h // 2
                sl = sc[h // 8][:, h % 8, :]
                P.op("act", "activation", ["sc%d" % j, "negm"], ["sc%d" % j, "rs%d" % h], out=sl, in_=sl, func=ACT.Exp, bias=negm[:, h:h + 1], scale=1.0,
                     accum_out=rs[:, h:h + 1])
            P.op("dve", "tensor_tensor", ["rs%d" % h for h in range(16)] + ["sk"], ["den"], out=den, in0=rs, in1=sk, op=ALU.add)
            P.op("dve", "reciprocal", ["den"], ["rden"], out=rden, in_=den)
            if SWA_CUT <= 5:
                return
            for j in range(8):
                bank = j % 4
                for hh in range(2):
                    h = 2 * j + hh
                    for part in range(2):
                        P.op("pe", "transpose", ["sc%d" % j, "const"], ["p%d" % bank], out=pb[bank][:, (hh * 2 + part) * 128:(hh * 2 + part + 1) * 128],
                             in_=sc[h // 8][:, h % 8, part * 128:(part + 1) * 128], identity=ident[:])
                dstv = pT[j // 4][:, 2 * (j % 4):2 * (j % 4) + 2, :, :]
                srcv = pb[bank][:, :].rearrange("p (h c m) -> p h c m", h=2, c=2)
                if j % 2 == 0:
                    P.op("act", "activation", ["p%d" % bank], ["pT%d" % j], out=dstv, in_=srcv, func=ACT.Copy)
                else:
                    P.op("dve", "tensor_copy", ["p%d" % bank], ["pT%d" % j], out=dstv, in_=srcv)
            if SWA_CUT <= 6:
                return
            for h in range(16):
                j = h // 2
                g = h // 4
                bank = 4 + h // 8
                oreg = pb[bank][:, (h % 8) * 64:(h % 8 + 1) * 64]
                P.op("pe", "matmul", ["pT%d" % j, "v%d" % prv], ["p%d" % bank], oreg, lhsT=pT[h // 8][:, h % 8, 0, :], rhs=vbd[prv][:, g * 64:(g + 1) * 64],
                     start=True, stop=False)
                P.op("pe", "matmul", ["pT%d" % j, "v%d" % cur], ["p%d" % bank], oreg, lhsT=pT[h // 8][:, h % 8, 1, :], rhs=vbd[cur][:, g * 64:(g + 1) * 64],
                     start=False, stop=True)
            if SWA_CUT <= 7:
                return
            for b2 in range(2):
                P.op("dve", "tensor_tensor", ["p%d" % (4 + b2), "rden"], ["o"], out=o[:, b2 * 512:(b2 + 1) * 512].rearrange("p (h d) -> p h d", h=8),
                     in0=pb[4 + b2][:, :].rearrange("p (h d) -> p h d", h=8), in1=rden[:, b2 * 8:(b2 + 1) * 8].unsqueeze(2).to_broadcast([128, 8, 64]), op=ALU.mult)
            if SWA_CUT <= 8:
                return
            out_proj("o", o, swo_d, 2, xa, xname, bname="swo")

        def peer(xa, xname, l):
            mi = 2 if l == 0 else 8
            gi = 1 if l == 0 else 3
            hTn = modulate(xa, xname, mi, bf16=True, need32=not (BF16_PROJ and BF16_PQ))
            qT = SV(0, 0, 2048).rearrange("p (g t) -> p g t", g=16)
            s = SV(1, 0, 2048).rearrange("p (g n) -> p g n", g=16)
            sw = SV(2, 0, 2048).rearrange("p (g n) -> p g n", g=16)
            cand = SV(3, 0, 2048).rearrange("p (h a b) -> p h a b", h=8, a=16)
            cw = SV(4, 0, 2048).rearrange("p (h a b) -> p h a b", h=8, a=16)
            v16 = sm[:, 0:256].rearrange("p (g k) -> p g k", g=16)
            c16 = sm[:, 256:384].rearrange("p (h k) -> p h k", h=8)
            dd = sm[:, 384:512].rearrange("p (h k) -> p h k", h=8)
            Z = sm[:, 512:520]
            lnZ = sm[:, 520:528]
            bia = sm[:, 528:536]
            for bi in range(4):
                if BF16_PROJ and BF16_PQ:
                    nm, wb = wload_b("pwq%d" % l, bi * 512, 512)
                    rsrc, rn = hTb, ["hTb"] * KC
                else:
                    nm, wb = wload(pwq_d[l], bi * 512, 512)
                    rsrc, rn = hT, ["hT%d" % kc for kc in range(KC)]
                for gl in range(4):
                    g = bi * 4 + gl
                    for kc in range(KC):
                        P.op("pe", "matmul", [nm, rn[kc]], ["p%d" % bi], pb[bi][:, gl * 128:(gl + 1) * 128], lhsT=wb[:, kc, gl * 128:(gl + 1) * 128], rhs=rsrc[:, kc, :],
                             start=(kc == 0), stop=(kc == KC - 1))
                if bi % 2 == 0:
                    P.op("act", "activation", ["p%d" % bi], ["qT%d" % bi], out=qT[:, bi * 4:bi * 4 + 4, :], in_=pb[bi][:, :].rearrange("p (a b) -> p a b", a=4), func=ACT.Copy)
                else:
                    P.op("dve", "tensor_copy", ["p%d" % bi], ["qT%d" % bi], out=qT[:, bi * 4:bi * 4 + 4, :], in_=pb[bi][:, :].rearrange("p (a b) -> p a b", a=4))
            for g in range(16):
                bank = 4 + g // 4
                P.op("pe", "matmul", ["qT%d" % (g // 4), "const"], ["p%d" % bank], pb[bank][:, (g % 4) * 128:(g % 4 + 1) * 128], lhsT=qT[:, g, :], rhs=skT[:, l, g % 2, :],
                     start=True, stop=True)
            for b4 in range(4):
                bank = 4 + b4
                if b4 % 2 == 0:
                    P.op("act", "activation", ["p%d" % bank], ["s%d" % b4], out=s[:, b4 * 4:b4 * 4 + 4, :], in_=pb[bank][:, :].rearrange("p (a b) -> p a b", a=4), func=ACT.Copy)
                else:
                    P.op("dve", "tensor_copy", ["p%d" % bank], ["s%d" % b4], out=s[:, b4 * 4:b4 * 4 + 4, :], in_=pb[bank][:, :].rearrange("p (a b) -> p a b", a=4))
            snames_all = ["s%d" % b4 for b4 in range(4)]
            for g in range(16):
                P.op("dve", "max", ["s%d" % (g // 4)], ["v16a%d" % g], out=v16[:, g, 0:8], in_=s[:, g, :])
            for g in range(16):
                P.op("dve", "match_replace", ["s%d" % (g // 4), "v16a%d" % g], ["sw%d" % g], out=sw[:, g, :], in_to_replace=v16[:, g, 0:8], in_values=s[:, g, :], imm_value=NEG)
            for g in range(16):
                P.op("dve", "max", ["sw%d" % g], ["v16b%d" % g], out=v16[:, g, 8:16], in_=sw[:, g, :])
            v16n = ["v16a%d" % g for g in range(16)] + ["v16b%d" % g for g in range(16)]
            Ev = sm[:, 536:792].rearrange("p (g k) -> p g k", g=16)
            rZ = sm[:, 520:528]
            E = sw
            swn = ["sw%d" % g for g in range(16)]
            P.op("dve", "tensor_tensor", snames_all + v16n + swn, ["E"], out=E, in0=s, in1=v16[:, :, 0:1].to_broadcast([128, 16, 128]), op=ALU.subtract)
            P.op("act", "activation", ["E"], ["E"], out=E, in_=E, func=ACT.Exp)
            P.op("dve", "tensor_tensor", v16n, ["Ev"], out=Ev, in0=v16, in1=v16[:, :, 0:1].to_broadcast([128, 16, 16]), op=ALU.subtract)
            P.op("act", "activation", ["Ev"], ["Ev"], out=Ev, in_=Ev, func=ACT.Exp)
            E4 = E.rearrange("p (h c) n -> p h c n", c=2)
            Ev4 = Ev.rearrange("p (h c) k -> p h c k", c=2)
            c16n = ["c16a%d" % h for h in range(8)] + ["c16b%d" % h for h in range(8)]
            P.op("dve", "tensor_tensor", ["Ev"], ["cand"], out=cand,
                 in0=Ev4[:, :, 0, :].unsqueeze(3).to_broadcast([128, 8, 16, 16]), in1=Ev4[:, :, 1, :].unsqueeze(2).to_broadcast([128, 8, 16, 16]), op=ALU.mult)
            for h in range(8):
                P.op("dve", "max", ["cand"], ["c16a%d" % h], out=c16[:, h, 0:8], in_=cand[:, h])
            for h in range(8):
                P.op("dve", "match_replace", ["cand", "c16a%d" % h], ["cw%d" % h], out=cw[:, h], in_to_replace=c16[:, h, 0:8], in_values=cand[:, h], imm_value=-1.0)
            for h in range(8):
                P.op("dve", "max", ["cw%d" % h], ["c16b%d" % h], out=c16[:, h, 8:16], in_=cw[:, h])
            P.op("dve", "tensor_reduce", c16n, ["Z"], out=Z, in_=c16, axis=AX.X, op=ALU.add)
            P.op("dve", "reciprocal", ["Z"], ["rZ"], out=rZ, in_=Z)
            P.op("dve", "tensor_tensor", ["E", "rZ"], ["E"], out=E4[:, :, 0, :], in0=E4[:, :, 0, :], in1=rZ.unsqueeze(2).to_broadcast([128, 8, 128]), op=ALU.mult)
            thr = sm[:, 528:536]
            P.op("dve", "scalar_tensor_tensor", c16n + ["rZ"], ["thr"], out=thr, in0=c16[:, :, 15], scalar=float(1.0 - 2.0 ** -20), in1=rZ, op0=ALU.mult, op1=ALU.mult)
            EEs = [SV(i, 0, 1024).rearrange("p (i j) -> p i j", i=8) for i in (5, 6, 0)]
            gtall = SV(7, 0, 2048).bitcast(BF16)
            Gts = [gtall[:, k * 1024:(k + 1) * 1024] for k in range(4)]
            gk = [0]

            def gbuild_head(n, h):
                k = gk[0] % 3
                k4 = gk[0] % 4
                gk[0] += 1
                EE, Gt = EEs[k], Gts[k4]
                een, gtn = "EE%d" % k, "Gt%d" % k4
                eenl = [een + ".%d" % ii for ii in range(8)]
                i0 = n * 8
                if h % 2 == ACT_PAR:
                    for ii in range(8):
                        P.op("act", "activation", ["E", "thr"], [eenl[ii]], out=EE[:, ii, :], in_=E4[:, h, 1, :], func=ACT.Copy, scale=E4[:, h, 0, i0 + ii:i0 + ii + 1])
                else:
                    P.op("dve" if h % 4 == 3 else "pool", "tensor_tensor", ["E", "thr"], eenl, out=EE, in0=E4[:, h, 0, i0:i0 + 8].unsqueeze(2).to_broadcast([128, 8, 128]),
                         in1=E4[:, h, 1, :].unsqueeze(1).to_broadcast([128, 8, 128]), op=ALU.mult)
                P.op("dve", "scalar_tensor_tensor", eenl + ["thr"], [gtn], out=Gt.rearrange("p (i j) -> p i j", i=8), in0=EE, scalar=thr[:, h:h + 1], in1=EE,
                     op0=ALU.is_ge, op1=ALU.mult)
                pend_acc.append((n, h, gtn, Gt))

            pend_acc = []

            def flush_pe_acc():
                for (n, h, gtn, Gt) in pend_acc:
                    for c in range(2):
                        bank = 2 + 2 * (n % 2) + c
                        P.op("pe", "matmul", [gtn, "identb"], ["p%d" % bank], pb[bank][:, :], lhsT=identb[:], rhs=Gt[:, c * 512:(c + 1) * 512], start=(h == 0), stop=(h == 7))
                del pend_acc[:]

            utv = utb_d[l].rearrange("(kc p) e -> p kc e", p=128)
            chain = [None]
            p1b = pb[1][:, :].bitcast(BF16)

            def tail(prev):
                (ebp, GA, GAT, sn, wnv, vblk, first, last) = prev
                half = p1b[:, (ebp % 2) * 512:(ebp % 2 + 1) * 512]
                for j in range(4):
                    P.op("pe", "transpose", [sn + ".GA", "identb"], ["p1"], out=half[:, j * 128:(j + 1) * 128], in_=GA[:, j * 128:(j + 1) * 128], identity=identb[:])
                P.op("act", "activation", ["p1"], [sn + ".GAT"], out=GAT, in_=half.rearrange("p (a b) -> p a b", a=4), func=ACT.Copy)
                for j in range(4):
                    for db in range(2):
                        P.op("pe", "matmul", [sn + ".GAT", wnv], ["p%d" % (6 + db)], pb[6 + db][:, :], lhsT=GAT[:, j, :], rhs=vblk[:, j, db * 512:(db + 1) * 512],
                             start=(first and j == 0), stop=(last and j == 3))

            for h in range(8):
                gbuild_head(0, h)
                if h % 4 == 3:
                    flush_pe_acc()
            for nb in range(32):
                n = nb // 2
                e0 = nb * 512
                i = wsel[0]
                wsel[0] ^= 1
                wnu = "wbuf%du" % i
                wnv = "wbuf%dv" % i
                wbb = wbuf[i][:].rearrange("p a b -> p (a b)").bitcast(BF16)
                ublk = wbb[:, 0:4096].rearrange("p (kc e) -> p kc e", kc=8)
                vblk = wbb[:, 4096:8192].rearrange("p (j d) -> p j d", j=4)
                extra = ["wbuf%d" % i] if nb < 2 else []
                P.dma(wnu, [], [wnu] + extra, [(ublk, utv[:, :, e0:e0 + 512])])
                P.dma(wnv, [], [wnv] + extra, [(vblk, vb_d[l][e0:e0 + 512, :].rearrange("(j p) d -> p j d", p=128))])
                si = 10 + (nb % 2)
                sn = "slot%d" % si
                Ag = SV(si, 0, 512)
                GA = SV(si, 512, 768).bitcast(BF16)
                GAT = SV(si, 1024, 1280).bitcast(BF16).rearrange("p (a b) -> p a b", a=4)
                for kc in range(KC):
                    P.op("pe", "matmul", [wnu, "hTb"], ["p0"], pb[0][:, :], lhsT=hTb[:, kc, :], rhs=ublk[:, kc, :], start=(kc == 0), stop=(kc == KC - 1))
                flush_pe_acc()
                P.op("act", "activation", ["p0"], [sn + ".Ag"], out=Ag, in_=pb[0][:, :], func=ACT.Gelu)
                gbank = 2 + 2 * (n % 2) + (nb % 2)
                P.op("dve", "tensor_tensor", [sn + ".Ag", "p%d" % gbank], [sn + ".GA"], out=GA, in0=pb[gbank][:, :], in1=Ag, op=ALU.mult)
                if chain[0] is not None:
                    tail(chain[0])
                if n < 15:
                    for h in range(4 * (nb % 2), 4 * (nb % 2) + 4):
                        gbuild_head(n + 1, h)
                chain[0] = (nb, GA, GAT, sn, wnv, vblk, nb == 0, nb == 31)
            tail(chain[0])
            for db in range(2):
                P.op("dve", "tensor_tensor", ["p%d" % (6 + db), "gates"], ["ytmp%d" % db], out=slot[8][:, db * 512:(db + 1) * 512],
                     in0=pb[6 + db][:, :], in1=gates[:, gi, db * 512:(db + 1) * 512], op=ALU.mult)
                P.op("pool", "tensor_tensor", ["ytmp%d" % db, xname], [xname], out=xa[:, db * 512:(db + 1) * 512], in0=xa[:, db * 512:(db + 1) * 512],
                     in1=slot[8][:, db * 512:(db + 1) * 512], op=ALU.add)

        def final_norm(xa, xname, it):
            P.op("act", "activation", [xname], ["junk", "ss0"], out=junk[:], in_=xa, func=ACT.Square, accum_out=ss[:, 0:1])
            P.op("dve", "tensor_scalar", ["ss0"], ["ss1"], out=ss[:, 1:2], in0=ss[:, 0:1], scalar1=1.0 / D, scalar2=EPS, op0=ALU.mult, op1=ALU.add)
            P.op("act", "activation", ["ss1"], ["ss2"], out=ss[:, 2:3], in_=ss[:, 1:2], func=ACT.Sqrt)
            P.op("dve", "reciprocal", ["ss2"], ["ss3"], out=ss[:, 3:4], in_=ss[:, 2:3])
            P.op("dve", "scalar_tensor_tensor", [xname, "ss3", "const"], ["xn"], out=xn[:], in0=xa, scalar=ss[:, 3:4], in1=gfrow[:], op0=ALU.mult, op1=ALU.mult)
            P.dma("yout", ["xn"], [], [(y_d[it * 128:(it + 1) * 128, :], xn[:])])

        stages = os.environ.get("YOCO_STAGES", "gla,peer0,kv,swa,peer1").split(",")
        for it in range(NT):
            xa = xt[it % 2][:]
            xname = "xt%d" % (it % 2)
            P.dma(xname, [], [xname], [(xa, x_d[it * 128:(it + 1) * 128, :])])
            if "gla" in stages:
                gla(xa, xname, it)
                P.barrier()
            if "peer0" in stages:
                peer(xa, xname, 0)
                P.barrier()
            if "kv" in stages:
                kv_phase(xa, xname, it)
                P.barrier()
            if "swa" in stages:
                swa(xa, xname, it)
                P.barrier()
            if "peer1" in stages:
                peer(xa, xname, 1)
                P.barrier()
            final_norm(xa, xname, it)
            P.barrier()
        P.barrier()
        P.emit()
        print("yoco build: ops", P.nops, {e: P.cnt[e] for e in P.ENG}, flush=True)
    return nc


def prep_inputs(inp, NT, nb=8):
    f = np.float32
    S = NT * 128
    t = np.arange(128)
    same = (t[:, None] // 64) == (t[None, :] // 64)
    tri = (same & (t[:, None] <= t[None, :])).astype(f) * f(-1.0 / 16)
    blk = same.astype(f) * f(-1.0 / 16)
    csel = ((t[:, None] // 64) == np.arange(2)[None, :]).astype(f) * f(-1.0 / 16)
    maskT = (same & (t[:, None] <= t[None, :])).astype(f)
    qi = np.arange(128)[:, None]
    mi = np.arange(256)[None, :]
    valid = (mi > qi) & (mi <= qi + 128)
    swam = np.stack([np.where(valid & (mi >= 128), 0.0, NEG), np.where(valid, 0.0, NEG)], axis=1).astype(f)
    invf = (np.float32(500000.0) ** (-(np.arange(0, 16, 2, dtype=np.float32)) / np.float32(16))).astype(f)
    invf = np.ascontiguousarray(np.broadcast_to(invf[None, :], (128, 8)))

    def col(v):
        return np.ascontiguousarray(v.reshape(8, 128).T)

    def row(v):
        return np.ascontiguousarray(np.broadcast_to(v[None, :], (128, v.shape[0])))

    mod_b = inp["mod_b"]
    shared = {
        "invf": invf, "ident": np.eye(128, dtype=f), "tri": tri, "blk": blk, "csel": csel, "maskT": maskT, "swam": swam,
        "modw0": np.ascontiguousarray(inp["mod_w"][0]), "modw1": np.ascontiguousarray(inp["mod_w"][1]),
        "modbcol": np.ascontiguousarray(np.stack([mod_b[l].reshape(48, 128).T for l in range(2)], axis=1)),
        "gaterow": np.ascontiguousarray(np.stack([row(mod_b[0, 2048:3072]), row(mod_b[0, 5120:6144]),
                                                   row(mod_b[1, 2048:3072]), row(mod_b[1, 5120:6144])], axis=1)),
        "kvmodw": np.ascontiguousarray(inp["kv_mod_w"]),
        "kvmodbcol": np.ascontiguousarray(inp["kv_mod_b"].reshape(16, 128).T),
        "ngcol": np.ascontiguousarray(np.stack([col(inp["norm_g"][0, 0]), col(inp["norm_g"][0, 1]), col(inp["norm_g"][1, 0]),
                                                col(inp["norm_g"][1, 1]), col(inp["kv_norm_g"])], axis=1)),
        "gfrow": row(inp["final_norm_g"]),
        "w_in": np.ascontiguousarray(inp["gla_w_in"][0]), "wg2": np.ascontiguousarray(inp["gla_w_g2"][0]),
        "bg2row": row(inp["gla_b_g2"][0]), "gnrow": row(inp["gla_norm_g"][0]), "gwo": np.ascontiguousarray(inp["gla_w_out"][0]),
        "kvw": np.ascontiguousarray(inp["kv_w"]), "swq": np.ascontiguousarray(inp["swa_w_q"][0]),
        "sinkrow": row(inp["swa_sinks"][0]), "swo": np.ascontiguousarray(inp["swa_w_out"][0]),
        "pwq0": np.ascontiguousarray(inp["peer_w_q"][0]), "pwq1": np.ascontiguousarray(inp["peer_w_q"][1]),
        "skT": np.ascontiguousarray(np.transpose(inp["peer_subkeys"], (3, 0, 1, 2))),
        "ut0": np.ascontiguousarray(inp["peer_u"][0].T), "ut1": np.ascontiguousarray(inp["peer_u"][1].T),
        "v0": np.ascontiguousarray(inp["peer_v"][0]), "v1": np.ascontiguousarray(inp["peer_v"][1]),
    }
    maps = []
    for b in range(nb):
        m = dict(shared)
        m["x"] = np.ascontiguousarray(inp["x"][b, :S])
        m["ccol"] = col(inp["c"][b])
        m["pos"] = np.ascontiguousarray(inp["positions"][b, :S].reshape(NT, 128).T.astype(np.int32))
        maps.append(m)
    return maps


_NC_CACHE = {}


def kernel(**inputs):
    inputs = {k: np.asarray(v) for k, v in inputs.items()}
    NT = SEQ // 128
    if NT not in _NC_CACHE:
        _NC_CACHE[NT] = build(NT)
    nc = _NC_CACHE[NT]
    maps = prep_inputs(inputs, NT)
    res = run_bass_kernel_spmd(nc, maps, core_ids=list(range(8)))
    out = np.stack([np.asarray(r["y"]) for r in res.results], axis=0)
    return out.astype(np.float32)
```

```python
import os
from contextlib import ExitStack
import numpy as np
import concourse.bass as bass
import concourse.mybir as mybir
from concourse.bass_utils import run_bass_kernel_spmd

F32 = mybir.dt.float32
BF16 = mybir.dt.bfloat16
I32 = mybir.dt.int32
ACT = mybir.ActivationFunctionType
ALU = mybir.AluOpType
AX = mybir.AxisListType

D = 1024
KC = 8
SEQ = 8192
NEXP = 16384
EPS = 1e-6
NEG = -1e30
ACT_PAR = int(os.environ.get('ACT_PAR', '-1'))
BF16_PROJ = os.environ.get('BF16_PROJ', '1') == '1'
BF16_PQ = os.environ.get('BF16_PQ', '1') == '1'
SWA_CUT = int(os.environ.get('SWA_CUT', '99'))
MODEV = os.environ.get('MODEV', 'dve')
GLA_CUT = int(os.environ.get('GLA_CUT', '99'))
PI = float(np.pi)


class Prog:
    ENG = ("pe", "dve", "act", "pool", "sp")

    def __init__(self, nc, es):
        self.nc = nc
        self.es = es
        self.ops = {e: [] for e in self.ENG}
        self.sem = {e: es.enter_context(nc.semaphore("sem_" + e)) for e in self.ENG}
        self.cnt = {e: 0 for e in self.ENG}
        self.dsem = {}
        self.dcnt = {}
        self.lastw = {}
        self.readers = {}
        self.waited = {e: {} for e in self.ENG}
        self.nops = 0

    def _deps(self, eng, r, w):
        deps = {}

        def add(tok):
            if tok is None:
                return
            s, v = tok
            if eng == "pe" and s.name == self.sem["pe"].name:
                return
            if deps.get(s.name, (None, 0))[1] < v:
                deps[s.name] = (s, v)

        for b in r:
            add(self.lastw.get(b))
        for b in w:
            add(self.lastw.get(b))
            for tok in self.readers.get(b, {}).values():
                add(tok)
        out = []
        for key, (s, v) in deps.items():
            if self.waited[eng].get(key, 0) < v:
                self.waited[eng][key] = v
                out.append((s, v))
        return out

    def _commit(self, tok, r, w):
        for b in w:
            self.lastw[b] = tok
            self.readers[b] = {}
        for b in r:
            self.readers.setdefault(b, {})[tok[0].name] = tok

    def op(self, eng, meth, r, w, *a, **k):
        w = list(w) + [b for b in r if len(b) == 2 and b[0] == "p" and b[1].isdigit()]
        waits = self._deps(eng, r, w)
        self.cnt[eng] += 1
        tok = (self.sem[eng], self.cnt[eng])
        self.ops[eng].append((waits, meth, a, k, self.sem[eng], 1))
        self._commit(tok, r, w)
        self.nops += 1

    def dma(self, key, r, w, pairs, eng="sp"):
        if key not in self.dsem:
            self.dsem[key] = self.es.enter_context(self.nc.semaphore("dsem_" + key))
            self.dcnt[key] = 0
        waits = self._deps(eng, r, w)
        for (o, i) in pairs:
            self.dcnt[key] += 16
            self.ops[eng].append((waits, "dma_start", (), dict(out=o, in_=i), self.dsem[key], 16))
            waits = []
            self.nops += 1
        tok = (self.dsem[key], self.dcnt[key])
        self._commit(tok, r, w)

    def barrier(self):
        allw = [(self.sem[e], self.cnt[e]) for e in self.ENG if self.cnt[e] > 0]
        allw += [(s, self.dcnt[k]) for k, s in self.dsem.items()]
        for e in self.ENG:
            ws = []
            for (s, v) in allw:
                if s.name == self.sem[e].name:
                    continue
                if self.waited[e].get(s.name, 0) < v:
                    self.waited[e][s.name] = v
                    ws.append((s, v))
            if ws:
                self.ops[e].append((ws, None, (), {}, None, 0))
        self.lastw = {}
        self.readers = {}

    def emit(self):
        nc = self.nc
        with nc.Block() as block:
            def run(engname):
                def body(engine):
                    for waits, meth, a, k, sem, inc in self.ops[engname]:
                        for (s, v) in waits:
                            engine.wait_ge(s, v)
                        if meth is None:
                            continue
                        getattr(engine, meth)(*a, **k).then_inc(sem, inc)
                return body
            block.tensor(run("pe"))
            block.vector(run("dve"))
            block.scalar(run("act"))
            block.gpsimd(run("pool"))
            block.sync(run("sp"))


def build(NT, dbg=None):
    nc = bass.Bass("TRN2", target_bir_lowering=False)
    S = NT * 128

    def din(name, shape, dt=F32):
        return nc.dram_tensor(name, list(shape), dt, kind="ExternalInput").ap()

    x_d = din("x", [S, D])
    y_d = nc.dram_tensor("y", [S, D], F32, kind="ExternalOutput").ap()
    ccol_d = din("ccol", [128, 8])
    pos_d = din("pos", [128, NT], I32)
    invf_d = din("invf", [128, 8])
    modw_d = [din("modw0", [D, 6 * D]), din("modw1", [D, 6 * D])]
    modbcol_d = din("modbcol", [128, 2, 48])
    gaterow_d = din("gaterow", [128, 4, D])
    kvmodw_d = din("kvmodw", [D, 2 * D])
    kvmodbcol_d = din("kvmodbcol", [128, 16])
    ngcol_d = din("ngcol", [128, 5, 8])
    gfrow_d = din("gfrow", [128, D])
    w_in_d = din("w_in", [D, 3088])
    wg2_d = din("wg2", [16, 512])
    bg2row_d = din("bg2row", [128, 512])
    gnrow_d = din("gnrow", [128, 256])
    gwo_d = din("gwo", [D, D])
    kvw_d = din("kvw", [D, 512])
    swq_d = din("swq", [D, D])
    sinkrow_d = din("sinkrow", [128, 16])
    swo_d = din("swo", [D, D])
    pwq_d = [din("pwq0", [D, 2048]), din("pwq1", [D, 2048])]
    skT_d = din("skT", [128, 2, 2, 128])
    ut_d = [din("ut0", [D, NEXP]), din("ut1", [D, NEXP])]
    v_d = [din("v0", [NEXP, D]), din("v1", [NEXP, D])]
    ident_d = din("ident", [128, 128])
    tri_d = din("tri", [128, 128])
    blk_d = din("blk", [128, 128])
    csel_d = din("csel", [128, 2])
    maskT_d = din("maskT", [128, 128])
    swam_d = din("swam", [128, 2, 256])
    utb_d = [nc.dram_tensor("utb%d" % l, [D, NEXP], BF16, kind="Internal").ap() for l in range(2)]
    vb_d = [nc.dram_tensor("vb%d" % l, [NEXP, D], BF16, kind="Internal").ap() for l in range(2)]
    wb16 = {nm: nc.dram_tensor("b16_" + nm, [D, w], BF16, kind="Internal").ap()
            for nm, w in (("w_in", 3072), ("gwo", D), ("kvw", 512), ("swq", D), ("swo", D), ("pwq0", 2048), ("pwq1", 2048))}
    wsrc = {"w_in": w_in_d, "gwo": gwo_d, "kvw": kvw_d, "swq": swq_d, "swo": swo_d, "pwq0": pwq_d[0], "pwq1": pwq_d[1]}
    dbg_d = None
    if dbg is not None:
        dbg_d = nc.dram_tensor("dbg", [128, 8192], F32, kind="ExternalOutput").ap()

    es = ExitStack()
    with es:
        P = Prog(nc, es)

        def sb(name, shape, dt=F32):
            return es.enter_context(nc.sbuf_tensor("sb_" + name, list(shape), dt))

        def psum(name):
            return es.enter_context(nc.psum_tensor(name, [128, 512], F32))

        ident = sb("ident", [128, 128])
        identb = sb("identb", [128, 128], BF16)
        tri = sb("tri", [128, 128])
        blk = sb("blk", [128, 128])
        csel = sb("csel", [128, 2])
        maskT = sb("maskT", [128, 128])
        swam = sb("swam", [128, 2, 256])
        gates = sb("gates", [128, 4, D])
        gfrow = sb("gfrow", [128, D])
        gnrow = sb("gnrow", [128, 256])
        bg2row = sb("bg2row", [128, 512])
        sinkrow = sb("sinkrow", [128, 16])
        wg2 = sb("wg2", [16, 512])
        skT = sb("skT", [128, 2, 2, 128])
        modc = sb("modc", [128, 10, 8])
        ngcol = sb("ngcol", [128, 5, 8])
        modbcol = sb("modbcol", [128, 2, 48])
        kvmodbcol = sb("kvmodbcol", [128, 16])
        ccol = sb("ccol", [128, 8])
        cact = sb("cact", [128, 8])
        cbc = sb("cbc", [128, 8, 128])
        cosT = sb("cosT", [128, NT, 8])
        sinT = sb("sinT", [128, NT, 8])
        posi = sb("posi", [128, NT], I32)
        invf = sb("invf", [128, 8])
        xt = [sb("xt0", [128, D]), sb("xt1", [128, D])]
        xn = sb("xn", [128, D])
        hT = sb("hT", [128, KC, 128])
        hTb = sb("hTb", [128, KC, 128], BF16)
        junk = sb("junk", [128, D], BF16)
        ss = sb("ss", [128, 8])
        sm = sb("sm", [128, 1024])
        wbuf = [sb("wbuf0", [128, KC, 512]), sb("wbuf1", [128, KC, 512])]
        NSLOT = 12
        slot = [sb("slot%d" % i, [128, 2048]) for i in range(NSLOT)]
        Sst = [sb("Sa", [128, 4, 256]), sb("Sb", [128, 4, 256])]
        kTd = [sb("kTd0", [128, 4, 128]), sb("kTd1", [128, 4, 128])]
        vbd = [sb("vbd0", [128, 256]), sb("vbd1", [128, 256])]
        pb = [psum("p%d" % i) for i in range(8)]

        def SV(i, lo, hi):
            return slot[i][:, lo:hi]

        consts = [(ident, ident_d), (tri, tri_d), (blk, blk_d), (csel, csel_d), (maskT, maskT_d), (swam, swam_d),
                  (gates, gaterow_d), (gfrow, gfrow_d), (gnrow, gnrow_d), (bg2row, bg2row_d), (sinkrow, sinkrow_d),
                  (wg2, wg2_d), (skT, skT_d), (ngcol, ngcol_d), (modbcol, modbcol_d), (kvmodbcol, kvmodbcol_d),
                  (ccol, ccol_d), (posi, pos_d), (invf, invf_d)]
        P.dma("const", [], ["const"], [(t[:], d) for (t, d) in consts])
        P.op("dve", "memset", [], ["Sa"], Sst[0][:], 0.0)
        P.op("dve", "memset", [], ["k1"], kTd[1][:], 0.0)
        P.op("dve", "memset", [], ["v1"], vbd[1][:], 0.0)
        P.op("dve", "tensor_copy", ["const"], ["identb"], out=identb[:], in_=ident[:])
        P.op("act", "activation", ["const"], ["cact"], out=cact[:], in_=ccol[:], func=ACT.Silu)
        P.op("dve", "tensor_copy", ["cact"], ["cbc"], out=cbc[:], in_=cact[:].unsqueeze(2).to_broadcast([128, 8, 128]))
        posf = sm[:, 0:NT]
        ang = slot[0][:, 0:NT * 8].rearrange("p (a b) -> p a b", b=8)
        ang2 = slot[0][:, 1024:1024 + NT * 8].rearrange("p (a b) -> p a b", b=8)
        kf = slot[1][:, 0:NT * 8].rearrange("p (a b) -> p a b", b=8)
        ki = slot[2][:, 0:NT * 8].bitcast(I32).rearrange("p (a b) -> p a b", b=8)
        P.op("dve", "tensor_copy", ["const"], ["posf"], out=posf, in_=posi[:])
        P.op("dve", "tensor_tensor", ["posf", "const"], ["ang"], out=ang, in0=posf.unsqueeze(2).to_broadcast([128, NT, 8]),
             in1=invf[:].unsqueeze(1).to_broadcast([128, NT, 8]), op=ALU.mult)
        P.op("dve", "tensor_scalar_add", ["ang"], ["ang2"], out=ang2, in0=ang, scalar1=PI / 2)
        for (src, nm, dst) in ((ang, "ang", sinT), (ang2, "ang2", cosT)):
            P.op("dve", "tensor_scalar", [nm], ["ki"], out=ki, in0=src, scalar1=float(1.0 / (2 * PI)), scalar2=None, op0=ALU.mult)
            P.op("dve", "tensor_copy", ["ki"], ["kf"], out=kf, in_=ki)
            P.op("dve", "scalar_tensor_tensor", ["kf", nm], [nm], out=src, in0=kf, scalar=float(-2 * PI), in1=src, op0=ALU.mult, op1=ALU.add)
            P.op("dve", "tensor_single_scalar", [nm], ["kf"], out=kf, in_=src, scalar=PI, op=ALU.is_gt)
            P.op("dve", "scalar_tensor_tensor", ["kf", nm], [nm], out=src, in0=kf, scalar=float(-2 * PI), in1=src, op0=ALU.mult, op1=ALU.add)
            P.op("dve", "tensor_single_scalar", [nm], ["kf"], out=kf, in_=src, scalar=-PI, op=ALU.is_lt)
            P.op("dve", "scalar_tensor_tensor", ["kf", nm], [nm], out=src, in0=kf, scalar=float(2 * PI), in1=src, op0=ALU.mult, op1=ALU.add)
            P.op("act", "activation", [nm], [nm + "_out"], out=dst[:], in_=src, func=ACT.Sin)

        wsel = [0]

        def wload(dram_w, c0, ncols):
            i = wsel[0]
            wsel[0] ^= 1
            nm = "wbuf%d" % i
            P.dma(nm, [], [nm], [(wbuf[i][:, :, 0:ncols], dram_w[:, c0:c0 + ncols].rearrange("(kc p) n -> p kc n", p=128))])
            return nm, wbuf[i]

        mcol = sm[:, 64:64 + 96].rearrange("p (a b) -> p a b", b=8)
        for l in range(2):
            for bi in range(12):
                nm, wb = wload(modw_d[l], bi * 512, 512)
                kind = bi // 2
                if kind in (2, 5):
                    gi = l * 2 + (0 if kind == 2 else 1)
                    half = bi % 2
                    for kc in range(KC):
                        P.op("pe", "matmul", [nm, "cbc"], ["p0"], pb[0][:, :], lhsT=cbc[:, kc, :], rhs=wb[:, kc, :], start=(kc == 0), stop=(kc == KC - 1))
                    P.op("dve", "tensor_tensor", ["p0", "const"], ["gates"], out=gates[:, gi, half * 512:(half + 1) * 512], in0=pb[0][:, :],
                         in1=gates[:, gi, half * 512:(half + 1) * 512], op=ALU.add)
                else:
                    for j in range(4):
                        for kc in range(KC):
                            P.op("pe", "matmul", [nm, "cact"], ["p1"], pb[1][:, j:j + 1], lhsT=wb[:, kc, j * 128:(j + 1) * 128], rhs=cact[:, kc:kc + 1],
                                 start=(kc == 0), stop=(kc == KC - 1))
                    vi = {0: 0, 1: 1, 3: 2, 4: 3}[kind]
                    P.op("dve", "tensor_tensor", ["p1", "const"], ["mcol"], out=mcol[:, l * 4 + vi, (bi % 2) * 4:(bi % 2) * 4 + 4], in0=pb[1][:, 0:4],
                         in1=modbcol[:, l, bi * 4:bi * 4 + 4], op=ALU.add)
        for bi in range(4):
            nm, wb = wload(kvmodw_d, bi * 512, 512)
            for j in range(4):
                for kc in range(KC):
                    P.op("pe", "matmul", [nm, "cact"], ["p1"], pb[1][:, j:j + 1], lhsT=wb[:, kc, j * 128:(j + 1) * 128], rhs=cact[:, kc:kc + 1],
                         start=(kc == 0), stop=(kc == KC - 1))
            P.op("dve", "tensor_tensor", ["p1", "const"], ["mcol"], out=mcol[:, 8 + bi // 2, (bi % 2) * 4:(bi % 2) * 4 + 4], in0=pb[1][:, 0:4],
                 in1=kvmodbcol[:, bi * 4:bi * 4 + 4], op=ALU.add)
        for (mi, gi, shi, sci) in ((0, 0, 0, 1), (2, 1, 2, 3), (4, 4, 8, 9), (6, 2, 4, 5), (8, 3, 6, 7)):
            P.op("dve", "scalar_tensor_tensor", ["mcol", "const"], ["modc"], out=modc[:, mi, :], in0=mcol[:, sci, :], scalar=1.0, in1=ngcol[:, gi, :],
                 op0=ALU.add, op1=ALU.mult)
            P.op("dve", "tensor_copy", ["mcol"], ["modc"], out=modc[:, mi + 1, :], in_=mcol[:, shi, :])
        P.barrier()

        cv = [0]

        def convert(src_ap, dst_ap, W=4096):
            i = cv[0] % 3
            cv[0] += 1
            H = W // 2
            P.dma("cin%d" % i, [], ["cin%d" % i], [(slot[2 * i][:, 0:H], src_ap[:, 0:H]), (slot[2 * i + 1][:, 0:H], src_ap[:, H:W])])
            ob = slot[6 + i][:, :].bitcast(BF16)
            eng = ("dve", "act", "pool")[i]
            if eng == "act":
                P.op("act", "activation", ["cin%d" % i], ["cob%d" % i], out=ob[:, 0:H], in_=slot[2 * i][:, 0:H], func=ACT.Copy)
                P.op("act", "activation", ["cin%d" % i], ["cob%d" % i], out=ob[:, H:W], in_=slot[2 * i + 1][:, 0:H], func=ACT.Copy)
            else:
                P.op(eng, "tensor_copy", ["cin%d" % i], ["cob%d" % i], out=ob[:, 0:H], in_=slot[2 * i][:, 0:H])
                P.op(eng, "tensor_copy", ["cin%d" % i], ["cob%d" % i], out=ob[:, H:W], in_=slot[2 * i + 1][:, 0:H])
            P.dma("cout%d" % i, ["cob%d" % i], [], [(dst_ap, ob[:, 0:W])])

        if BF16_PROJ:
            for nm in ("w_in", "gwo", "kvw", "swq", "swo") + (("pwq0", "pwq1") if BF16_PQ else ()):
                W = wb16[nm].shape[1]
                for kc in range(KC):
                    convert(wsrc[nm][kc * 128:(kc + 1) * 128, 0:W], wb16[nm][kc * 128:(kc + 1) * 128, :], W=W)

        for l in range(2):
            for kc in range(KC):
                for e4 in range(4):
                    convert(ut_d[l][kc * 128:(kc + 1) * 128, e4 * 4096:(e4 + 1) * 4096],
                            utb_d[l][kc * 128:(kc + 1) * 128, e4 * 4096:(e4 + 1) * 4096])
            for r in range(32):
                convert(v_d[l][r * 512:(r + 1) * 512, :].rearrange("(p j) d -> p (j d)", j=4),
                        vb_d[l][r * 512:(r + 1) * 512, :].rearrange("(p j) d -> p (j d)", j=4))
        P.barrier()

        def modulate(xa, xname, mi, bf16=False, need32=True):
            P.op("act", "activation", [xname], ["junk", "ss0"], out=junk[:], in_=xa, func=ACT.Square, accum_out=ss[:, 0:1])
            P.op("dve", "tensor_scalar", ["ss0"], ["ss1"], out=ss[:, 1:2], in0=ss[:, 0:1], scalar1=1.0 / D, scalar2=EPS, op0=ALU.mult, op1=ALU.add)
            P.op("act", "activation", ["ss1"], ["ss2"], out=ss[:, 2:3], in_=ss[:, 1:2], func=ACT.Sqrt)
            P.op("dve", "reciprocal", ["ss2"], ["ss3"], out=ss[:, 3:4], in_=ss[:, 2:3])
            P.op("dve", "tensor_scalar", [xname, "ss3"], ["xn"], out=xn[:], in0=xa, scalar1=ss[:, 3:4], scalar2=None, op0=ALU.mult)
            for kc in range(KC):
                bnk = kc // 4
                P.op("pe", "transpose", ["xn", "const"], ["p%d" % bnk], out=pb[bnk][:, (kc % 4) * 128:(kc % 4 + 1) * 128],
                     in_=xn[:, kc * 128:(kc + 1) * 128], identity=ident[:])
            direct = bf16 and not need32
            for kc in range(KC):
                bnk = kc // 4
                src = pb[bnk][:, (kc % 4) * 128:(kc % 4 + 1) * 128]
                dst = hTb[:, kc, :] if direct else hT[:, kc, :]
                dn = "hTb" if direct else "hT%d" % kc
                P.op("dve", "tensor_scalar", ["p%d" % bnk, "modc"], [dn], out=dst, in0=src,
                     scalar1=modc[:, mi, kc:kc + 1], scalar2=modc[:, mi + 1, kc:kc + 1], op0=ALU.mult, op1=ALU.add)
            if bf16 and need32:
                P.op("act", "activation", ["hT%d" % kc for kc in range(4)], ["hTb"], out=hTb[:, 0:4, :], in_=hT[:, 0:4, :], func=ACT.Copy)
                P.op("act", "activation", ["hT%d" % kc for kc in range(4, 8)], ["hTb"], out=hTb[:, 4:8, :], in_=hT[:, 4:8, :], func=ACT.Copy)
            return ["hT%d" % kc for kc in range(KC)]

        def wload_b(wname, c0, ncols):
            i = wsel[0]
            wsel[0] ^= 1
            nm = "wbuf%d" % i
            wv = wbuf[i][:].rearrange("p a b -> p (a b)").bitcast(BF16)[:, 0:4096].rearrange("p (kc n) -> p kc n", kc=8)
            P.dma(nm, [], [nm], [(wv[:, :, 0:ncols], wb16[wname][:, c0:c0 + ncols].rearrange("(kc p) n -> p kc n", p=128))])
            return nm, wv

        def dense_block(hTn, dram_w, c0, ncols, bank, bname=None):
            if BF16_PROJ and bname is not None:
                nm, wb = wload_b(bname, c0, ncols)
                for kc in range(KC):
                    P.op("pe", "matmul", [nm, "hTb"], ["p%d" % bank], pb[bank][:, 0:ncols], lhsT=hTb[:, kc, :], rhs=wb[:, kc, 0:ncols],
                         start=(kc == 0), stop=(kc == KC - 1))
                return
            nm, wb = wload(dram_w, c0, ncols)
            for kc in range(KC):
                P.op("pe", "matmul", [nm, "hT%d" % kc], ["p%d" % bank], pb[bank][:, 0:ncols], lhsT=hT[:, kc, :], rhs=wb[:, kc, 0:ncols],
                     start=(kc == 0), stop=(kc == KC - 1))

        def out_proj(onm, oap, dram_w, gi, xa, xname, bname=None):
            useb = BF16_PROJ and bname is not None
            if useb:
                oT = SV(5, 1024, 1536).bitcast(BF16).rearrange("p (a b) -> p a b", a=8)
            else:
                oT = SV(5, 1024, 2048).rearrange("p (a b) -> p a b", a=8)
            for kc in range(KC):
                bnk = kc // 4
                P.op("pe", "transpose", [onm, "const"], ["p%d" % bnk], out=pb[bnk][:, (kc % 4) * 128:(kc % 4 + 1) * 128],
                     in_=oap[:, kc * 128:(kc + 1) * 128], identity=ident[:])
            P.op("act", "activation", ["p0"], ["oT0"], out=oT[:, 0:4, :], in_=pb[0][:, :].rearrange("p (a b) -> p a b", a=4), func=ACT.Copy)
            P.op("dve", "tensor_copy", ["p1"], ["oT1"], out=oT[:, 4:8, :], in_=pb[1][:, :].rearrange("p (a b) -> p a b", a=4))
            for half in range(2):
                if useb:
                    nm, wb = wload_b(bname, half * 512, 512)
                else:
                    nm, wb = wload(dram_w, half * 512, 512)
                bank = 2 + half
                for kc in range(KC):
                    P.op("pe", "matmul", [nm, "oT%d" % (kc // 4)], ["p%d" % bank], pb[bank][:, :], lhsT=oT[:, kc, :], rhs=wb[:, kc, 0:512],
                         start=(kc == 0), stop=(kc == KC - 1))
                P.op("dve", "tensor_tensor", ["p%d" % bank, "gates"], ["ytmp%d" % half], out=sm[:, half * 512:(half + 1) * 512], in0=pb[bank][:, :],
                     in1=gates[:, gi, half * 512:(half + 1) * 512], op=ALU.mult)
                P.op("pool", "tensor_tensor", ["ytmp%d" % half, xname], [xname], out=xa[:, half * 512:(half + 1) * 512],
                     in0=xa[:, half * 512:(half + 1) * 512], in1=sm[:, half * 512:(half + 1) * 512], op=ALU.add)

        def gla(xa, xname, it):
            hTn = modulate(xa, xname, 0, bf16=BF16_PROJ)
            if GLA_CUT <= 0:
                return
            qk = SV(0, 0, 1024)
            la = SV(0, 1024, 1536)
            zb = SV(0, 1536, 2048)
            vv = SV(1, 0, 1024)
            og = SV(1, 1024, 2048)
            eb = SV(2, 0, 512)
            enb = SV(2, 512, 1024)
            ebl = SV(2, 1024, 1536)
            scm = SV(2, 1536, 2048).rearrange("p (a b) -> p a b", a=4)
            qt = SV(3, 0, 512)
            kt = SV(3, 512, 1024)
            kdec = SV(3, 1024, 1536)
            glT = slot[3][0:16, 1536:1664]
            dec = SV(3, 1664, 1672)
            qtT0 = SV(4, 0, 512).rearrange("p (a b) -> p a b", a=4)
            qtT1 = SV(4, 512, 1024).rearrange("p (a b) -> p a b", a=4)
            ktT = SV(4, 1024, 1536).rearrange("p (a b) -> p a b", a=4)
            qtT = SV(4, 1536, 2048).rearrange("p (a b) -> p a b", a=4)
            osb = SV(5, 0, 1024)
            dsts = [(qk[:, 0:512], "q"), (qk[:, 512:1024], "k"), (vv[:, 0:512], "v0"), (vv[:, 512:1024], "v1"),
                    (og[:, 0:512], "og0"), (og[:, 512:1024], "og1")]
            for bi, (dst, dn) in enumerate(dsts):
                bank = 2 + (bi % 2)
                dense_block(hTn, w_in_d, bi * 512, 512, bank, bname="w_in")
                if bi % 2 == 0:
                    P.op("act", "activation", ["p%d" % bank], [dn], out=dst, in_=pb[bank][:, :], func=ACT.Copy)
                else:
                    P.op("dve", "tensor_copy", ["p%d" % bank], [dn], out=dst, in_=pb[bank][:, :])
            if GLA_CUT <= 1:
                return
            nm, wb = wload(w_in_d, 3072, 16)
            for kc in range(KC):
                P.op("pe", "matmul", [nm, "hT%d" % kc], ["p4"], pb[4][0:16, 0:128], lhsT=wb[:, kc, 0:16], rhs=hT[:, kc, :], start=(kc == 0), stop=(kc == KC - 1))
            P.op("dve", "tensor_copy", ["p4"], ["glT"], out=glT, in_=pb[4][0:16, 0:128])
            P.op("pe", "matmul", ["glT", "const"], ["p5"], pb[5][:, :], lhsT=glT, rhs=wg2[:, :], start=True, stop=True)
            P.op("dve", "tensor_tensor", ["p5", "const"], ["zb"], out=zb, in0=pb[5][:, :], in1=bg2row[:], op=ALU.add)
            P.op("act", "activation", ["zb"], ["zb"], out=zb, in_=zb, func=ACT.Exp, scale=-1.0)
            P.op("act", "activation", ["zb"], ["la"], out=la, in_=zb, func=ACT.Ln, bias=1.0)
            if GLA_CUT <= 2:
                return
            P.op("pe", "matmul", ["la", "const"], ["p4"], pb[4][:, :], lhsT=tri[:], rhs=la, start=True, stop=True)
            P.op("pe", "matmul", ["la", "const"], ["p5"], pb[5][:, :], lhsT=blk[:], rhs=la, start=True, stop=True)
            for h in range(4):
                P.op("pe", "matmul", ["la", "const"], ["p6"], pb[6][:, 2 * h:2 * h + 2], lhsT=la[:, h * 128:(h + 1) * 128], rhs=csel[:], start=True, stop=True)
            P.op("act", "activation", ["p4"], ["eb"], out=eb, in_=pb[4][:, :], func=ACT.Exp)
            P.op("act", "activation", ["p4"], ["enb"], out=enb, in_=pb[4][:, :], func=ACT.Exp, scale=-1.0)
            P.op("act", "activation", ["p5"], ["ebl"], out=ebl, in_=pb[5][:, :], func=ACT.Exp)
            P.op("act", "activation", ["p6"], ["dec"], out=dec, in_=pb[6][:, 0:8], func=ACT.Exp)
            P.op("dve", "scalar_tensor_tensor", ["q", "eb"], ["qt"], out=qt, in0=qk[:, 0:512], scalar=float(128 ** -0.5), in1=eb, op0=ALU.mult, op1=ALU.mult)
            P.op("dve", "tensor_tensor", ["k", "enb"], ["kt"], out=kt, in0=qk[:, 512:1024], in1=enb, op=ALU.mult)
            P.op("pool", "tensor_tensor", ["kt", "ebl"], ["kdec"], out=kdec, in0=kt, in1=ebl, op=ALU.mult)
            if GLA_CUT <= 3:
                return
            for h in range(4):
                P.op("pe", "transpose", ["qt", "const"], ["p0"], out=pb[0][:, h * 128:(h + 1) * 128], in_=qt[:, h * 128:(h + 1) * 128], identity=ident[:])
            for h in range(4):
                P.op("pe", "transpose", ["kt", "const"], ["p1"], out=pb[1][:, h * 128:(h + 1) * 128], in_=kt[:, h * 128:(h + 1) * 128], identity=ident[:])
            p0v = pb[0][:, :].rearrange("p (a b) -> p a b", a=4)
            P.op("act", "activation", ["p0"], ["qtT"], out=qtT, in_=p0v, func=ACT.Copy)
            P.op("pool", "memset", [], ["qtT0", "qtT1"], slot[4][:, 0:1024], 0.0)
            P.op("dve", "tensor_copy", ["p0"], ["qtT0"], out=qtT0[:, :, 0:64], in_=p0v[:, :, 0:64])
            P.op("dve", "tensor_copy", ["p0"], ["qtT1"], out=qtT1[:, :, 64:128], in_=p0v[:, :, 64:128])
            P.op("act", "activation", ["p1"], ["ktT"], out=ktT, in_=pb[1][:, :].rearrange("p (a b) -> p a b", a=4), func=ACT.Copy)
            if GLA_CUT <= 4:
                return
            for h in range(4):
                P.op("pe", "matmul", ["ktT", "qtT"], ["p4"], pb[4][:, h * 128:(h + 1) * 128], lhsT=ktT[:, h, :], rhs=qtT[:, h, :], start=True, stop=True)
            P.op("dve", "tensor_tensor", ["p4", "const"], ["scm"], out=scm, in0=pb[4][:, :].rearrange("p (a b) -> p a b", a=4),
                 in1=maskT[:].unsqueeze(1).to_broadcast([128, 4, 128]), op=ALU.mult)
            if GLA_CUT <= 5:
                return
            Sa, Sb = Sst[0], Sst[1]
            for h in range(4):
                vh = vv[:, h * 256:(h + 1) * 256]
                vn = "v%d" % (h // 2)
                kvb = 5 + (h % 2)
                ob = 2 + (h // 2)
                oreg = pb[ob][:, (h % 2) * 256:(h % 2 + 1) * 256]
                P.op("pe", "matmul", ["kdec", vn], ["p%d" % kvb], pb[kvb][:, 0:256], lhsT=kdec[0:64, h * 128:(h + 1) * 128], rhs=vv[0:64, h * 256:(h + 1) * 256],
                     start=True, stop=True)
                P.op("dve", "scalar_tensor_tensor", ["Sa", "dec", "p%d" % kvb], ["Sb"], out=Sb[:, h, :], in0=Sa[:, h, :], scalar=dec[:, 2 * h:2 * h + 1],
                     in1=pb[kvb][:, 0:256], op0=ALU.mult, op1=ALU.add)
                P.op("pe", "matmul", ["scm", vn], ["p%d" % ob], oreg, lhsT=scm[:, h, :], rhs=vh, start=True, stop=False)
                P.op("pe", "matmul", ["qtT0", "Sa"], ["p%d" % ob], oreg, lhsT=qtT0[:, h, :], rhs=Sa[:, h, :], start=False, stop=False)
                P.op("pe", "matmul", ["qtT1", "Sb"], ["p%d" % ob], oreg, lhsT=qtT1[:, h, :], rhs=Sb[:, h, :], start=False, stop=True)
                P.op("pe", "matmul", ["kdec", vn], ["p%d" % kvb], pb[kvb][:, 256:512], lhsT=kdec[64:128, h * 128:(h + 1) * 128], rhs=vv[64:128, h * 256:(h + 1) * 256],
                     start=True, stop=True)
                P.op("dve", "scalar_tensor_tensor", ["Sb", "dec", "p%d" % kvb], ["Sa"], out=Sa[:, h, :], in0=Sb[:, h, :], scalar=dec[:, 2 * h + 1:2 * h + 2],
                     in1=pb[kvb][:, 256:512], op0=ALU.mult, op1=ALU.add)
            if GLA_CUT <= 6:
                return
            for h in range(4):
                ob = 2 + (h // 2)
                oreg = pb[ob][:, (h % 2) * 256:(h % 2 + 1) * 256]
                P.op("act", "activation", ["p%d" % ob], ["junk", "oss%d" % h], out=junk[:, 0:256], in_=oreg, func=ACT.Square, accum_out=ss[:, 4 + h:5 + h])
            P.op("dve", "tensor_scalar", ["oss%d" % h for h in range(4)], ["orv"], out=sm[:, 0:4], in0=ss[:, 4:8], scalar1=1.0 / 256, scalar2=EPS, op0=ALU.mult, op1=ALU.add)
            P.op("act", "activation", ["orv"], ["ors"], out=sm[:, 4:8], in_=sm[:, 0:4], func=ACT.Sqrt)
            P.op("dve", "reciprocal", ["ors"], ["orr"], out=sm[:, 8:12], in_=sm[:, 4:8])
            for h in range(4):
                ob = 2 + (h // 2)
                oreg = pb[ob][:, (h % 2) * 256:(h % 2 + 1) * 256]
                P.op("dve", "scalar_tensor_tensor", ["p%d" % ob, "orr", "const"], ["osb"], out=osb[:, h * 256:(h + 1) * 256], in0=oreg, scalar=sm[:, 8 + h:9 + h],
                     in1=gnrow[:], op0=ALU.mult, op1=ALU.mult)
            P.op("act", "activation", ["og0", "og1"], ["ogs"], out=og, in_=og, func=ACT.Silu)
            P.op("dve", "tensor_tensor", ["osb", "ogs"], ["osb"], out=osb, in0=osb, in1=og, op=ALU.mult)
            if GLA_CUT <= 7:
                return
            out_proj("osb", osb, gwo_d, 0, xa, xname, bname="gwo")

        def kv_phase(xa, xname, it):
            cur = it % 2
            hTn = modulate(xa, xname, 4, bf16=BF16_PROJ, need32=not BF16_PROJ)
            dense_block(hTn, kvw_d, 0, 512, 2, bname="kvw")
            kdup = SV(6, 0, 512).rearrange("p (g c d) -> p g c d", g=4, c=2)
            tmp = SV(6, 512, 768).rearrange("p (a g d) -> p a g d", a=8, g=4)
            kp = pb[2][:, 0:256].rearrange("p (g d) -> p g d", g=4)
            P.op("act", "activation", ["p2"], ["v%d" % cur], out=vbd[cur][:], in_=pb[2][:, 256:512], func=ACT.Copy)
            P.op("dve", "tensor_copy", ["p2"], ["kdup"], out=kdup[:, :, 0, :], in_=kp)
            cb = cosT[:, it, :].unsqueeze(1).to_broadcast([128, 4, 8])
            sbb = sinT[:, it, :].unsqueeze(1).to_broadcast([128, 4, 8])
            x1 = kdup[:, :, 0, 0:8]
            x2 = kdup[:, :, 0, 8:16]
            P.op("dve", "tensor_tensor", ["kdup"], ["t0"], out=tmp[:, 0], in0=x1, in1=cb, op=ALU.mult)
            P.op("dve", "tensor_tensor", ["kdup"], ["t1"], out=tmp[:, 1], in0=x2, in1=sbb, op=ALU.mult)
            P.op("dve", "tensor_tensor", ["kdup"], ["t2"], out=tmp[:, 2], in0=x2, in1=cb, op=ALU.mult)
            P.op("dve", "tensor_tensor", ["kdup"], ["t3"], out=tmp[:, 3], in0=x1, in1=sbb, op=ALU.mult)
            P.op("dve", "tensor_tensor", ["t0", "t1"], ["kdup"], out=x1, in0=tmp[:, 0], in1=tmp[:, 1], op=ALU.subtract)
            P.op("dve", "tensor_tensor", ["t2", "t3"], ["kdup"], out=x2, in0=tmp[:, 2], in1=tmp[:, 3], op=ALU.add)
            P.op("dve", "tensor_copy", ["kdup"], ["kdup"], out=kdup[:, :, 1, :], in_=kdup[:, :, 0, :])
            kflat = SV(6, 0, 512)
            for g in range(4):
                P.op("pe", "transpose", ["kdup", "const"], ["p0"], out=pb[0][:, g * 128:(g + 1) * 128], in_=kflat[:, g * 128:(g + 1) * 128], identity=ident[:])
            P.op("act", "activation", ["p0"], ["k%d" % cur], out=kTd[cur][:], in_=pb[0][:, :].rearrange("p (a b) -> p a b", a=4), func=ACT.Copy)

        def swa(xa, xname, it):
            cur = it % 2
            prv = 1 - cur
            hTn = modulate(xa, xname, 6, bf16=BF16_PROJ, need32=not BF16_PROJ)
            q = SV(0, 0, 1024)
            q3 = q.rearrange("p (h d) -> p h d", h=16)
            qT = SV(0, 1024, 2048).rearrange("p (a b) -> p a b", a=8)
            sc = [SV(1, 0, 2048).rearrange("p (h m) -> p h m", h=8), SV(2, 0, 2048).rearrange("p (h m) -> p h m", h=8)]
            pT = [SV(3, 0, 2048).rearrange("p (h c m) -> p h c m", h=8, c=2), SV(4, 0, 2048).rearrange("p (h c m) -> p h c m", h=8, c=2)]
            o = SV(5, 0, 1024)
            tmp = SV(6, 1024, 2048).rearrange("p (a h d) -> p a h d", a=4, h=16)
            for half in range(2):
                dense_block(hTn, swq_d, half * 512, 512, 2 + half, bname="swq")
                if half == 0:
                    P.op("act", "activation", ["p2"], ["q"], out=q[:, 0:512], in_=pb[2][:, :], func=ACT.Copy)
                else:
                    P.op("dve", "tensor_copy", ["p3"], ["q"], out=q[:, 512:1024], in_=pb[3][:, :])
            if SWA_CUT <= 1:
                return
            cb = cosT[:, it, :].unsqueeze(1).to_broadcast([128, 16, 8])
            sbb = sinT[:, it, :].unsqueeze(1).to_broadcast([128, 16, 8])
            x1 = q3[:, :, 0:8]
            x2 = q3[:, :, 8:16]
            tv = SV(6, 1024, 1536).rearrange("p (a h d) -> p a h d", a=4, h=16)
            P.op("dve", "tensor_tensor", ["q"], ["t0"], out=tv[:, 0], in0=x1, in1=cb, op=ALU.mult)
            P.op("dve", "tensor_tensor", ["q"], ["t1"], out=tv[:, 1], in0=x2, in1=sbb, op=ALU.mult)
            P.op("dve", "tensor_tensor", ["q"], ["t2"], out=tv[:, 2], in0=x2, in1=cb, op=ALU.mult)
            P.op("dve", "tensor_tensor", ["q"], ["t3"], out=tv[:, 3], in0=x1, in1=sbb, op=ALU.mult)
            P.op("dve", "tensor_tensor", ["t0", "t1"], ["q"], out=x1, in0=tv[:, 0], in1=tv[:, 1], op=ALU.subtract)
            P.op("dve", "tensor_tensor", ["t2", "t3"], ["q"], out=x2, in0=tv[:, 2], in1=tv[:, 3], op=ALU.add)
            if SWA_CUT <= 2:
                return
            for j in range(8):
                bnk = j // 4
                P.op("pe", "transpose", ["q", "const"], ["p%d" % bnk], out=pb[bnk][:, (j % 4) * 128:(j % 4 + 1) * 128], in_=q[:, j * 128:(j + 1) * 128], identity=ident[:])
            P.op("act", "activation", ["p0"], ["qT0"], out=qT[:, 0:4, :], in_=pb[0][:, :].rearrange("p (a b) -> p a b", a=4), func=ACT.Copy)
            P.op("dve", "tensor_copy", ["p1"], ["qT1"], out=qT[:, 4:8, :], in_=pb[1][:, :].rearrange("p (a b) -> p a b", a=4))
            if SWA_CUT <= 3:
                return
            mk = swam[:, 0 if it == 0 else 1, :]
            for grp in range(4):
                bankE = 4 + 2 * (grp % 2)
                bankO = bankE + 1
                for pj in range(2):
                    j = 2 * grp + pj
                    for hh in range(2):
                        h = 2 * j + hh
                        g = h // 4
                        base = 64 * hh
                        bank = bankE if hh == 0 else bankO
                        col = pj * 256
                        P.op("pe", "matmul", ["qT%d" % (j // 4), "k%d" % prv], ["p%d" % bank], pb[bank][:, col:col + 128],
                             lhsT=qT[base:base + 64, j, :], rhs=kTd[prv][base:base + 64, g, :], start=True, stop=True)
                        P.op("pe", "matmul", ["qT%d" % (j // 4), "k%d" % cur], ["p%d" % bank], pb[bank][:, col + 128:col + 256],
                             lhsT=qT[base:base + 64, j, :], rhs=kTd[cur][base:base + 64, g, :], start=True, stop=True)
                for hh in range(2):
                    bank = bankE if hh == 0 else bankO
                    for pj in range(2):
                        j = 2 * grp + pj
                        h = 2 * j + hh
                        P.op("dve", "scalar_tensor_tensor", ["p%d" % bank, "const"], ["sc%d" % j], out=sc[h // 8][:, h % 8, :],
                             in0=pb[bank][:, pj * 256:(pj + 1) * 256], scalar=0.125, in1=mk, op0=ALU.mult, op1=ALU.add)
            if SWA_CUT <= 4:
                return
            rmax = sm[:, 0:16]
            mm = sm[:, 16:32]
            negm = sm[:, 32:48]
            rs = sm[:, 48:64]
            sk = sm[:, 64:80]
            den = sm[:, 80:96]
            rden = sm[:, 96:112]
            for half in range(2):
                P.op("dve", "tensor_reduce", ["sc%d" % j for j in range(half * 4, half * 4 + 4)], ["rmax%d" % half], out=rmax[:, half * 8:(half + 1) * 8],
                     in_=sc[half], axis=AX.X, op=ALU.max)
            P.op("dve", "tensor_tensor", ["rmax0", "rmax1", "const"], ["mm"], out=mm, in0=rmax, in1=sinkrow[:], op=ALU.max)
            P.op("dve", "tensor_scalar", ["mm"], ["negm"], out=negm, in0=mm, scalar1=-1.0, scalar2=None, op0=ALU.mult)
            P.op("dve", "tensor_tensor", ["mm", "const"], ["sk"], out=sk, in0=sinkrow[:], in1=mm, op=ALU.subtract)
            P.op("act", "activation", ["sk"], ["sk"], out=sk, in_=sk, func=ACT.Exp)
            for h in range(16):
                j = h // 2
                sl = sc[h // 8][:, h % 8, :]
                P.op("act", "activation", ["sc%d" % j, "negm"], ["sc%d" % j, "rs%d" % h], out=sl, in_=sl, func=ACT.Exp, bias=negm[:, h:h + 1], scale=1.0,
                     accum_out=rs[:, h:h + 1])
            P.op("dve", "tensor_tensor", ["rs%d" % h for h in range(16)] + ["sk"], ["den"], out=den, in0=rs, in1=sk, op=ALU.add)
            P.op("dve", "reciprocal", ["den"], ["rden"], out=rden, in_=den)
            if SWA_CUT <= 5:
                return
            for j in range(8):
                bank = j % 4
                for hh in range(2):
                    h = 2 * j + hh
                    for part in range(2):
                        P.op("pe", "transpose", ["sc%d" % j, "const"], ["p%d" % bank], out=pb[bank][:, (hh * 2 + part) * 128:(hh * 2 + part + 1) * 128],
                             in_=sc[h // 8][:, h % 8, part * 128:(part + 1) * 128], identity=ident[:])
                dstv = pT[j // 4][:, 2 * (j % 4):2 * (j % 4) + 2, :, :]
                srcv = pb[bank][:, :].rearrange("p (h c m) -> p h c m", h=2, c=2)
                if j % 2 == 0:
                    P.op("act", "activation", ["p%d" % bank], ["pT%d" % j], out=dstv, in_=srcv, func=ACT.Copy)
                else:
                    P.op("dve", "tensor_copy", ["p%d" % bank], ["pT%d" % j], out=dstv, in_=srcv)
            if SWA_CUT <= 6:
                return
            for h in range(16):
                j = h // 2
                g = h // 4
                bank = 4 + h // 8
                oreg = pb[bank][:, (h % 8) * 64:(h % 8 + 1) * 64]
                P.op("pe", "matmul", ["pT%d" % j, "v%d" % prv], ["p%d" % bank], oreg, lhsT=pT[h // 8][:, h % 8, 0, :], rhs=vbd[prv][:, g * 64:(g + 1) * 64],
                     start=True, stop=False)
                P.op("pe", "matmul", ["pT%d" % j, "v%d" % cur], ["p%d" % bank], oreg, lhsT=pT[h // 8][:, h % 8, 1, :], rhs=vbd[cur][:, g * 64:(g + 1) * 64],
                     start=False, stop=True)
            if SWA_CUT <= 7:
                return
            for b2 in range(2):
                P.op("dve", "tensor_tensor", ["p%d" % (4 + b2), "rden"], ["o"], out=o[:, b2 * 512:(b2 + 1) * 512].rearrange("p (h d) -> p h d", h=8),
                     in0=pb[4 + b2][:, :].rearrange("p (h d) -> p h d", h=8), in1=rden[:, b2 * 8:(b2 + 1) * 8].unsqueeze(2).to_broadcast([128, 8, 64]), op=ALU.mult)
            if SWA_CUT <= 8:
                return
            out_proj("o", o, swo_d, 2, xa, xname, bname="swo")

        def peer(xa, xname, l):
            mi = 2 if l == 0 else 8
            gi = 1 if l == 0 else 3
            hTn = modulate(xa, xname, mi, bf16=True, need32=not (BF16_PROJ and BF16_PQ))
            qT = SV(0, 0, 2048).rearrange("p (g t) -> p g t", g=16)
            s = SV(1, 0, 2048).rearrange("p (g n) -> p g n", g=16)
            sw = SV(2, 0, 2048).rearrange("p (g n) -> p g n", g=16)
            cand = SV(3, 0, 2048).rearrange("p (h a b) -> p h a b", h=8, a=16)
            cw = SV(4, 0, 2048).rearrange("p (h a b) -> p h a b", h=8, a=16)
            v16 = sm[:, 0:256].rearrange("p (g k) -> p g k", g=16)
            c16 = sm[:, 256:384].rearrange("p (h k) -> p h k", h=8)
            dd = sm[:, 384:512].rearrange("p (h k) -> p h k", h=8)
            Z = sm[:, 512:520]
            lnZ = sm[:, 520:528]
            bia = sm[:, 528:536]
            for bi in range(4):
                if BF16_PROJ and BF16_PQ:
                    nm, wb = wload_b("pwq%d" % l, bi * 512, 512)
                    rsrc, rn = hTb, ["hTb"] * KC
                else:
                    nm, wb = wload(pwq_d[l], bi * 512, 512)
                    rsrc, rn = hT, ["hT%d" % kc for kc in range(KC)]
                for gl in range(4):
                    g = bi * 4 + gl
                    for kc in range(KC):
                        P.op("pe", "matmul", [nm, rn[kc]], ["p%d" % bi], pb[bi][:, gl * 128:(gl + 1) * 128], lhsT=wb[:, kc, gl * 128:(gl + 1) * 128], rhs=rsrc[:, kc, :],
                             start=(kc == 0), stop=(kc == KC - 1))
                if bi % 2 == 0:
                    P.op("act", "activation", ["p%d" % bi], ["qT%d" % bi], out=qT[:, bi * 4:bi * 4 + 4, :], in_=pb[bi][:, :].rearrange("p (a b) -> p a b", a=4), func=ACT.Copy)
                else:
                    P.op("dve", "tensor_copy", ["p%d" % bi], ["qT%d" % bi], out=qT[:, bi * 4:bi * 4 + 4, :], in_=pb[bi][:, :].rearrange("p (a b) -> p a b", a=4))
            for g in range(16):
                bank = 4 + g // 4
                P.op("pe", "matmul", ["qT%d" % (g // 4), "const"], ["p%d" % bank], pb[bank][:, (g % 4) * 128:(g % 4 + 1) * 128], lhsT=qT[:, g, :], rhs=skT[:, l, g % 2, :],
                     start=True, stop=True)
            for b4 in range(4):
                bank = 4 + b4
                if b4 % 2 == 0:
                    P.op("act", "activation", ["p%d" % bank], ["s%d" % b4], out=s[:, b4 * 4:b4 * 4 + 4, :], in_=pb[bank][:, :].rearrange("p (a b) -> p a b", a=4), func=ACT.Copy)
                else:
                    P.op("dve", "tensor_copy", ["p%d" % bank], ["s%d" % b4], out=s[:, b4 * 4:b4 * 4 + 4, :], in_=pb[bank][:, :].rearrange("p (a b) -> p a b", a=4))
            snames_all = ["s%d" % b4 for b4 in range(4)]
            for g in range(16):
                P.op("dve", "max", ["s%d" % (g // 4)], ["v16a%d" % g], out=v16[:, g, 0:8], in_=s[:, g, :])
            for g in range(16):
                P.op("dve", "match_replace", ["s%d" % (g // 4), "v16a%d" % g], ["sw%d" % g], out=sw[:, g, :], in_to_replace=v16[:, g, 0:8], in_values=s[:, g, :], imm_value=NEG)
            for g in range(16):
                P.op("dve", "max", ["sw%d" % g], ["v16b%d" % g], out=v16[:, g, 8:16], in_=sw[:, g, :])
            v16n = ["v16a%d" % g for g in range(16)] + ["v16b%d" % g for g in range(16)]
            Ev = sm[:, 536:792].rearrange("p (g k) -> p g k", g=16)
            rZ = sm[:, 520:528]
            E = sw
            swn = ["sw%d" % g for g in range(16)]
            P.op("dve", "tensor_tensor", snames_all + v16n + swn, ["E"], out=E, in0=s, in1=v16[:, :, 0:1].to_broadcast([128, 16, 128]), op=ALU.subtract)
            P.op("act", "activation", ["E"], ["E"], out=E, in_=E, func=ACT.Exp)
            P.op("dve", "tensor_tensor", v16n, ["Ev"], out=Ev, in0=v16, in1=v16[:, :, 0:1].to_broadcast([128, 16, 16]), op=ALU.subtract)
            P.op("act", "activation", ["Ev"], ["Ev"], out=Ev, in_=Ev, func=ACT.Exp)
            E4 = E.rearrange("p (h c) n -> p h c n", c=2)
            Ev4 = Ev.rearrange("p (h c) k -> p h c k", c=2)
            c16n = ["c16a%d" % h for h in range(8)] + ["c16b%d" % h for h in range(8)]
            P.op("dve", "tensor_tensor", ["Ev"], ["cand"], out=cand,
                 in0=Ev4[:, :, 0, :].unsqueeze(3).to_broadcast([128, 8, 16, 16]), in1=Ev4[:, :, 1, :].unsqueeze(2).to_broadcast([128, 8, 16, 16]), op=ALU.mult)
            for h in range(8):
                P.op("dve", "max", ["cand"], ["c16a%d" % h], out=c16[:, h, 0:8], in_=cand[:, h])
            for h in range(8):
                P.op("dve", "match_replace", ["cand", "c16a%d" % h], ["cw%d" % h], out=cw[:, h], in_to_replace=c16[:, h, 0:8], in_values=cand[:, h], imm_value=-1.0)
            for h in range(8):
                P.op("dve", "max", ["cw%d" % h], ["c16b%d" % h], out=c16[:, h, 8:16], in_=cw[:, h])
            P.op("dve", "tensor_reduce", c16n, ["Z"], out=Z, in_=c16, axis=AX.X, op=ALU.add)
            P.op("dve", "reciprocal", ["Z"], ["rZ"], out=rZ, in_=Z)
            P.op("dve", "tensor_tensor", ["E", "rZ"], ["E"], out=E4[:, :, 0, :], in0=E4[:, :, 0, :], in1=rZ.unsqueeze(2).to_broadcast([128, 8, 128]), op=ALU.mult)
            thr = sm[:, 528:536]
            P.op("dve", "scalar_tensor_tensor", c16n + ["rZ"], ["thr"], out=thr, in0=c16[:, :, 15], scalar=float(1.0 - 2.0 ** -20), in1=rZ, op0=ALU.mult, op1=ALU.mult)
            EEs = [SV(i, 0, 1024).rearrange("p (i j) -> p i j", i=8) for i in (5, 6, 0)]
            gtall = SV(7, 0, 2048).bitcast(BF16)
            Gts = [gtall[:, k * 1024:(k + 1) * 1024] for k in range(4)]
            gk = [0]

            def gbuild_head(n, h):
                k = gk[0] % 3
                k4 = gk[0] % 4
                gk[0] += 1
                EE, Gt = EEs[k], Gts[k4]
                een, gtn = "EE%d" % k, "Gt%d" % k4
                eenl = [een + ".%d" % ii for ii in range(8)]
                i0 = n * 8
                if h % 2 == ACT_PAR:
                    for ii in range(8):
                        P.op("act", "activation", ["E", "thr"], [eenl[ii]], out=EE[:, ii, :], in_=E4[:, h, 1, :], func=ACT.Copy, scale=E4[:, h, 0, i0 + ii:i0 + ii + 1])
                else:
                    P.op("dve" if h % 4 == 3 else "pool", "tensor_tensor", ["E", "thr"], eenl, out=EE, in0=E4[:, h, 0, i0:i0 + 8].unsqueeze(2).to_broadcast([128, 8, 128]),
                         in1=E4[:, h, 1, :].unsqueeze(1).to_broadcast([128, 8, 128]), op=ALU.mult)
                P.op("dve", "scalar_tensor_tensor", eenl + ["thr"], [gtn], out=Gt.rearrange("p (i j) -> p i j", i=8), in0=EE, scalar=thr[:, h:h + 1], in1=EE,
                     op0=ALU.is_ge, op1=ALU.mult)
                pend_acc.append((n, h, gtn, Gt))

            pend_acc = []

            def flush_pe_acc():
                for (n, h, gtn, Gt) in pend_acc:
                    for c in range(2):
                        bank = 2 + 2 * (n % 2) + c
                        P.op("pe", "matmul", [gtn, "identb"], ["p%d" % bank], pb[bank][:, :], lhsT=identb[:], rhs=Gt[:, c * 512:(c + 1) * 512], start=(h == 0), stop=(h == 7))
                del pend_acc[:]

            utv = utb_d[l].rearrange("(kc p) e -> p kc e", p=128)
            chain = [None]
            p1b = pb[1][:, :].bitcast(BF16)

            def tail_T(prev):
                (ebp, GA, GAT, sn, wnv, vblk, first, last) = prev
                half = p1b[:, (ebp % 2) * 512:(ebp % 2 + 1) * 512]
                for j in range(4):
                    P.op("pe", "transpose", [sn + ".GA", "identb"], ["p1"], out=half[:, j * 128:(j + 1) * 128], in_=GA[:, j * 128:(j + 1) * 128], identity=identb[:])
                P.op("act", "activation", ["p1"], [sn + ".GAT"], out=GAT, in_=half.rearrange("p (a b) -> p a b", a=4), func=ACT.Copy)

            def tail_O(prev):
                (ebp, GA, GAT, sn, wnv, vblk, first, last) = prev
                for j in range(4):
                    for db in range(2):
                        P.op("pe", "matmul", [sn + ".GAT", wnv], ["p%d" % (6 + db)], pb[6 + db][:, :], lhsT=GAT[:, j, :], rhs=vblk[:, j, db * 512:(db + 1) * 512],
                             start=(first and j == 0), stop=(last and j == 3))

            for h in range(8):
                gbuild_head(0, h)
                if h % 4 == 3:
                    flush_pe_acc()
            for nb in range(32):
                n = nb // 2
                e0 = nb * 512
                i = wsel[0]
                wsel[0] ^= 1
                wnu = "wbuf%du" % i
                wnv = "wbuf%dv" % i
                wbb = wbuf[i][:].rearrange("p a b -> p (a b)").bitcast(BF16)
                ublk = wbb[:, 0:4096].rearrange("p (kc e) -> p kc e", kc=8)
                vblk = wbb[:, 4096:8192].rearrange("p (j d) -> p j d", j=4)
                extra = ["wbuf%d" % i] if nb < 2 else []
                P.dma(wnu, [], [wnu] + extra, [(ublk, utv[:, :, e0:e0 + 512])])
                P.dma(wnv, [], [wnv] + extra, [(vblk, vb_d[l][e0:e0 + 512, :].rearrange("(j p) d -> p j d", p=128))])
                si = 10 + (nb % 2)
                sn = "slot%d" % si
                Ag = SV(si, 0, 512)
                GA = SV(si, 512, 768).bitcast(BF16)
                GAT = SV(si, 1024, 1280).bitcast(BF16).rearrange("p (a b) -> p a b", a=4)
                for kc in range(KC):
                    P.op("pe", "matmul", [wnu, "hTb"], ["p0"], pb[0][:, :], lhsT=hTb[:, kc, :], rhs=ublk[:, kc, :], start=(kc == 0), stop=(kc == KC - 1))
                P.op("act", "activation", ["p0"], [sn + ".Ag"], out=Ag, in_=pb[0][:, :], func=ACT.Gelu)
                if chain[0] is not None:
                    tail_T(chain[0])
                flush_pe_acc()
                gbank = 2 + 2 * (n % 2) + (nb % 2)
                P.op("dve", "tensor_tensor", [sn + ".Ag", "p%d" % gbank], [sn + ".GA"], out=GA, in0=pb[gbank][:, :], in1=Ag, op=ALU.mult)
                if chain[0] is not None:
                    tail_O(chain[0])
                if n < 15:
                    for h in range(4 * (nb % 2), 4 * (nb % 2) + 4):
                        gbuild_head(n + 1, h)
                chain[0] = (nb, GA, GAT, sn, wnv, vblk, nb == 0, nb == 31)
            tail_T(chain[0])
            tail_O(chain[0])
            for db in range(2):
                P.op("dve", "tensor_tensor", ["p%d" % (6 + db), "gates"], ["ytmp%d" % db], out=slot[8][:, db * 512:(db + 1) * 512],
                     in0=pb[6 + db][:, :], in1=gates[:, gi, db * 512:(db + 1) * 512], op=ALU.mult)
                P.op("pool", "tensor_tensor", ["ytmp%d" % db, xname], [xname], out=xa[:, db * 512:(db + 1) * 512], in0=xa[:, db * 512:(db + 1) * 512],
                     in1=slot[8][:, db * 512:(db + 1) * 512], op=ALU.add)

        def final_norm(xa, xname, it):
            P.op("act", "activation", [xname], ["junk", "ss0"], out=junk[:], in_=xa, func=ACT.Square, accum_out=ss[:, 0:1])
            P.op("dve", "tensor_scalar", ["ss0"], ["ss1"], out=ss[:, 1:2], in0=ss[:, 0:1], scalar1=1.0 / D, scalar2=EPS, op0=ALU.mult, op1=ALU.add)
            P.op("act", "activation", ["ss1"], ["ss2"], out=ss[:, 2:3], in_=ss[:, 1:2], func=ACT.Sqrt)
            P.op("dve", "reciprocal", ["ss2"], ["ss3"], out=ss[:, 3:4], in_=ss[:, 2:3])
            P.op("dve", "scalar_tensor_tensor", [xname, "ss3", "const"], ["xn"], out=xn[:], in0=xa, scalar=ss[:, 3:4], in1=gfrow[:], op0=ALU.mult, op1=ALU.mult)
            P.dma("yout", ["xn"], [], [(y_d[it * 128:(it + 1) * 128, :], xn[:])])

        stages = os.environ.get("YOCO_STAGES", "gla,peer0,kv,swa,peer1").split(",")
        for it in range(NT):
            xa = xt[it % 2][:]
            xname = "xt%d" % (it % 2)
            P.dma(xname, [], [xname], [(xa, x_d[it * 128:(it + 1) * 128, :])])
            if "gla" in stages:
                gla(xa, xname, it)
                P.barrier()
            if "peer0" in stages:
                peer(xa, xname, 0)
                P.barrier()
            if "kv" in stages:
                kv_phase(xa, xname, it)
                P.barrier()
            if "swa" in stages:
                swa(xa, xname, it)
                P.barrier()
            if "peer1" in stages:
                peer(xa, xname, 1)
                P.barrier()
            final_norm(xa, xname, it)
            P.barrier()
        P.barrier()
        P.emit()
        print("yoco build: ops", P.nops, {e: P.cnt[e] for e in P.ENG}, flush=True)
    return nc


def prep_inputs(inp, NT, nb=8):
    f = np.float32
    S = NT * 128
    t = np.arange(128)
    same = (t[:, None] // 64) == (t[None, :] // 64)
    tri = (same & (t[:, None] <= t[None, :])).astype(f) * f(-1.0 / 16)
    blk = same.astype(f) * f(-1.0 / 16)
    csel = ((t[:, None] // 64) == np.arange(2)[None, :]).astype(f) * f(-1.0 / 16)
    maskT = (same & (t[:, None] <= t[None, :])).astype(f)
    qi = np.arange(128)[:, None]
    mi = np.arange(256)[None, :]
    valid = (mi > qi) & (mi <= qi + 128)
    swam = np.stack([np.where(valid & (mi >= 128), 0.0, NEG), np.where(valid, 0.0, NEG)], axis=1).astype(f)
    invf = (np.float32(500000.0) ** (-(np.arange(0, 16, 2, dtype=np.float32)) / np.float32(16))).astype(f)
    invf = np.ascontiguousarray(np.broadcast_to(invf[None, :], (128, 8)))

    def col(v):
        return np.ascontiguousarray(v.reshape(8, 128).T)

    def row(v):
        return np.ascontiguousarray(np.broadcast_to(v[None, :], (128, v.shape[0])))

    mod_b = inp["mod_b"]
    shared = {
        "invf": invf, "ident": np.eye(128, dtype=f), "tri": tri, "blk": blk, "csel": csel, "maskT": maskT, "swam": swam,
        "modw0": np.ascontiguousarray(inp["mod_w"][0]), "modw1": np.ascontiguousarray(inp["mod_w"][1]),
        "modbcol": np.ascontiguousarray(np.stack([mod_b[l].reshape(48, 128).T for l in range(2)], axis=1)),
        "gaterow": np.ascontiguousarray(np.stack([row(mod_b[0, 2048:3072]), row(mod_b[0, 5120:6144]),
                                                   row(mod_b[1, 2048:3072]), row(mod_b[1, 5120:6144])], axis=1)),
        "kvmodw": np.ascontiguousarray(inp["kv_mod_w"]),
        "kvmodbcol": np.ascontiguousarray(inp["kv_mod_b"].reshape(16, 128).T),
        "ngcol": np.ascontiguousarray(np.stack([col(inp["norm_g"][0, 0]), col(inp["norm_g"][0, 1]), col(inp["norm_g"][1, 0]),
                                                col(inp["norm_g"][1, 1]), col(inp["kv_norm_g"])], axis=1)),
        "gfrow": row(inp["final_norm_g"]),
        "w_in": np.ascontiguousarray(inp["gla_w_in"][0]), "wg2": np.ascontiguousarray(inp["gla_w_g2"][0]),
        "bg2row": row(inp["gla_b_g2"][0]), "gnrow": row(inp["gla_norm_g"][0]), "gwo": np.ascontiguousarray(inp["gla_w_out"][0]),
        "kvw": np.ascontiguousarray(inp["kv_w"]), "swq": np.ascontiguousarray(inp["swa_w_q"][0]),
        "sinkrow": row(inp["swa_sinks"][0]), "swo": np.ascontiguousarray(inp["swa_w_out"][0]),
        "pwq0": np.ascontiguousarray(inp["peer_w_q"][0]), "pwq1": np.ascontiguousarray(inp["peer_w_q"][1]),
        "skT": np.ascontiguousarray(np.transpose(inp["peer_subkeys"], (3, 0, 1, 2))),
        "ut0": np.ascontiguousarray(inp["peer_u"][0].T), "ut1": np.ascontiguousarray(inp["peer_u"][1].T),
        "v0": np.ascontiguousarray(inp["peer_v"][0]), "v1": np.ascontiguousarray(inp["peer_v"][1]),
    }
    maps = []
    for b in range(nb):
        m = dict(shared)
        m["x"] = np.ascontiguousarray(inp["x"][b, :S])
        m["ccol"] = col(inp["c"][b])
        m["pos"] = np.ascontiguousarray(inp["positions"][b, :S].reshape(NT, 128).T.astype(np.int32))
        maps.append(m)
    return maps


_NC_CACHE = {}


def kernel(**inputs):
    inputs = {k: np.asarray(v) for k, v in inputs.items()}
    NT = SEQ // 128
    if NT not in _NC_CACHE:
        _NC_CACHE[NT] = build(NT)
    nc = _NC_CACHE[NT]
    maps = prep_inputs(inputs, NT)
    res = run_bass_kernel_spmd(nc, maps, core_ids=list(range(8)))
    out = np.stack([np.asarray(r["y"]) for r in res.results], axis=0)
    return out.astype(np.float32)
```

```python
import os
from contextlib import ExitStack
import numpy as np
import concourse.bass as bass
import concourse.mybir as mybir
from concourse.bass_utils import run_bass_kernel_spmd

F32 = mybir.dt.float32
BF16 = mybir.dt.bfloat16
I32 = mybir.dt.int32
ACT = mybir.ActivationFunctionType
ALU = mybir.AluOpType
AX = mybir.AxisListType

D = 1024
KC = 8
SEQ = 8192
NEXP = 16384
EPS = 1e-6
NEG = -1e30
ACT_PAR = int(os.environ.get('ACT_PAR', '-1'))
BF16_PROJ = os.environ.get('BF16_PROJ', '1') == '1'
BF16_PQ = os.environ.get('BF16_PQ', '1') == '1'
SWA_CUT = int(os.environ.get('SWA_CUT', '99'))
MODEV = os.environ.get('MODEV', 'dve')
GLA_CUT = int(os.environ.get('GLA_CUT', '99'))
PI = float(np.pi)


class Prog:
    ENG = ("pe", "dve", "act", "pool", "sp")

    def __init__(self, nc, es):
        self.nc = nc
        self.es = es
        self.ops = {e: [] for e in self.ENG}
        self.sem = {e: es.enter_context(nc.semaphore("sem_" + e)) for e in self.ENG}
        self.cnt = {e: 0 for e in self.ENG}
        self.dsem = {}
        self.dcnt = {}
        self.lastw = {}
        self.readers = {}
        self.waited = {e: {} for e in self.ENG}
        self.nops = 0

    def _deps(self, eng, r, w):
        deps = {}

        def add(tok):
            if tok is None:
                return
            s, v = tok
            if eng == "pe" and s.name == self.sem["pe"].name:
                return
            if deps.get(s.name, (None, 0))[1] < v:
                deps[s.name] = (s, v)

        for b in r:
            add(self.lastw.get(b))
        for b in w:
            add(self.lastw.get(b))
            for tok in self.readers.get(b, {}).values():
                add(tok)
        out = []
        for key, (s, v) in deps.items():
            if self.waited[eng].get(key, 0) < v:
                self.waited[eng][key] = v
                out.append((s, v))
        return out

    def _commit(self, tok, r, w):
        for b in w:
            self.lastw[b] = tok
            self.readers[b] = {}
        for b in r:
            self.readers.setdefault(b, {})[tok[0].name] = tok

    def op(self, eng, meth, r, w, *a, **k):
        w = list(w) + [b for b in r if len(b) == 2 and b[0] == "p" and b[1].isdigit()]
        waits = self._deps(eng, r, w)
        self.cnt[eng] += 1
        tok = (self.sem[eng], self.cnt[eng])
        self.ops[eng].append((waits, meth, a, k, self.sem[eng], 1))
        self._commit(tok, r, w)
        self.nops += 1

    def dma(self, key, r, w, pairs, eng="sp"):
        if key not in self.dsem:
            self.dsem[key] = self.es.enter_context(self.nc.semaphore("dsem_" + key))
            self.dcnt[key] = 0
        waits = self._deps(eng, r, w)
        for (o, i) in pairs:
            self.dcnt[key] += 16
            self.ops[eng].append((waits, "dma_start", (), dict(out=o, in_=i), self.dsem[key], 16))
            waits = []
            self.nops += 1
        tok = (self.dsem[key], self.dcnt[key])
        self._commit(tok, r, w)

    def barrier(self):
        allw = [(self.sem[e], self.cnt[e]) for e in self.ENG if self.cnt[e] > 0]
        allw += [(s, self.dcnt[k]) for k, s in self.dsem.items()]
        for e in self.ENG:
            ws = []
            for (s, v) in allw:
                if s.name == self.sem[e].name:
                    continue
                if self.waited[e].get(s.name, 0) < v:
                    self.waited[e][s.name] = v
                    ws.append((s, v))
            if ws:
                self.ops[e].append((ws, None, (), {}, None, 0))
        self.lastw = {}
        self.readers = {}

    def emit(self):
        nc = self.nc
        with nc.Block() as block:
            def run(engname):
                def body(engine):
                    for waits, meth, a, k, sem, inc in self.ops[engname]:
                        for (s, v) in waits:
                            engine.wait_ge(s, v)
                        if meth is None:
                            continue
                        getattr(engine, meth)(*a, **k).then_inc(sem, inc)
                return body
            block.tensor(run("pe"))
            block.vector(run("dve"))
            block.scalar(run("act"))
            block.gpsimd(run("pool"))
            block.sync(run("sp"))


def build(NT, dbg=None):
    nc = bass.Bass("TRN2", target_bir_lowering=False)
    S = NT * 128

    def din(name, shape, dt=F32):
        return nc.dram_tensor(name, list(shape), dt, kind="ExternalInput").ap()

    x_d = din("x", [S, D])
    y_d = nc.dram_tensor("y", [S, D], F32, kind="ExternalOutput").ap()
    ccol_d = din("ccol", [128, 8])
    pos_d = din("pos", [128, NT], I32)
    invf_d = din("invf", [128, 8])
    modw_d = [din("modw0", [D, 6 * D]), din("modw1", [D, 6 * D])]
    modbcol_d = din("modbcol", [128, 2, 48])
    gaterow_d = din("gaterow", [128, 4, D])
    kvmodw_d = din("kvmodw", [D, 2 * D])
    kvmodbcol_d = din("kvmodbcol", [128, 16])
    ngcol_d = din("ngcol", [128, 5, 8])
    gfrow_d = din("gfrow", [128, D])
    w_in_d = din("w_in", [D, 3088])
    wg2_d = din("wg2", [16, 512])
    bg2row_d = din("bg2row", [128, 512])
    gnrow_d = din("gnrow", [128, 256])
    gwo_d = din("gwo", [D, D])
    kvw_d = din("kvw", [D, 512])
    swq_d = din("swq", [D, D])
    sinkrow_d = din("sinkrow", [128, 16])
    swo_d = din("swo", [D, D])
    pwq_d = [din("pwq0", [D, 2048]), din("pwq1", [D, 2048])]
    skT_d = din("skT", [128, 2, 2, 128])
    ut_d = [din("ut0", [D, NEXP]), din("ut1", [D, NEXP])]
    v_d = [din("v0", [NEXP, D]), din("v1", [NEXP, D])]
    ident_d = din("ident", [128, 128])
    tri_d = din("tri", [128, 128])
    blk_d = din("blk", [128, 128])
    csel_d = din("csel", [128, 2])
    maskT_d = din("maskT", [128, 128])
    swam_d = din("swam", [128, 2, 256])
    utb_d = [nc.dram_tensor("utb%d" % l, [D, NEXP], BF16, kind="Internal").ap() for l in range(2)]
    vb_d = [nc.dram_tensor("vb%d" % l, [NEXP, D], BF16, kind="Internal").ap() for l in range(2)]
    wb16 = {nm: nc.dram_tensor("b16_" + nm, [D, w], BF16, kind="Internal").ap()
            for nm, w in (("w_in", 3072), ("gwo", D), ("kvw", 512), ("swq", D), ("swo", D), ("pwq0", 2048), ("pwq1", 2048))}
    wsrc = {"w_in": w_in_d, "gwo": gwo_d, "kvw": kvw_d, "swq": swq_d, "swo": swo_d, "pwq0": pwq_d[0], "pwq1": pwq_d[1]}
    dbg_d = None
    if dbg is not None:
        dbg_d = nc.dram_tensor("dbg", [128, 8192], F32, kind="ExternalOutput").ap()

    es = ExitStack()
    with es:
        P = Prog(nc, es)

        def sb(name, shape, dt=F32):
            return es.enter_context(nc.sbuf_tensor("sb_" + name, list(shape), dt))

        def psum(name):
            return es.enter_context(nc.psum_tensor(name, [128, 512], F32))

        ident = sb("ident", [128, 128])
        identb = sb("identb", [128, 128], BF16)
        tri = sb("tri", [128, 128])
        blk = sb("blk", [128, 128])
        csel = sb("csel", [128, 2])
        maskT = sb("maskT", [128, 128])
        swam = sb("swam", [128, 2, 256])
        gates = sb("gates", [128, 4, D])
        gfrow = sb("gfrow", [128, D])
        gnrow = sb("gnrow", [128, 256])
        bg2row = sb("bg2row", [128, 512])
        sinkrow = sb("sinkrow", [128, 16])
        wg2 = sb("wg2", [16, 512])
        skT = sb("skT", [128, 2, 2, 128])
        modc = sb("modc", [128, 10, 8])
        ngcol = sb("ngcol", [128, 5, 8])
        modbcol = sb("modbcol", [128, 2, 48])
        kvmodbcol = sb("kvmodbcol", [128, 16])
        ccol = sb("ccol", [128, 8])
        cact = sb("cact", [128, 8])
        cbc = sb("cbc", [128, 8, 128])
        cosT = sb("cosT", [128, NT, 8])
        sinT = sb("sinT", [128, NT, 8])
        posi = sb("posi", [128, NT], I32)
        invf = sb("invf", [128, 8])
        xt = [sb("xt0", [128, D]), sb("xt1", [128, D])]
        xn = sb("xn", [128, D])
        hT = sb("hT", [128, KC, 128])
        hTb = sb("hTb", [128, KC, 128], BF16)
        junk = sb("junk", [128, D], BF16)
        ss = sb("ss", [128, 8])
        sm = sb("sm", [128, 1024])
        wbuf = [sb("wbuf0", [128, KC, 512]), sb("wbuf1", [128, KC, 512])]
        NSLOT = 12
        slot = [sb("slot%d" % i, [128, 2048]) for i in range(NSLOT)]
        Sst = [sb("Sa", [128, 4, 256]), sb("Sb", [128, 4, 256])]
        kTd = [sb("kTd0", [128, 4, 128]), sb("kTd1", [128, 4, 128])]
        vbd = [sb("vbd0", [128, 256]), sb("vbd1", [128, 256])]
        pb = [psum("p%d" % i) for i in range(8)]

        def SV(i, lo, hi):
            return slot[i][:, lo:hi]

        consts = [(ident, ident_d), (tri, tri_d), (blk, blk_d), (csel, csel_d), (maskT, maskT_d), (swam, swam_d),
                  (gates, gaterow_d), (gfrow, gfrow_d), (gnrow, gnrow_d), (bg2row, bg2row_d), (sinkrow, sinkrow_d),
                  (wg2, wg2_d), (skT, skT_d), (ngcol, ngcol_d), (modbcol, modbcol_d), (kvmodbcol, kvmodbcol_d),
                  (ccol, ccol_d), (posi, pos_d), (invf, invf_d)]
        P.dma("const", [], ["const"], [(t[:], d) for (t, d) in consts])
        P.op("dve", "memset", [], ["Sa"], Sst[0][:], 0.0)
        P.op("dve", "memset", [], ["k1"], kTd[1][:], 0.0)
        P.op("dve", "memset", [], ["v1"], vbd[1][:], 0.0)
        P.op("dve", "tensor_copy", ["const"], ["identb"], out=identb[:], in_=ident[:])
        P.op("act", "activation", ["const"], ["cact"], out=cact[:], in_=ccol[:], func=ACT.Silu)
        P.op("dve", "tensor_copy", ["cact"], ["cbc"], out=cbc[:], in_=cact[:].unsqueeze(2).to_broadcast([128, 8, 128]))
        posf = sm[:, 0:NT]
        ang = slot[0][:, 0:NT * 8].rearrange("p (a b) -> p a b", b=8)
        ang2 = slot[0][:, 1024:1024 + NT * 8].rearrange("p (a b) -> p a b", b=8)
        kf = slot[1][:, 0:NT * 8].rearrange("p (a b) -> p a b", b=8)
        ki = slot[2][:, 0:NT * 8].bitcast(I32).rearrange("p (a b) -> p a b", b=8)
        P.op("dve", "tensor_copy", ["const"], ["posf"], out=posf, in_=posi[:])
        P.op("dve", "tensor_tensor", ["posf", "const"], ["ang"], out=ang, in0=posf.unsqueeze(2).to_broadcast([128, NT, 8]),
             in1=invf[:].unsqueeze(1).to_broadcast([128, NT, 8]), op=ALU.mult)
        P.op("dve", "tensor_scalar_add", ["ang"], ["ang2"], out=ang2, in0=ang, scalar1=PI / 2)
        for (src, nm, dst) in ((ang, "ang", sinT), (ang2, "ang2", cosT)):
            P.op("dve", "tensor_scalar", [nm], ["ki"], out=ki, in0=src, scalar1=float(1.0 / (2 * PI)), scalar2=None, op0=ALU.mult)
            P.op("dve", "tensor_copy", ["ki"], ["kf"], out=kf, in_=ki)
            P.op("dve", "scalar_tensor_tensor", ["kf", nm], [nm], out=src, in0=kf, scalar=float(-2 * PI), in1=src, op0=ALU.mult, op1=ALU.add)
            P.op("dve", "tensor_single_scalar", [nm], ["kf"], out=kf, in_=src, scalar=PI, op=ALU.is_gt)
            P.op("dve", "scalar_tensor_tensor", ["kf", nm], [nm], out=src, in0=kf, scalar=float(-2 * PI), in1=src, op0=ALU.mult, op1=ALU.add)
            P.op("dve", "tensor_single_scalar", [nm], ["kf"], out=kf, in_=src, scalar=-PI, op=ALU.is_lt)
            P.op("dve", "scalar_tensor_tensor", ["kf", nm], [nm], out=src, in0=kf, scalar=float(2 * PI), in1=src, op0=ALU.mult, op1=ALU.add)
            P.op("act", "activation", [nm], [nm + "_out"], out=dst[:], in_=src, func=ACT.Sin)

        wsel = [0]

        def wload(dram_w, c0, ncols):
            i = wsel[0]
            wsel[0] ^= 1
            nm = "wbuf%d" % i
            P.dma(nm, [], [nm], [(wbuf[i][:, :, 0:ncols], dram_w[:, c0:c0 + ncols].rearrange("(kc p) n -> p kc n", p=128))])
            return nm, wbuf[i]

        mcol = sm[:, 64:64 + 96].rearrange("p (a b) -> p a b", b=8)
        for l in range(2):
            for bi in range(12):
                nm, wb = wload(modw_d[l], bi * 512, 512)
                kind = bi // 2
                if kind in (2, 5):
                    gi = l * 2 + (0 if kind == 2 else 1)
                    half = bi % 2
                    for kc in range(KC):
                        P.op("pe", "matmul", [nm, "cbc"], ["p0"], pb[0][:, :], lhsT=cbc[:, kc, :], rhs=wb[:, kc, :], start=(kc == 0), stop=(kc == KC - 1))
                    P.op("dve", "tensor_tensor", ["p0", "const"], ["gates"], out=gates[:, gi, half * 512:(half + 1) * 512], in0=pb[0][:, :],
                         in1=gates[:, gi, half * 512:(half + 1) * 512], op=ALU.add)
                else:
                    for j in range(4):
                        for kc in range(KC):
                            P.op("pe", "matmul", [nm, "cact"], ["p1"], pb[1][:, j:j + 1], lhsT=wb[:, kc, j * 128:(j + 1) * 128], rhs=cact[:, kc:kc + 1],
                                 start=(kc == 0), stop=(kc == KC - 1))
                    vi = {0: 0, 1: 1, 3: 2, 4: 3}[kind]
                    P.op("dve", "tensor_tensor", ["p1", "const"], ["mcol"], out=mcol[:, l * 4 + vi, (bi % 2) * 4:(bi % 2) * 4 + 4], in0=pb[1][:, 0:4],
                         in1=modbcol[:, l, bi * 4:bi * 4 + 4], op=ALU.add)
        for bi in range(4):
            nm, wb = wload(kvmodw_d, bi * 512, 512)
            for j in range(4):
                for kc in range(KC):
                    P.op("pe", "matmul", [nm, "cact"], ["p1"], pb[1][:, j:j + 1], lhsT=wb[:, kc, j * 128:(j + 1) * 128], rhs=cact[:, kc:kc + 1],
                         start=(kc == 0), stop=(kc == KC - 1))
            P.op("dve", "tensor_tensor", ["p1", "const"], ["mcol"], out=mcol[:, 8 + bi // 2, (bi % 2) * 4:(bi % 2) * 4 + 4], in0=pb[1][:, 0:4],
                 in1=kvmodbcol[:, bi * 4:bi * 4 + 4], op=ALU.add)
        for (mi, gi, shi, sci) in ((0, 0, 0, 1), (2, 1, 2, 3), (4, 4, 8, 9), (6, 2, 4, 5), (8, 3, 6, 7)):
            P.op("dve", "scalar_tensor_tensor", ["mcol", "const"], ["modc"], out=modc[:, mi, :], in0=mcol[:, sci, :], scalar=1.0, in1=ngcol[:, gi, :],
                 op0=ALU.add, op1=ALU.mult)
            P.op("dve", "tensor_copy", ["mcol"], ["modc"], out=modc[:, mi + 1, :], in_=mcol[:, shi, :])
        P.barrier()

        cv = [0]

        def convert(src_ap, dst_ap, W=4096):
            i = cv[0] % 3
            cv[0] += 1
            H = W // 2
            P.dma("cin%d" % i, [], ["cin%d" % i], [(slot[2 * i][:, 0:H], src_ap[:, 0:H]), (slot[2 * i + 1][:, 0:H], src_ap[:, H:W])])
            ob = slot[6 + i][:, :].bitcast(BF16)
            eng = ("dve", "dve", "pool")[i]
            if eng == "act":
                P.op("act", "activation", ["cin%d" % i], ["cob%d" % i], out=ob[:, 0:H], in_=slot[2 * i][:, 0:H], func=ACT.Copy)
                P.op("act", "activation", ["cin%d" % i], ["cob%d" % i], out=ob[:, H:W], in_=slot[2 * i + 1][:, 0:H], func=ACT.Copy)
            else:
                P.op(eng, "tensor_copy", ["cin%d" % i], ["cob%d" % i], out=ob[:, 0:H], in_=slot[2 * i][:, 0:H])
                P.op(eng, "tensor_copy", ["cin%d" % i], ["cob%d" % i], out=ob[:, H:W], in_=slot[2 * i + 1][:, 0:H])
            P.dma("cout%d" % i, ["cob%d" % i], [], [(dst_ap, ob[:, 0:W])], eng="act")

        if BF16_PROJ:
            for nm in ("w_in", "gwo", "kvw", "swq", "swo") + (("pwq0", "pwq1") if BF16_PQ else ()):
                W = wb16[nm].shape[1]
                for kc in range(KC):
                    convert(wsrc[nm][kc * 128:(kc + 1) * 128, 0:W], wb16[nm][kc * 128:(kc + 1) * 128, :], W=W)

        for l in range(2):
            for kc in range(KC):
                for e4 in range(4):
                    convert(ut_d[l][kc * 128:(kc + 1) * 128, e4 * 4096:(e4 + 1) * 4096],
                            utb_d[l][kc * 128:(kc + 1) * 128, e4 * 4096:(e4 + 1) * 4096])
            for r in range(32):
                convert(v_d[l][r * 512:(r + 1) * 512, :].rearrange("(p j) d -> p (j d)", j=4),
                        vb_d[l][r * 512:(r + 1) * 512, :].rearrange("(p j) d -> p (j d)", j=4))
        P.barrier()

        def modulate(xa, xname, mi, bf16=False, need32=True):
            P.op("act", "activation", [xname], ["junk", "ss0"], out=junk[:], in_=xa, func=ACT.Square, accum_out=ss[:, 0:1])
            P.op("dve", "tensor_scalar", ["ss0"], ["ss1"], out=ss[:, 1:2], in0=ss[:, 0:1], scalar1=1.0 / D, scalar2=EPS, op0=ALU.mult, op1=ALU.add)
            P.op("act", "activation", ["ss1"], ["ss2"], out=ss[:, 2:3], in_=ss[:, 1:2], func=ACT.Sqrt)
            P.op("dve", "reciprocal", ["ss2"], ["ss3"], out=ss[:, 3:4], in_=ss[:, 2:3])
            P.op("dve", "tensor_scalar", [xname, "ss3"], ["xn"], out=xn[:], in0=xa, scalar1=ss[:, 3:4], scalar2=None, op0=ALU.mult)
            for kc in range(KC):
                bnk = kc // 4
                P.op("pe", "transpose", ["xn", "const"], ["p%d" % bnk], out=pb[bnk][:, (kc % 4) * 128:(kc % 4 + 1) * 128],
                     in_=xn[:, kc * 128:(kc + 1) * 128], identity=ident[:])
            direct = bf16 and not need32
            for kc in range(KC):
                bnk = kc // 4
                src = pb[bnk][:, (kc % 4) * 128:(kc % 4 + 1) * 128]
                dst = hTb[:, kc, :] if direct else hT[:, kc, :]
                dn = "hTb" if direct else "hT%d" % kc
                P.op("dve", "tensor_scalar", ["p%d" % bnk, "modc"], [dn], out=dst, in0=src,
                     scalar1=modc[:, mi, kc:kc + 1], scalar2=modc[:, mi + 1, kc:kc + 1], op0=ALU.mult, op1=ALU.add)
            if bf16 and need32:
                P.op("act", "activation", ["hT%d" % kc for kc in range(4)], ["hTb"], out=hTb[:, 0:4, :], in_=hT[:, 0:4, :], func=ACT.Copy)
                P.op("act", "activation", ["hT%d" % kc for kc in range(4, 8)], ["hTb"], out=hTb[:, 4:8, :], in_=hT[:, 4:8, :], func=ACT.Copy)
            return ["hT%d" % kc for kc in range(KC)]

        def wload_b(wname, c0, ncols):
            i = wsel[0]
            wsel[0] ^= 1
            nm = "wbuf%d" % i
            wv = wbuf[i][:].rearrange("p a b -> p (a b)").bitcast(BF16)[:, 0:4096].rearrange("p (kc n) -> p kc n", kc=8)
            P.dma(nm, [], [nm], [(wv[:, :, 0:ncols], wb16[wname][:, c0:c0 + ncols].rearrange("(kc p) n -> p kc n", p=128))])
            return nm, wv

        def dense_block(hTn, dram_w, c0, ncols, bank, bname=None):
            if BF16_PROJ and bname is not None:
                nm, wb = wload_b(bname, c0, ncols)
                for kc in range(KC):
                    P.op("pe", "matmul", [nm, "hTb"], ["p%d" % bank], pb[bank][:, 0:ncols], lhsT=hTb[:, kc, :], rhs=wb[:, kc, 0:ncols],
                         start=(kc == 0), stop=(kc == KC - 1))
                return
            nm, wb = wload(dram_w, c0, ncols)
            for kc in range(KC):
                P.op("pe", "matmul", [nm, "hT%d" % kc], ["p%d" % bank], pb[bank][:, 0:ncols], lhsT=hT[:, kc, :], rhs=wb[:, kc, 0:ncols],
                     start=(kc == 0), stop=(kc == KC - 1))

        def out_proj(onm, oap, dram_w, gi, xa, xname, bname=None):
            useb = BF16_PROJ and bname is not None
            if useb:
                oT = SV(5, 1024, 1536).bitcast(BF16).rearrange("p (a b) -> p a b", a=8)
            else:
                oT = SV(5, 1024, 2048).rearrange("p (a b) -> p a b", a=8)
            for kc in range(KC):
                bnk = kc // 4
                P.op("pe", "transpose", [onm, "const"], ["p%d" % bnk], out=pb[bnk][:, (kc % 4) * 128:(kc % 4 + 1) * 128],
                     in_=oap[:, kc * 128:(kc + 1) * 128], identity=ident[:])
            P.op("act", "activation", ["p0"], ["oT0"], out=oT[:, 0:4, :], in_=pb[0][:, :].rearrange("p (a b) -> p a b", a=4), func=ACT.Copy)
            P.op("dve", "tensor_copy", ["p1"], ["oT1"], out=oT[:, 4:8, :], in_=pb[1][:, :].rearrange("p (a b) -> p a b", a=4))
            for half in range(2):
                if useb:
                    nm, wb = wload_b(bname, half * 512, 512)
                else:
                    nm, wb = wload(dram_w, half * 512, 512)
                bank = 2 + half
                for kc in range(KC):
                    P.op("pe", "matmul", [nm, "oT%d" % (kc // 4)], ["p%d" % bank], pb[bank][:, :], lhsT=oT[:, kc, :], rhs=wb[:, kc, 0:512],
                         start=(kc == 0), stop=(kc == KC - 1))
                P.op("dve", "tensor_tensor", ["p%d" % bank, "gates"], ["ytmp%d" % half], out=sm[:, half * 512:(half + 1) * 512], in0=pb[bank][:, :],
                     in1=gates[:, gi, half * 512:(half + 1) * 512], op=ALU.mult)
                P.op("pool", "tensor_tensor", ["ytmp%d" % half, xname], [xname], out=xa[:, half * 512:(half + 1) * 512],
                     in0=xa[:, half * 512:(half + 1) * 512], in1=sm[:, half * 512:(half + 1) * 512], op=ALU.add)

        def gla(xa, xname, it):
            hTn = modulate(xa, xname, 0, bf16=BF16_PROJ)
            if GLA_CUT <= 0:
                return
            qk = SV(0, 0, 1024)
            la = SV(0, 1024, 1536)
            zb = SV(0, 1536, 2048)
            vv = SV(1, 0, 1024)
            og = SV(1, 1024, 2048)
            eb = SV(2, 0, 512)
            enb = SV(2, 512, 1024)
            ebl = SV(2, 1024, 1536)
            scm = SV(2, 1536, 2048).rearrange("p (a b) -> p a b", a=4)
            qt = SV(3, 0, 512)
            kt = SV(3, 512, 1024)
            kdec = SV(3, 1024, 1536)
            glT = slot[3][0:16, 1536:1664]
            dec = SV(3, 1664, 1672)
            qtT0 = SV(4, 0, 512).rearrange("p (a b) -> p a b", a=4)
            qtT1 = SV(4, 512, 1024).rearrange("p (a b) -> p a b", a=4)
            ktT = SV(4, 1024, 1536).rearrange("p (a b) -> p a b", a=4)
            qtT = SV(4, 1536, 2048).rearrange("p (a b) -> p a b", a=4)
            osb = SV(5, 0, 1024)
            dsts = [(qk[:, 0:512], "q"), (qk[:, 512:1024], "k"), (vv[:, 0:512], "v0"), (vv[:, 512:1024], "v1"),
                    (og[:, 0:512], "og0"), (og[:, 512:1024], "og1")]
            for bi, (dst, dn) in enumerate(dsts):
                bank = 2 + (bi % 2)
                dense_block(hTn, w_in_d, bi * 512, 512, bank, bname="w_in")
                if bi % 2 == 0:
                    P.op("act", "activation", ["p%d" % bank], [dn], out=dst, in_=pb[bank][:, :], func=ACT.Copy)
                else:
                    P.op("dve", "tensor_copy", ["p%d" % bank], [dn], out=dst, in_=pb[bank][:, :])
            if GLA_CUT <= 1:
                return
            nm, wb = wload(w_in_d, 3072, 16)
            for kc in range(KC):
                P.op("pe", "matmul", [nm, "hT%d" % kc], ["p4"], pb[4][0:16, 0:128], lhsT=wb[:, kc, 0:16], rhs=hT[:, kc, :], start=(kc == 0), stop=(kc == KC - 1))
            P.op("dve", "tensor_copy", ["p4"], ["glT"], out=glT, in_=pb[4][0:16, 0:128])
            P.op("pe", "matmul", ["glT", "const"], ["p5"], pb[5][:, :], lhsT=glT, rhs=wg2[:, :], start=True, stop=True)
            P.op("dve", "tensor_tensor", ["p5", "const"], ["zb"], out=zb, in0=pb[5][:, :], in1=bg2row[:], op=ALU.add)
            P.op("act", "activation", ["zb"], ["zb"], out=zb, in_=zb, func=ACT.Exp, scale=-1.0)
            P.op("act", "activation", ["zb"], ["la"], out=la, in_=zb, func=ACT.Ln, bias=1.0)
            if GLA_CUT <= 2:
                return
            P.op("pe", "matmul", ["la", "const"], ["p4"], pb[4][:, :], lhsT=tri[:], rhs=la, start=True, stop=True)
            P.op("pe", "matmul", ["la", "const"], ["p5"], pb[5][:, :], lhsT=blk[:], rhs=la, start=True, stop=True)
            for h in range(4):
                P.op("pe", "matmul", ["la", "const"], ["p6"], pb[6][:, 2 * h:2 * h + 2], lhsT=la[:, h * 128:(h + 1) * 128], rhs=csel[:], start=True, stop=True)
            P.op("act", "activation", ["p4"], ["eb"], out=eb, in_=pb[4][:, :], func=ACT.Exp)
            P.op("act", "activation", ["p4"], ["enb"], out=enb, in_=pb[4][:, :], func=ACT.Exp, scale=-1.0)
            P.op("act", "activation", ["p5"], ["ebl"], out=ebl, in_=pb[5][:, :], func=ACT.Exp)
            P.op("act", "activation", ["p6"], ["dec"], out=dec, in_=pb[6][:, 0:8], func=ACT.Exp)
            P.op("dve", "scalar_tensor_tensor", ["q", "eb"], ["qt"], out=qt, in0=qk[:, 0:512], scalar=float(128 ** -0.5), in1=eb, op0=ALU.mult, op1=ALU.mult)
            P.op("dve", "tensor_tensor", ["k", "enb"], ["kt"], out=kt, in0=qk[:, 512:1024], in1=enb, op=ALU.mult)
            P.op("pool", "tensor_tensor", ["kt", "ebl"], ["kdec"], out=kdec, in0=kt, in1=ebl, op=ALU.mult)
            if GLA_CUT <= 3:
                return
            for h in range(4):
                P.op("pe", "transpose", ["qt", "const"], ["p0"], out=pb[0][:, h * 128:(h + 1) * 128], in_=qt[:, h * 128:(h + 1) * 128], identity=ident[:])
            for h in range(4):
                P.op("pe", "transpose", ["kt", "const"], ["p1"], out=pb[1][:, h * 128:(h + 1) * 128], in_=kt[:, h * 128:(h + 1) * 128], identity=ident[:])
            p0v = pb[0][:, :].rearrange("p (a b) -> p a b", a=4)
            P.op("act", "activation", ["p0"], ["qtT"], out=qtT, in_=p0v, func=ACT.Copy)
            P.op("pool", "memset", [], ["qtT0", "qtT1"], slot[4][:, 0:1024], 0.0)
            P.op("dve", "tensor_copy", ["p0"], ["qtT0"], out=qtT0[:, :, 0:64], in_=p0v[:, :, 0:64])
            P.op("dve", "tensor_copy", ["p0"], ["qtT1"], out=qtT1[:, :, 64:128], in_=p0v[:, :, 64:128])
            P.op("act", "activation", ["p1"], ["ktT"], out=ktT, in_=pb[1][:, :].rearrange("p (a b) -> p a b", a=4), func=ACT.Copy)
            if GLA_CUT <= 4:
                return
            for h in range(4):
                P.op("pe", "matmul", ["ktT", "qtT"], ["p4"], pb[4][:, h * 128:(h + 1) * 128], lhsT=ktT[:, h, :], rhs=qtT[:, h, :], start=True, stop=True)
            P.op("dve", "tensor_tensor", ["p4", "const"], ["scm"], out=scm, in0=pb[4][:, :].rearrange("p (a b) -> p a b", a=4),
                 in1=maskT[:].unsqueeze(1).to_broadcast([128, 4, 128]), op=ALU.mult)
            if GLA_CUT <= 5:
                return
            Sa, Sb = Sst[0], Sst[1]
            for h in range(4):
                vh = vv[:, h * 256:(h + 1) * 256]
                vn = "v%d" % (h // 2)
                kvb = 5 + (h % 2)
                ob = 2 + (h // 2)
                oreg = pb[ob][:, (h % 2) * 256:(h % 2 + 1) * 256]
                P.op("pe", "matmul", ["kdec", vn], ["p%d" % kvb], pb[kvb][:, 0:256], lhsT=kdec[0:64, h * 128:(h + 1) * 128], rhs=vv[0:64, h * 256:(h + 1) * 256],
                     start=True, stop=True)
                P.op("dve", "scalar_tensor_tensor", ["Sa", "dec", "p%d" % kvb], ["Sb"], out=Sb[:, h, :], in0=Sa[:, h, :], scalar=dec[:, 2 * h:2 * h + 1],
                     in1=pb[kvb][:, 0:256], op0=ALU.mult, op1=ALU.add)
                P.op("pe", "matmul", ["scm", vn], ["p%d" % ob], oreg, lhsT=scm[:, h, :], rhs=vh, start=True, stop=False)
                P.op("pe", "matmul", ["qtT0", "Sa"], ["p%d" % ob], oreg, lhsT=qtT0[:, h, :], rhs=Sa[:, h, :], start=False, stop=False)
                P.op("pe", "matmul", ["qtT1", "Sb"], ["p%d" % ob], oreg, lhsT=qtT1[:, h, :], rhs=Sb[:, h, :], start=False, stop=True)
                P.op("pe", "matmul", ["kdec", vn], ["p%d" % kvb], pb[kvb][:, 256:512], lhsT=kdec[64:128, h * 128:(h + 1) * 128], rhs=vv[64:128, h * 256:(h + 1) * 256],
                     start=True, stop=True)
                P.op("dve", "scalar_tensor_tensor", ["Sb", "dec", "p%d" % kvb], ["Sa"], out=Sa[:, h, :], in0=Sb[:, h, :], scalar=dec[:, 2 * h + 1:2 * h + 2],
                     in1=pb[kvb][:, 256:512], op0=ALU.mult, op1=ALU.add)
            if GLA_CUT <= 6:
                return
            for h in range(4):
                ob = 2 + (h // 2)
                oreg = pb[ob][:, (h % 2) * 256:(h % 2 + 1) * 256]
                P.op("act", "activation", ["p%d" % ob], ["junk", "oss%d" % h], out=junk[:, 0:256], in_=oreg, func=ACT.Square, accum_out=ss[:, 4 + h:5 + h])
            P.op("dve", "tensor_scalar", ["oss%d" % h for h in range(4)], ["orv"], out=sm[:, 0:4], in0=ss[:, 4:8], scalar1=1.0 / 256, scalar2=EPS, op0=ALU.mult, op1=ALU.add)
            P.op("act", "activation", ["orv"], ["ors"], out=sm[:, 4:8], in_=sm[:, 0:4], func=ACT.Sqrt)
            P.op("dve", "reciprocal", ["ors"], ["orr"], out=sm[:, 8:12], in_=sm[:, 4:8])
            for h in range(4):
                ob = 2 + (h // 2)
                oreg = pb[ob][:, (h % 2) * 256:(h % 2 + 1) * 256]
                P.op("dve", "scalar_tensor_tensor", ["p%d" % ob, "orr", "const"], ["osb"], out=osb[:, h * 256:(h + 1) * 256], in0=oreg, scalar=sm[:, 8 + h:9 + h],
                     in1=gnrow[:], op0=ALU.mult, op1=ALU.mult)
            P.op("act", "activation", ["og0", "og1"], ["ogs"], out=og, in_=og, func=ACT.Silu)
            P.op("dve", "tensor_tensor", ["osb", "ogs"], ["osb"], out=osb, in0=osb, in1=og, op=ALU.mult)
            if GLA_CUT <= 7:
                return
            out_proj("osb", osb, gwo_d, 0, xa, xname, bname="gwo")

        def kv_phase(xa, xname, it):
            cur = it % 2
            hTn = modulate(xa, xname, 4, bf16=BF16_PROJ, need32=not BF16_PROJ)
            dense_block(hTn, kvw_d, 0, 512, 2, bname="kvw")
            kdup = SV(6, 0, 512).rearrange("p (g c d) -> p g c d", g=4, c=2)
            tmp = SV(6, 512, 768).rearrange("p (a g d) -> p a g d", a=8, g=4)
            kp = pb[2][:, 0:256].rearrange("p (g d) -> p g d", g=4)
            P.op("act", "activation", ["p2"], ["v%d" % cur], out=vbd[cur][:], in_=pb[2][:, 256:512], func=ACT.Copy)
            P.op("dve", "tensor_copy", ["p2"], ["kdup"], out=kdup[:, :, 0, :], in_=kp)
            cb = cosT[:, it, :].unsqueeze(1).to_broadcast([128, 4, 8])
            sbb = sinT[:, it, :].unsqueeze(1).to_broadcast([128, 4, 8])
            x1 = kdup[:, :, 0, 0:8]
            x2 = kdup[:, :, 0, 8:16]
            P.op("dve", "tensor_tensor", ["kdup"], ["t0"], out=tmp[:, 0], in0=x1, in1=cb, op=ALU.mult)
            P.op("dve", "tensor_tensor", ["kdup"], ["t1"], out=tmp[:, 1], in0=x2, in1=sbb, op=ALU.mult)
            P.op("dve", "tensor_tensor", ["kdup"], ["t2"], out=tmp[:, 2], in0=x2, in1=cb, op=ALU.mult)
            P.op("dve", "tensor_tensor", ["kdup"], ["t3"], out=tmp[:, 3], in0=x1, in1=sbb, op=ALU.mult)
            P.op("dve", "tensor_tensor", ["t0", "t1"], ["kdup"], out=x1, in0=tmp[:, 0], in1=tmp[:, 1], op=ALU.subtract)
            P.op("dve", "tensor_tensor", ["t2", "t3"], ["kdup"], out=x2, in0=tmp[:, 2], in1=tmp[:, 3], op=ALU.add)
            P.op("dve", "tensor_copy", ["kdup"], ["kdup"], out=kdup[:, :, 1, :], in_=kdup[:, :, 0, :])
            kflat = SV(6, 0, 512)
            for g in range(4):
                P.op("pe", "transpose", ["kdup", "const"], ["p0"], out=pb[0][:, g * 128:(g + 1) * 128], in_=kflat[:, g * 128:(g + 1) * 128], identity=ident[:])
            P.op("act", "activation", ["p0"], ["k%d" % cur], out=kTd[cur][:], in_=pb[0][:, :].rearrange("p (a b) -> p a b", a=4), func=ACT.Copy)

        def swa(xa, xname, it):
            cur = it % 2
            prv = 1 - cur
            hTn = modulate(xa, xname, 6, bf16=BF16_PROJ, need32=not BF16_PROJ)
            q = SV(0, 0, 1024)
            q3 = q.rearrange("p (h d) -> p h d", h=16)
            qT = SV(0, 1024, 2048).rearrange("p (a b) -> p a b", a=8)
            sc = [SV(1, 0, 2048).rearrange("p (h m) -> p h m", h=8), SV(2, 0, 2048).rearrange("p (h m) -> p h m", h=8)]
            pT = [SV(3, 0, 2048).rearrange("p (h c m) -> p h c m", h=8, c=2), SV(4, 0, 2048).rearrange("p (h c m) -> p h c m", h=8, c=2)]
            o = SV(5, 0, 1024)
            tmp = SV(6, 1024, 2048).rearrange("p (a h d) -> p a h d", a=4, h=16)
            for half in range(2):
                dense_block(hTn, swq_d, half * 512, 512, 2 + half, bname="swq")
                if half == 0:
                    P.op("act", "activation", ["p2"], ["q"], out=q[:, 0:512], in_=pb[2][:, :], func=ACT.Copy)
                else:
                    P.op("dve", "tensor_copy", ["p3"], ["q"], out=q[:, 512:1024], in_=pb[3][:, :])
            if SWA_CUT <= 1:
                return
            cb = cosT[:, it, :].unsqueeze(1).to_broadcast([128, 16, 8])
            sbb = sinT[:, it, :].unsqueeze(1).to_broadcast([128, 16, 8])
            x1 = q3[:, :, 0:8]
            x2 = q3[:, :, 8:16]
            tv = SV(6, 1024, 1536).rearrange("p (a h d) -> p a h d", a=4, h=16)
            P.op("dve", "tensor_tensor", ["q"], ["t0"], out=tv[:, 0], in0=x1, in1=cb, op=ALU.mult)
            P.op("dve", "tensor_tensor", ["q"], ["t1"], out=tv[:, 1], in0=x2, in1=sbb, op=ALU.mult)
            P.op("dve", "tensor_tensor", ["q"], ["t2"], out=tv[:, 2], in0=x2, in1=cb, op=ALU.mult)
            P.op("dve", "tensor_tensor", ["q"], ["t3"], out=tv[:, 3], in0=x1, in1=sbb, op=ALU.mult)
            P.op("dve", "tensor_tensor", ["t0", "t1"], ["q"], out=x1, in0=tv[:, 0], in1=tv[:, 1], op=ALU.subtract)
            P.op("dve", "tensor_tensor", ["t2", "t3"], ["q"], out=x2, in0=tv[:, 2], in1=tv[:, 3], op=ALU.add)
            if SWA_CUT <= 2:
                return
            for j in range(8):
                bnk = j // 4
                P.op("pe", "transpose", ["q", "const"], ["p%d" % bnk], out=pb[bnk][:, (j % 4) * 128:(j % 4 + 1) * 128], in_=q[:, j * 128:(j + 1) * 128], identity=ident[:])
            P.op("act", "activation", ["p0"], ["qT0"], out=qT[:, 0:4, :], in_=pb[0][:, :].rearrange("p (a b) -> p a b", a=4), func=ACT.Copy)
            P.op("dve", "tensor_copy", ["p1"], ["qT1"], out=qT[:, 4:8, :], in_=pb[1][:, :].rearrange("p (a b) -> p a b", a=4))
            if SWA_CUT <= 3:
                return
            mk = swam[:, 0 if it == 0 else 1, :]
            for grp in range(4):
                bankE = 4 + 2 * (grp % 2)
                bankO = bankE + 1
                for pj in range(2):
                    j = 2 * grp + pj
                    for hh in range(2):
                        h = 2 * j + hh
                        g = h // 4
                        base = 64 * hh
                        bank = bankE if hh == 0 else bankO
                        col = pj * 256
                        P.op("pe", "matmul", ["qT%d" % (j // 4), "k%d" % prv], ["p%d" % bank], pb[bank][:, col:col + 128],
                             lhsT=qT[base:base + 64, j, :], rhs=kTd[prv][base:base + 64, g, :], start=True, stop=True)
                        P.op("pe", "matmul", ["qT%d" % (j // 4), "k%d" % cur], ["p%d" % bank], pb[bank][:, col + 128:col + 256],
                             lhsT=qT[base:base + 64, j, :], rhs=kTd[cur][base:base + 64, g, :], start=True, stop=True)
                for hh in range(2):
                    bank = bankE if hh == 0 else bankO
                    for pj in range(2):
                        j = 2 * grp + pj
                        h = 2 * j + hh
                        P.op("dve", "scalar_tensor_tensor", ["p%d" % bank, "const"], ["sc%d" % j], out=sc[h // 8][:, h % 8, :],
                             in0=pb[bank][:, pj * 256:(pj + 1) * 256], scalar=0.125, in1=mk, op0=ALU.mult, op1=ALU.add)
            if SWA_CUT <= 4:
                return
            rmax = sm[:, 0:16]
            mm = sm[:, 16:32]
            negm = sm[:, 32:48]
            rs = sm[:, 48:64]
            sk = sm[:, 64:80]
            den = sm[:, 80:96]
            rden = sm[:, 96:112]
            for half in range(2):
                P.op("dve", "tensor_reduce", ["sc%d" % j for j in range(half * 4, half * 4 + 4)], ["rmax%d" % half], out=rmax[:, half * 8:(half + 1) * 8],
                     in_=sc[half], axis=AX.X, op=ALU.max)
            P.op("dve", "tensor_tensor", ["rmax0", "rmax1", "const"], ["mm"], out=mm, in0=rmax, in1=sinkrow[:], op=ALU.max)
            P.op("dve", "tensor_scalar", ["mm"], ["negm"], out=negm, in0=mm, scalar1=-1.0, scalar2=None, op0=ALU.mult)
            P.op("dve", "tensor_tensor", ["mm", "const"], ["sk"], out=sk, in0=sinkrow[:], in1=mm, op=ALU.subtract)
            P.op("act", "activation", ["sk"], ["sk"], out=sk, in_=sk, func=ACT.Exp)
            for h in range(16):
                j = h // 2
                sl = sc[h // 8][:, h % 8, :]
                P.op("act", "activation", ["sc%d" % j, "negm"], ["sc%d" % j, "rs%d" % h], out=sl, in_=sl, func=ACT.Exp, bias=negm[:, h:h + 1], scale=1.0,
                     accum_out=rs[:, h:h + 1])
            P.op("dve", "tensor_tensor", ["rs%d" % h for h in range(16)] + ["sk"], ["den"], out=den, in0=rs, in1=sk, op=ALU.add)
            P.op("dve", "reciprocal", ["den"], ["rden"], out=rden, in_=den)
            if SWA_CUT <= 5:
                return
            for j in range(8):
                bank = j % 4
                for hh in range(2):
                    h = 2 * j + hh
                    for part in range(2):
                        P.op("pe", "transpose", ["sc%d" % j, "const"], ["p%d" % bank], out=pb[bank][:, (hh * 2 + part) * 128:(hh * 2 + part + 1) * 128],
                             in_=sc[h // 8][:, h % 8, part * 128:(part + 1) * 128], identity=ident[:])
                dstv = pT[j // 4][:, 2 * (j % 4):2 * (j % 4) + 2, :, :]
                srcv = pb[bank][:, :].rearrange("p (h c m) -> p h c m", h=2, c=2)
                if j % 2 == 0:
                    P.op("act", "activation", ["p%d" % bank], ["pT%d" % j], out=dstv, in_=srcv, func=ACT.Copy)
                else:
                    P.op("dve", "tensor_copy", ["p%d" % bank], ["pT%d" % j], out=dstv, in_=srcv)
            if SWA_CUT <= 6:
                return
            for h in range(16):
                j = h // 2
                g = h // 4
                bank = 4 + h // 8
                oreg = pb[bank][:, (h % 8) * 64:(h % 8 + 1) * 64]
                P.op("pe", "matmul", ["pT%d" % j, "v%d" % prv], ["p%d" % bank], oreg, lhsT=pT[h // 8][:, h % 8, 0, :], rhs=vbd[prv][:, g * 64:(g + 1) * 64],
                     start=True, stop=False)
                P.op("pe", "matmul", ["pT%d" % j, "v%d" % cur], ["p%d" % bank], oreg, lhsT=pT[h // 8][:, h % 8, 1, :], rhs=vbd[cur][:, g * 64:(g + 1) * 64],
                     start=False, stop=True)
            if SWA_CUT <= 7:
                return
            for b2 in range(2):
                P.op("dve", "tensor_tensor", ["p%d" % (4 + b2), "rden"], ["o"], out=o[:, b2 * 512:(b2 + 1) * 512].rearrange("p (h d) -> p h d", h=8),
                     in0=pb[4 + b2][:, :].rearrange("p (h d) -> p h d", h=8), in1=rden[:, b2 * 8:(b2 + 1) * 8].unsqueeze(2).to_broadcast([128, 8, 64]), op=ALU.mult)
            if SWA_CUT <= 8:
                return
            out_proj("o", o, swo_d, 2, xa, xname, bname="swo")

        def peer(xa, xname, l):
            mi = 2 if l == 0 else 8
            gi = 1 if l == 0 else 3
            hTn = modulate(xa, xname, mi, bf16=True, need32=not (BF16_PROJ and BF16_PQ))
            qT = SV(0, 0, 2048).rearrange("p (g t) -> p g t", g=16)
            s = SV(1, 0, 2048).rearrange("p (g n) -> p g n", g=16)
            sw = SV(2, 0, 2048).rearrange("p (g n) -> p g n", g=16)
            cand = SV(3, 0, 2048).rearrange("p (h a b) -> p h a b", h=8, a=16)
            cw = SV(4, 0, 2048).rearrange("p (h a b) -> p h a b", h=8, a=16)
            v16 = sm[:, 0:256].rearrange("p (g k) -> p g k", g=16)
            c16 = sm[:, 256:384].rearrange("p (h k) -> p h k", h=8)
            dd = sm[:, 384:512].rearrange("p (h k) -> p h k", h=8)
            Z = sm[:, 512:520]
            lnZ = sm[:, 520:528]
            bia = sm[:, 528:536]
            for bi in range(4):
                if BF16_PROJ and BF16_PQ:
                    nm, wb = wload_b("pwq%d" % l, bi * 512, 512)
                    rsrc, rn = hTb, ["hTb"] * KC
                else:
                    nm, wb = wload(pwq_d[l], bi * 512, 512)
                    rsrc, rn = hT, ["hT%d" % kc for kc in range(KC)]
                for gl in range(4):
                    g = bi * 4 + gl
                    for kc in range(KC):
                        P.op("pe", "matmul", [nm, rn[kc]], ["p%d" % bi], pb[bi][:, gl * 128:(gl + 1) * 128], lhsT=wb[:, kc, gl * 128:(gl + 1) * 128], rhs=rsrc[:, kc, :],
                             start=(kc == 0), stop=(kc == KC - 1))
                if bi % 2 == 0:
                    P.op("act", "activation", ["p%d" % bi], ["qT%d" % bi], out=qT[:, bi * 4:bi * 4 + 4, :], in_=pb[bi][:, :].rearrange("p (a b) -> p a b", a=4), func=ACT.Copy)
                else:
                    P.op("dve", "tensor_copy", ["p%d" % bi], ["qT%d" % bi], out=qT[:, bi * 4:bi * 4 + 4, :], in_=pb[bi][:, :].rearrange("p (a b) -> p a b", a=4))
            for g in range(16):
                bank = 4 + g // 4
                P.op("pe", "matmul", ["qT%d" % (g // 4), "const"], ["p%d" % bank], pb[bank][:, (g % 4) * 128:(g % 4 + 1) * 128], lhsT=qT[:, g, :], rhs=skT[:, l, g % 2, :],
                     start=True, stop=True)
            for b4 in range(4):
                bank = 4 + b4
                if b4 % 2 == 0:
                    P.op("act", "activation", ["p%d" % bank], ["s%d" % b4], out=s[:, b4 * 4:b4 * 4 + 4, :], in_=pb[bank][:, :].rearrange("p (a b) -> p a b", a=4), func=ACT.Copy)
                else:
                    P.op("dve", "tensor_copy", ["p%d" % bank], ["s%d" % b4], out=s[:, b4 * 4:b4 * 4 + 4, :], in_=pb[bank][:, :].rearrange("p (a b) -> p a b", a=4))
            snames_all = ["s%d" % b4 for b4 in range(4)]
            for g in range(16):
                P.op("dve", "max", ["s%d" % (g // 4)], ["v16a%d" % g], out=v16[:, g, 0:8], in_=s[:, g, :])
            for g in range(16):
                P.op("dve", "match_replace", ["s%d" % (g // 4), "v16a%d" % g], ["sw%d" % g], out=sw[:, g, :], in_to_replace=v16[:, g, 0:8], in_values=s[:, g, :], imm_value=NEG)
            for g in range(16):
                P.op("dve", "max", ["sw%d" % g], ["v16b%d" % g], out=v16[:, g, 8:16], in_=sw[:, g, :])
            v16n = ["v16a%d" % g for g in range(16)] + ["v16b%d" % g for g in range(16)]
            Ev = sm[:, 536:792].rearrange("p (g k) -> p g k", g=16)
            rZ = sm[:, 520:528]
            E = sw
            swn = ["sw%d" % g for g in range(16)]
            P.op("dve", "tensor_tensor", snames_all + v16n + swn, ["E"], out=E, in0=s, in1=v16[:, :, 0:1].to_broadcast([128, 16, 128]), op=ALU.subtract)
            P.op("act", "activation", ["E"], ["E"], out=E, in_=E, func=ACT.Exp)
            P.op("dve", "tensor_tensor", v16n, ["Ev"], out=Ev, in0=v16, in1=v16[:, :, 0:1].to_broadcast([128, 16, 16]), op=ALU.subtract)
            P.op("act", "activation", ["Ev"], ["Ev"], out=Ev, in_=Ev, func=ACT.Exp)
            E4 = E.rearrange("p (h c) n -> p h c n", c=2)
            Ev4 = Ev.rearrange("p (h c) k -> p h c k", c=2)
            c16n = ["c16a%d" % h for h in range(8)] + ["c16b%d" % h for h in range(8)]
            P.op("dve", "tensor_tensor", ["Ev"], ["cand"], out=cand,
                 in0=Ev4[:, :, 0, :].unsqueeze(3).to_broadcast([128, 8, 16, 16]), in1=Ev4[:, :, 1, :].unsqueeze(2).to_broadcast([128, 8, 16, 16]), op=ALU.mult)
            for h in range(8):
                P.op("dve", "max", ["cand"], ["c16a%d" % h], out=c16[:, h, 0:8], in_=cand[:, h])
            for h in range(8):
                P.op("dve", "match_replace", ["cand", "c16a%d" % h], ["cw%d" % h], out=cw[:, h], in_to_replace=c16[:, h, 0:8], in_values=cand[:, h], imm_value=-1.0)
            for h in range(8):
                P.op("dve", "max", ["cw%d" % h], ["c16b%d" % h], out=c16[:, h, 8:16], in_=cw[:, h])
            P.op("dve", "tensor_reduce", c16n, ["Z"], out=Z, in_=c16, axis=AX.X, op=ALU.add)
            P.op("dve", "reciprocal", ["Z"], ["rZ"], out=rZ, in_=Z)
            P.op("dve", "tensor_tensor", ["E", "rZ"], ["E"], out=E4[:, :, 0, :], in0=E4[:, :, 0, :], in1=rZ.unsqueeze(2).to_broadcast([128, 8, 128]), op=ALU.mult)
            thr = sm[:, 528:536]
            P.op("dve", "scalar_tensor_tensor", c16n + ["rZ"], ["thr"], out=thr, in0=c16[:, :, 15], scalar=float(1.0 - 2.0 ** -20), in1=rZ, op0=ALU.mult, op1=ALU.mult)
            EEs = [SV(i, 0, 1024).rearrange("p (i j) -> p i j", i=8) for i in (5, 6, 0)]
            gtall = SV(7, 0, 2048).bitcast(BF16)
            Gts = [gtall[:, k * 1024:(k + 1) * 1024] for k in range(4)]
            gk = [0]

            def gbuild_head(n, h):
                k = gk[0] % 3
                k4 = gk[0] % 4
                gk[0] += 1
                EE, Gt = EEs[k], Gts[k4]
                een, gtn = "EE%d" % k, "Gt%d" % k4
                eenl = [een + ".%d" % ii for ii in range(8)]
                i0 = n * 8
                if h % 2 == ACT_PAR:
                    for ii in range(8):
                        P.op("act", "activation", ["E", "thr"], [eenl[ii]], out=EE[:, ii, :], in_=E4[:, h, 1, :], func=ACT.Copy, scale=E4[:, h, 0, i0 + ii:i0 + ii + 1])
                else:
                    P.op("dve" if h % 4 == 3 else "pool", "tensor_tensor", ["E", "thr"], eenl, out=EE, in0=E4[:, h, 0, i0:i0 + 8].unsqueeze(2).to_broadcast([128, 8, 128]),
                         in1=E4[:, h, 1, :].unsqueeze(1).to_broadcast([128, 8, 128]), op=ALU.mult)
                P.op("dve", "scalar_tensor_tensor", eenl + ["thr"], [gtn], out=Gt.rearrange("p (i j) -> p i j", i=8), in0=EE, scalar=thr[:, h:h + 1], in1=EE,
                     op0=ALU.is_ge, op1=ALU.mult)
                pend_acc.append((n, h, gtn, Gt))

            pend_acc = []

            def flush_pe_acc():
                for (n, h, gtn, Gt) in pend_acc:
                    for c in range(2):
                        bank = 2 + 2 * (n % 2) + c
                        P.op("pe", "matmul", [gtn, "identb"], ["p%d" % bank], pb[bank][:, :], lhsT=identb[:], rhs=Gt[:, c * 512:(c + 1) * 512], start=(h == 0), stop=(h == 7))
                del pend_acc[:]

            utv = utb_d[l].rearrange("(kc p) e -> p kc e", p=128)
            chain = [None]
            p1b = pb[1][:, :].bitcast(BF16)

            def tail_T(prev):
                (ebp, GA, GAT, sn, wnv, vblk, first, last) = prev
                half = p1b[:, (ebp % 2) * 512:(ebp % 2 + 1) * 512]
                for j in range(4):
                    P.op("pe", "transpose", [sn + ".GA", "identb"], ["p1"], out=half[:, j * 128:(j + 1) * 128], in_=GA[:, j * 128:(j + 1) * 128], identity=identb[:])
                P.op("act", "activation", ["p1"], [sn + ".GAT"], out=GAT, in_=half.rearrange("p (a b) -> p a b", a=4), func=ACT.Copy)

            def tail_O(prev):
                (ebp, GA, GAT, sn, wnv, vblk, first, last) = prev
                for j in range(4):
                    for db in range(2):
                        P.op("pe", "matmul", [sn + ".GAT", wnv], ["p%d" % (6 + db)], pb[6 + db][:, :], lhsT=GAT[:, j, :], rhs=vblk[:, j, db * 512:(db + 1) * 512],
                             start=(first and j == 0), stop=(last and j == 3))

            for h in range(8):
                gbuild_head(0, h)
                if h % 4 == 3:
                    flush_pe_acc()
            for nb in range(32):
                n = nb // 2
                e0 = nb * 512
                i = wsel[0]
                wsel[0] ^= 1
                wnu = "wbuf%du" % i
                wnv = "wbuf%dv" % i
                wbb = wbuf[i][:].rearrange("p a b -> p (a b)").bitcast(BF16)
                ublk = wbb[:, 0:4096].rearrange("p (kc e) -> p kc e", kc=8)
                vblk = wbb[:, 4096:8192].rearrange("p (j d) -> p j d", j=4)
                extra = ["wbuf%d" % i] if nb < 2 else []
                P.dma(wnu, [], [wnu] + extra, [(ublk, utv[:, :, e0:e0 + 512])])
                P.dma(wnv, [], [wnv] + extra, [(vblk, vb_d[l][e0:e0 + 512, :].rearrange("(j p) d -> p j d", p=128))])
                si = 10 + (nb % 2)
                sn = "slot%d" % si
                Ag = SV(si, 0, 512)
                GA = SV(si, 512, 768).bitcast(BF16)
                GAT = SV(si, 1024, 1280).bitcast(BF16).rearrange("p (a b) -> p a b", a=4)
                for kc in range(KC):
                    P.op("pe", "matmul", [wnu, "hTb"], ["p0"], pb[0][:, :], lhsT=hTb[:, kc, :], rhs=ublk[:, kc, :], start=(kc == 0), stop=(kc == KC - 1))
                P.op("act", "activation", ["p0"], [sn + ".Ag"], out=Ag, in_=pb[0][:, :], func=ACT.Gelu)
                if chain[0] is not None:
                    tail_T(chain[0])
                flush_pe_acc()
                gbank = 2 + 2 * (n % 2) + (nb % 2)
                P.op("dve", "tensor_tensor", [sn + ".Ag", "p%d" % gbank], [sn + ".GA"], out=GA, in0=pb[gbank][:, :], in1=Ag, op=ALU.mult)
                if chain[0] is not None:
                    tail_O(chain[0])
                if n < 15:
                    for h in range(4 * (nb % 2), 4 * (nb % 2) + 4):
                        gbuild_head(n + 1, h)
                chain[0] = (nb, GA, GAT, sn, wnv, vblk, nb == 0, nb == 31)
            tail_T(chain[0])
            tail_O(chain[0])
            for db in range(2):
                P.op("dve", "tensor_tensor", ["p%d" % (6 + db), "gates"], ["ytmp%d" % db], out=slot[8][:, db * 512:(db + 1) * 512],
                     in0=pb[6 + db][:, :], in1=gates[:, gi, db * 512:(db + 1) * 512], op=ALU.mult)
                P.op("pool", "tensor_tensor", ["ytmp%d" % db, xname], [xname], out=xa[:, db * 512:(db + 1) * 512], in0=xa[:, db * 512:(db + 1) * 512],
                     in1=slot[8][:, db * 512:(db + 1) * 512], op=ALU.add)

        def final_norm(xa, xname, it):
            P.op("act", "activation", [xname], ["junk", "ss0"], out=junk[:], in_=xa, func=ACT.Square, accum_out=ss[:, 0:1])
            P.op("dve", "tensor_scalar", ["ss0"], ["ss1"], out=ss[:, 1:2], in0=ss[:, 0:1], scalar1=1.0 / D, scalar2=EPS, op0=ALU.mult, op1=ALU.add)
            P.op("act", "activation", ["ss1"], ["ss2"], out=ss[:, 2:3], in_=ss[:, 1:2], func=ACT.Sqrt)
            P.op("dve", "reciprocal", ["ss2"], ["ss3"], out=ss[:, 3:4], in_=ss[:, 2:3])
            P.op("dve", "scalar_tensor_tensor", [xname, "ss3", "const"], ["xn"], out=xn[:], in0=xa, scalar=ss[:, 3:4], in1=gfrow[:], op0=ALU.mult, op1=ALU.mult)
            P.dma("yout", ["xn"], [], [(y_d[it * 128:(it + 1) * 128, :], xn[:])])

        stages = os.environ.get("YOCO_STAGES", "gla,peer0,kv,swa,peer1").split(",")
        for it in range(NT):
            xa = xt[it % 2][:]
            xname = "xt%d" % (it % 2)
            P.dma(xname, [], [xname], [(xa, x_d[it * 128:(it + 1) * 128, :])])
            if "gla" in stages:
                gla(xa, xname, it)
                P.barrier()
            if "peer0" in stages:
                peer(xa, xname, 0)
                P.barrier()
            if "kv" in stages:
                kv_phase(xa, xname, it)
                P.barrier()
            if "swa" in stages:
                swa(xa, xname, it)
                P.barrier()
            if "peer1" in stages:
                peer(xa, xname, 1)
                P.barrier()
            final_norm(xa, xname, it)
            P.barrier()
        P.barrier()
        P.emit()
        print("yoco build: ops", P.nops, {e: P.cnt[e] for e in P.ENG}, flush=True)
    return nc


def prep_inputs(inp, NT, nb=8):
    f = np.float32
    S = NT * 128
    t = np.arange(128)
    same = (t[:, None] // 64) == (t[None, :] // 64)
    tri = (same & (t[:, None] <= t[None, :])).astype(f) * f(-1.0 / 16)
    blk = same.astype(f) * f(-1.0 / 16)
    csel = ((t[:, None] // 64) == np.arange(2)[None, :]).astype(f) * f(-1.0 / 16)
    maskT = (same & (t[:, None] <= t[None, :])).astype(f)
    qi = np.arange(128)[:, None]
    mi = np.arange(256)[None, :]
    valid = (mi > qi) & (mi <= qi + 128)
    swam = np.stack([np.where(valid & (mi >= 128), 0.0, NEG), np.where(valid, 0.0, NEG)], axis=1).astype(f)
    invf = (np.float32(500000.0) ** (-(np.arange(0, 16, 2, dtype=np.float32)) / np.float32(16))).astype(f)
    invf = np.ascontiguousarray(np.broadcast_to(invf[None, :], (128, 8)))

    def col(v):
        return np.ascontiguousarray(v.reshape(8, 128).T)

    def row(v):
        return np.ascontiguousarray(np.broadcast_to(v[None, :], (128, v.shape[0])))

    mod_b = inp["mod_b"]
    shared = {
        "invf": invf, "ident": np.eye(128, dtype=f), "tri": tri, "blk": blk, "csel": csel, "maskT": maskT, "swam": swam,
        "modw0": np.ascontiguousarray(inp["mod_w"][0]), "modw1": np.ascontiguousarray(inp["mod_w"][1]),
        "modbcol": np.ascontiguousarray(np.stack([mod_b[l].reshape(48, 128).T for l in range(2)], axis=1)),
        "gaterow": np.ascontiguousarray(np.stack([row(mod_b[0, 2048:3072]), row(mod_b[0, 5120:6144]),
                                                   row(mod_b[1, 2048:3072]), row(mod_b[1, 5120:6144])], axis=1)),
        "kvmodw": np.ascontiguousarray(inp["kv_mod_w"]),
        "kvmodbcol": np.ascontiguousarray(inp["kv_mod_b"].reshape(16, 128).T),
        "ngcol": np.ascontiguousarray(np.stack([col(inp["norm_g"][0, 0]), col(inp["norm_g"][0, 1]), col(inp["norm_g"][1, 0]),
                                                col(inp["norm_g"][1, 1]), col(inp["kv_norm_g"])], axis=1)),
        "gfrow": row(inp["final_norm_g"]),
        "w_in": np.ascontiguousarray(inp["gla_w_in"][0]), "wg2": np.ascontiguousarray(inp["gla_w_g2"][0]),
        "bg2row": row(inp["gla_b_g2"][0]), "gnrow": row(inp["gla_norm_g"][0]), "gwo": np.ascontiguousarray(inp["gla_w_out"][0]),
        "kvw": np.ascontiguousarray(inp["kv_w"]), "swq": np.ascontiguousarray(inp["swa_w_q"][0]),
        "sinkrow": row(inp["swa_sinks"][0]), "swo": np.ascontiguousarray(inp["swa_w_out"][0]),
        "pwq0": np.ascontiguousarray(inp["peer_w_q"][0]), "pwq1": np.ascontiguousarray(inp["peer_w_q"][1]),
        "skT": np.ascontiguousarray(np.transpose(inp["peer_subkeys"], (3, 0, 1, 2))),
        "ut0": np.ascontiguousarray(inp["peer_u"][0].T), "ut1": np.ascontiguousarray(inp["peer_u"][1].T),
        "v0": np.ascontiguousarray(inp["peer_v"][0]), "v1": np.ascontiguousarray(inp["peer_v"][1]),
    }
    maps = []
    for b in range(nb):
        m = dict(shared)
        m["x"] = np.ascontiguousarray(inp["x"][b, :S])
        m["ccol"] = col(inp["c"][b])
        m["pos"] = np.ascontiguousarray(inp["positions"][b, :S].reshape(NT, 128).T.astype(np.int32))
        maps.append(m)
    return maps


_NC_CACHE = {}


def kernel(**inputs):
    inputs = {k: np.asarray(v) for k, v in inputs.items()}
    NT = SEQ // 128
    if NT not in _NC_CACHE:
        _NC_CACHE[NT] = build(NT)
    nc = _NC_CACHE[NT]
    maps = prep_inputs(inputs, NT)
    res = run_bass_kernel_spmd(nc, maps, core_ids=list(range(8)))
    out = np.stack([np.asarray(r["y"]) for r in res.results], axis=0)
    return out.astype(np.float32)
```
